# Optimizing a Trainium2 kernel written in Bass

```python
import jax, jax.numpy as jnp
from jax import lax
import numpy as np

D_MODEL = 1024
BATCH = 2
SEQ = 16384
DEPTH = 4

D_MIX = D_MODEL
CONV_W = D_MIX // 4
CONV_K = 31
POOL_W = D_MIX // 4
POOL_WINDOWS = (2, 4, 8, 16)
POOL_G = 4
POOL_GW = POOL_W // POOL_G
HEAD_DIM = 64
ATTN_W = D_MIX // 2
N_HEADS = ATTN_W // HEAD_DIM
IDX_HEADS = 4
IDX_DIM = 64
TOPK_MAX = 256
Q_BLOCK = 128
ROPE_THETA = 10000.0
PLE_DIM = 256
EPS = 1e-6
SPLITS = (CONV_W, CONV_W, CONV_W,
          POOL_W, POOL_W,
          ATTN_W, ATTN_W, ATTN_W, ATTN_W,
          IDX_HEADS * IDX_DIM, IDX_DIM, IDX_HEADS)
D_IN = 3 * CONV_W + 2 * POOL_W + 4 * ATTN_W + IDX_HEADS * IDX_DIM + IDX_DIM + IDX_HEADS

kernel_name = "hybrid_conv_pool_dsa_trunk"


def rmsnorm(x, g):
    xf = x.astype(jnp.float32)
    y = xf * lax.rsqrt(jnp.mean(xf * xf, axis=-1, keepdims=True) + EPS)
    return (y * g.astype(jnp.float32)).astype(x.dtype)


def layernorm(x, g, b):
    xf = x.astype(jnp.float32)
    mu = jnp.mean(xf, axis=-1, keepdims=True)
    xc = xf - mu
    y = xc * lax.rsqrt(jnp.mean(xc * xc, axis=-1, keepdims=True) + EPS)
    return (y * g.astype(jnp.float32) + b.astype(jnp.float32)).astype(x.dtype)


def rope(x, pos):
    half = x.shape[-1] // 2
    inv = ROPE_THETA ** (-jnp.arange(half, dtype=jnp.float32) / half)
    ang = pos.astype(jnp.float32)[:, None] * inv[None, :]
    cos = jnp.cos(ang)[None, :, None, :]
    sin = jnp.sin(ang)[None, :, None, :]
    xf = x.astype(jnp.float32)
    x1, x2 = xf[..., :half], xf[..., half:]
    return jnp.concatenate([x1 * cos - x2 * sin, x2 * cos + x1 * sin], axis=-1).astype(x.dtype)


def conv_module(val, glu, dw_w, dw_b, ln_g, ln_b, pw_w, pw_b):
    u = val * jax.nn.sigmoid(glu)
    u = lax.conv_general_dilated(u, dw_w[:, None, :], window_strides=(1,),
                                 padding=[(CONV_K - 1, 0)],
                                 dimension_numbers=('NWC', 'WIO', 'NWC'),
                                 feature_group_count=CONV_W) + dw_b
    u = jax.nn.silu(layernorm(u, ln_g, ln_b))
    return u @ pw_w + pw_b


def pool_mixer(u, w, b, scale):
    B, S, _ = u.shape
    ug = u.reshape(B, S, POOL_G, POOL_GW)
    t = jnp.arange(S)
    outs = []
    for g, win in enumerate(POOL_WINDOWS):
        xg = ug[:, :, g].astype(jnp.float32)
        cs = jnp.cumsum(xg, axis=1)
        lag = jnp.pad(cs, ((0, 0), (win, 0), (0, 0)))[:, :S]
        cnt = jnp.minimum(t + 1, win).astype(jnp.float32)[None, :, None]
        outs.append((cs - lag) / cnt - xg)
    d = jnp.stack(outs, axis=2).astype(u.dtype)
    y = jnp.einsum('bsgc,gcd->bsgd', d, w).reshape(B, S, POOL_W) + b
    return y * scale


def sparse_attention(q, k, v, qi, ki, wi):
    B, S, H, Dh = q.shape
    L = k.shape[1]
    topk = min(TOPK_MAX, L // 4)
    nb = S // Q_BLOCK
    key_pos = jnp.arange(L)
    kf = ki.astype(jnp.float32)

    def to_blocks(a):
        return a.reshape((B, nb, Q_BLOCK) + a.shape[2:]).swapaxes(0, 1)

    def gather(a, i):
        return a[i]

    def block(args):
        qb, qib, wib, start = args
        qpos = start + jnp.arange(Q_BLOCK)
        causal = key_pos[None, :] <= qpos[:, None]
        s = jnp.einsum('bqhd,bsd->bqhs', qib.astype(jnp.float32), kf) * (IDX_DIM ** -0.5)
        score = jnp.einsum('bqh,bqhs->bqs', wib.astype(jnp.float32), jax.nn.relu(s))
        score = jnp.where(causal[None], score, -jnp.inf)
        _, idx = lax.top_k(score, topk)
        kg = jax.vmap(gather)(k, idx)
        vg = jax.vmap(gather)(v, idx)
        logits = jnp.einsum('bqhd,bqkhd->bhqk', qb.astype(jnp.float32),
                            kg.astype(jnp.float32)) * (HEAD_DIM ** -0.5)
        valid = idx <= qpos[None, :, None]
        logits = jnp.where(valid[:, None], logits, -jnp.inf)
        pr = jax.nn.softmax(logits, axis=-1)
        o = jnp.einsum('bhqk,bqkhd->bqhd', pr, vg.astype(jnp.float32))
        return o.astype(q.dtype)

    starts = jnp.arange(nb) * Q_BLOCK
    o = lax.map(block, (to_blocks(q), to_blocks(qi), to_blocks(wi), starts))
    return o.swapaxes(0, 1).reshape(B, S, H * Dh)


def setup_inputs(seed: int = 0) -> dict:
    key = jax.random.key(seed)
    ks = jax.random.split(key, 20)
    f32 = jnp.float32

    def nrm(k, shape, scale):
        return jax.random.normal(k, shape, f32) * scale

    return {
        'x': nrm(ks[0], (BATCH, SEQ, D_MODEL), 1.0),
        'p': nrm(ks[1], (DEPTH, BATCH, SEQ, PLE_DIM), 1.0),
        'norm_g': 1.0 + nrm(ks[2], (DEPTH, D_MODEL), 0.1),
        'w_in': nrm(ks[3], (DEPTH, D_MODEL, D_IN), D_MODEL ** -0.5),
        'b_in': nrm(ks[4], (DEPTH, D_IN), 0.02),
        'conv_dw_w': nrm(ks[5], (DEPTH, CONV_K, CONV_W), CONV_K ** -0.5),
        'conv_dw_b': nrm(ks[6], (DEPTH, CONV_W), 0.02),
        'conv_ln_g': 1.0 + nrm(ks[7], (DEPTH, CONV_W), 0.1),
        'conv_ln_b': nrm(ks[8], (DEPTH, CONV_W), 0.02),
        'conv_pw_w': nrm(ks[9], (DEPTH, CONV_W, CONV_W), CONV_W ** -0.5),
        'conv_pw_b': nrm(ks[10], (DEPTH, CONV_W), 0.02),
        'pool_w': nrm(ks[11], (DEPTH, POOL_G, POOL_GW, POOL_GW), POOL_GW ** -0.5),
        'pool_b': nrm(ks[12], (DEPTH, POOL_W), 0.02),
        'pool_scale': 1.0 + nrm(ks[13], (DEPTH, POOL_W), 0.1),
        'w_out': nrm(ks[14], (DEPTH, D_MIX, D_MODEL), D_MIX ** -0.5),
        'ple_w': nrm(ks[15], (DEPTH, PLE_DIM, D_MODEL), PLE_DIM ** -0.5),
        'ple_gate_w': nrm(ks[16], (DEPTH, D_MODEL, D_MODEL), D_MODEL ** -0.5),
        'final_norm_g': 1.0 + nrm(ks[17], (D_MODEL,), 0.1),
    }


def reference(x, p, norm_g, w_in, b_in, conv_dw_w, conv_dw_b, conv_ln_g, conv_ln_b,
              conv_pw_w, conv_pw_b, pool_w, pool_b, pool_scale, w_out, ple_w,
              ple_gate_w, final_norm_g):
    B, S, _ = x.shape
    pos = jnp.arange(S)
    cut = [int(c) for c in np.cumsum(SPLITS)[:-1]]
    h = x
    for i in range(DEPTH):
        n = rmsnorm(h, norm_g[i])
        z = n @ w_in[i] + b_in[i]
        (c_val, c_glu, c_gate, p_in, p_gate, q, k, v, a_gate,
         qi, ki, wi) = jnp.split(z, cut, axis=-1)
        ya = conv_module(c_val, c_glu, conv_dw_w[i], conv_dw_b[i], conv_ln_g[i],
                         conv_ln_b[i], conv_pw_w[i], conv_pw_b[i]) * jax.nn.silu(c_gate)
        yb = pool_mixer(p_in, pool_w[i], pool_b[i], pool_scale[i]) * jax.nn.silu(p_gate)
        q = rope(q.reshape(B, S, N_HEADS, HEAD_DIM), pos)
        k = rope(k.reshape(B, S, N_HEADS, HEAD_DIM), pos)
        v = v.reshape(B, S, N_HEADS, HEAD_DIM)
        qi = rope(qi.reshape(B, S, IDX_HEADS, IDX_DIM), pos)
        ki = rope(ki[:, :, None, :], pos)[:, :, 0, :]
        yc = sparse_attention(q, k, v, qi, ki, wi) * jax.nn.silu(a_gate)
        h = h + jnp.concatenate([ya, yb, yc], axis=-1) @ w_out[i]
        h = h + (p[i] @ ple_w[i]) * jax.nn.sigmoid(h @ ple_gate_w[i])
    return rmsnorm(h, final_norm_g)
```

```python
from contextlib import ExitStack
import numpy as np
import ml_dtypes
import concourse.bass as bass
import concourse.mybir as mybir
from concourse.bass_utils import run_bass_kernel_spmd


F32 = mybir.dt.float32
BF16 = mybir.dt.bfloat16
ALU = mybir.AluOpType
AF = mybir.ActivationFunctionType
AX = mybir.AxisListType


class Res:
    __slots__ = ("w", "r")

    def __init__(self):
        self.w = None
        self.r = {}


class Prog:
    ENGS = ("pe", "act", "dve", "pool", "sp")
    NDSEM = 24

    def __init__(self, nc, stack):
        self.nc = nc
        self.q = {e: [] for e in self.ENGS}
        self.cnt = {e: 0 for e in self.ENGS}
        self.sems = {}
        for e in ("pe", "act", "dve", "pool"):
            self.sems[e] = stack.enter_context(nc.semaphore("s_" + e))
        self.dsem = {}
        self.dcnt = {}
        self.drr = {}
        for qn in ("sp", "pool"):
            for i in range(self.NDSEM):
                k = "d_%s_%d" % (qn, i)
                self.sems[k] = stack.enter_context(nc.semaphore(k))
                self.dcnt[k] = 0
            self.drr[qn] = 0
        self.stack = stack

    def sb(self, name, shape, dt):
        h = self.stack.enter_context(self.nc.sbuf_tensor(name, list(shape), dt))
        return h

    def ps(self, name, shape, dt):
        h = self.stack.enter_context(self.nc.psum_tensor(name, list(shape), dt))
        return h

    def _deps(self, eng, reads, writes):
        deps = {}

        def add(tok):
            if tok is None:
                return
            k, v = tok
            if eng == "pe" and k == "pe":
                return
            if deps.get(k, 0) < v:
                deps[k] = v

        for r in reads:
            add(r.w)
        for w in writes:
            add(w.w)
            for k, v in w.r.items():
                add((k, v))
        return deps

    def _commit(self, tok, reads, writes):
        k, v = tok
        for r in reads:
            if r.r.get(k, 0) < v:
                r.r[k] = v
        for w in writes:
            w.w = tok
            w.r = {}

    @staticmethod
    def _flat(xs):
        out = []
        for x in xs:
            if isinstance(x, (list, tuple)):
                out.extend(Prog._flat(x))
            else:
                out.append(x)
        return out

    def op(self, eng, fn, reads=(), writes=()):
        reads = self._flat(reads); writes = self._flat(writes)
        deps = self._deps(eng, reads, writes)
        self.cnt[eng] += 1
        tok = (eng, self.cnt[eng])
        self.q[eng].append((deps, fn, tok, 1))
        self._commit(tok, reads, writes)
        return tok

    def dma(self, qn, out, in_, reads=(), writes=(), **kw):
        reads = self._flat(reads); writes = self._flat(writes)
        deps = self._deps(qn, reads, writes)
        i = self.drr[qn]
        self.drr[qn] = (i + 1) % self.NDSEM
        k = "d_%s_%d" % (qn, i)
        if self.dcnt[k] > 0:
            if deps.get(k, 0) < self.dcnt[k]:
                deps[k] = self.dcnt[k]
        self.dcnt[k] += 16
        tok = (k, self.dcnt[k])
        if qn == "pool":
            self.cnt["pool"] += 0
        self.q[qn].append((deps, lambda e: e.dma_start(out=out, in_=in_, **kw), tok, 16))
        self._commit(tok, reads, writes)
        return tok

    def emit(self, final_waits=None):
        nc = self.nc
        engobj = {"pe": "tensor", "act": "scalar", "dve": "vector", "pool": "gpsimd", "sp": "sync"}
        ce = ("pe", "act", "dve", "pool")
        needed = {e: set() for e in ce}
        for en in self.ENGS:
            known = {}
            for deps, fn, tok, inc in self.q[en]:
                for k, v in deps.items():
                    if known.get(k, 0) < v:
                        known[k] = v
                        if k in needed:
                            needed[k].add(v)
            if final_waits and en in final_waits:
                for k, v in final_waits[en].items():
                    if k in needed and known.get(k, 0) < v:
                        needed[k].add(v)
        rank = {e: {v: i + 1 for i, v in enumerate(sorted(needed[e]))} for e in ce}
        with nc.Block() as block:
            for en in self.ENGS:
                q = self.q[en]

                def body(e, q=q, en=en):
                    known = {}

                    def wait(k, v):
                        if known.get(k, 0) < v:
                            known[k] = v
                            e.wait_ge(self.sems[k], rank[k][v] if k in rank else v)

                    for deps, fn, tok, inc in q:
                        for k, v in deps.items():
                            wait(k, v)
                        ins = fn(e)
                        if tok[0] in rank:
                            if tok[1] in rank[tok[0]]:
                                ins.then_inc(self.sems[tok[0]], 1)
                        else:
                            ins.then_inc(self.sems[tok[0]], inc)
                    if final_waits and en in final_waits:
                        for k, v in final_waits[en].items():
                            wait(k, v)

                getattr(block, engobj[en])(body)

    def all_dma_tokens(self):
        return {k: v for k, v in self.dcnt.items() if v > 0}


D = 1024
DIN = 3652
EPS = 1e-6
HALO = 32
NT = 128 + HALO


def bc_mid(ap2d, n):
    a = ap2d.ap
    return AP(ap2d.tensor, ap2d.offset, [list(a[0]), [0, n], list(a[1])])


def bc_last(ap2d, n):
    a = ap2d.ap
    return AP(ap2d.tensor, ap2d.offset, [list(a[0]), list(a[1]), [0, n]])


def build_A(NS, stage=99):
    nc = bass.Bass("TRN2", target_bir_lowering=False)
    dr = lambda n, s, d, k="ExternalInput": nc.dram_tensor(n, list(s), d, kind=k).ap()
    hA = dr("hA", [NS * 128, D], F32)
    hH = dr("hH", [NS * HALO, D], F32)
    hok = dr("hok", [128, NS], F32)
    cs = dr("cs", [NS * 128, 64], F32)
    w_in = dr("w_in", [D, DIN], F32)
    b_bc = dr("b_bc", [1, DIN], F32)
    b_fm = dr("b_fm", [128, 10], F32)
    g_bc = dr("g_bc", [1, D], F32)
    wdw = dr("wdw", [128, 2, 31], F32)
    cvec = dr("cvec", [128, 7, 2], F32)
    pw_w = dr("pw_w", [128, 2, 256], F32)
    plw = dr("plw", [128, 2, 128], F32)
    rc0 = dr("rc0", [128, 2, 128], F32)
    ident_d = dr("ident", [128, 128], F32)
    O = "ExternalOutput"
    kT_o = dr("kT_o", [NS, 128, 4, 128], BF16, O)
    va_o = dr("va_o", [NS, 128, 528], BF16, O)
    ki_o = dr("ki_o", [NS, 64, 128], BF16, O)
    qT_o = dr("qT_o", [NS, 128, 4, 128], BF16, O)
    qi_o = dr("qi_o", [NS, 128, 2, 128], BF16, O)
    sg_o = dr("sg_o", [NS, 128, 4], F32, O)
    ag_o = dr("ag_o", [NS, 128, 512], BF16, O)
    yab_o = dr("yab_o", [NS, 128, 4, 128], BF16, O)

    with ExitStack() as st:
        P = Prog(nc, st)
        sb, ps = P.sb, P.ps
        Wb = sb("Wb", [128, 8, DIN], BF16); rWb = Res()
        bbc = sb("bbc", [128, DIN], F32); rbbc = Res()
        bfm = sb("bfm", [128, 10], F32); rbfm = Res()
        gbc = sb("gbc", [128, D], F32); rgbc = Res()
        wdw_t = sb("wdw_t", [128, 2, 31], F32); rwdw = Res()
        cv = sb("cv", [128, 7, 2], F32); rcv = Res()
        pwb = sb("pwb", [128, 2, 256], BF16); rpwb = Res()
        plb = sb("plb", [128, 2, 128], BF16); rplb = Res()
        rc0_t = sb("rc0_t", [128, 2, 128], F32); rrc0 = Res()
        hok_t = sb("hok_t", [128, NS], F32); rhok = Res()
        idf = sb("idf", [128, 128], F32); ridf = Res()
        idb = sb("idb", [128, 128], BF16); ridb = Res()
        onesf = sb("onesf", [128, 128], F32); rones = Res()
        stg = [sb("stg%d" % i, [128, 1024], F32) for i in range(2)]; rstg = [Res(), Res()]

        P.dma("sp", bbc[:], b_bc.partition_broadcast(128), writes=[rbbc])
        P.dma("sp", gbc[:], g_bc.partition_broadcast(128), writes=[rgbc])
        P.dma("sp", bfm[:], b_fm, writes=[rbfm])
        P.dma("sp", wdw_t[:], wdw, writes=[rwdw])
        P.dma("sp", cv[:], cvec, writes=[rcv])
        P.dma("sp", rc0_t[:], rc0, writes=[rrc0])
        P.dma("sp", hok_t[:], hok, writes=[rhok])
        P.dma("sp", idf[:], ident_d, writes=[ridf])
        P.op("dve", lambda e: e.tensor_copy(out=idb[:], in_=idf[:]), reads=[ridf], writes=[ridb])
        P.op("pool", lambda e: e.memset(onesf[:], 1.0 / 256.0), writes=[rones])
        i = 0
        P.dma("sp", stg[i][:, 0:512], pw_w.rearrange("p a b -> p (a b)"), writes=[rstg[i]])
        P.op("dve", lambda e: e.tensor_copy(out=pwb[:].rearrange("p a b -> p (a b)"), in_=stg[0][:, 0:512]),
             reads=[rstg[0]], writes=[rpwb])
        P.dma("sp", stg[1][:, 0:256], plw.rearrange("p a b -> p (a b)"), writes=[rstg[1]])
        P.op("dve", lambda e: e.tensor_copy(out=plb[:].rearrange("p a b -> p (a b)"), in_=stg[1][:, 0:256]),
             reads=[rstg[1]], writes=[rplb])
        ci = 0
        for kc in range(8):
            for c0 in range(0, DIN, 1024):
                cw = min(1024, DIN - c0)
                i = ci % 2
                P.dma("sp", stg[i][:, 0:cw], w_in[kc * 128:(kc + 1) * 128, c0:c0 + cw], writes=[rstg[i]])
                eng = ("dve", "act", "pool")[ci % 3]
                if eng == "act":
                    P.op("act", lambda e, i=i, kc=kc, c0=c0, cw=cw: e.copy(out=Wb[:, kc, c0:c0 + cw], in_=stg[i][:, 0:cw]),
                         reads=[rstg[i]], writes=[rWb])
                else:
                    P.op(eng, lambda e, i=i, kc=kc, c0=c0, cw=cw: e.tensor_copy(out=Wb[:, kc, c0:c0 + cw], in_=stg[i][:, 0:cw]),
                         reads=[rstg[i]], writes=[rWb])
                ci += 1

        def dbl(name, shape, dt):
            return [(sb("%s%d" % (name, i), shape, dt), Res()) for i in range(2)]

        hblk = dbl("hblk", [128, D], F32)
        hhal = dbl("hhal", [HALO, D], F32)
        cst = dbl("cst", [128, 64], F32)
        sq = dbl("sq", [128, D], F32)
        sqh = dbl("sqh", [HALO, D], F32)
        st1 = dbl("st1", [128, 4], F32)
        st1h = dbl("st1h", [HALO, 4], F32)
        hn = dbl("hn", [128, D], BF16)
        hnh = dbl("hnh", [HALO, D], BF16)
        nT = dbl("nT", [128, 8, NT], BF16)
        val = dbl("val", [128, 2, NT], F32)
        sgl = dbl("sgl", [128, 2, NT], F32)
        u = dbl("u", [128, 2, NT], F32)
        pin = dbl("pin", [128, 2, NT], F32)
        cgate = dbl("cgate", [128, 2, 128], F32)
        pgate = dbl("pgate", [128, 2, 128], F32)
        cacc = dbl("cacc", [128, 2, 128], F32)
        xc = dbl("xc", [128, 2, 128], F32)
        xsq = dbl("xsq", [128, 2, 128], F32)
        rstd_b = dbl("rstd_b", [128, 128], F32)
        yn = dbl("yn", [128, 2, 128], F32)
        sact = dbl("sact", [128, 2, 128], BF16)
        yab = dbl("yab", [128, 4, 128], BF16)
        ps2 = dbl("ps2", [128, 2, NT], F32)
        ps4 = dbl("ps4", [128, 2, NT], F32)
        ps8 = dbl("ps8", [128, NT], F32)
        ps16 = dbl("ps16", [128, NT], F32)
        dpl = dbl("dpl", [128, 2, 128], BF16)
        ptmp = dbl("ptmp", [128, 2, 128], F32)
        ztm = dbl("ztm", [128, 512], F32)
        zt2 = dbl("zt2", [128, 512], F32)
        rt1 = dbl("rt1", [128, 256], F32)
        rt2 = dbl("rt2", [128, 256], F32)
        rot = dbl("rot", [128, 512], BF16)
        qT = dbl("qT", [128, 4, 128], BF16)
        kT = dbl("kT", [128, 4, 128], BF16)
        va = dbl("va", [128, 8, 66], BF16)
        ag1 = dbl("ag1", [128, 512], F32)
        ag = dbl("ag", [128, 512], BF16)
        zi = dbl("zi", [128, 324], F32)
        wia = dbl("wia", [128, 8], F32)
        qik = dbl("qik", [128, 384], BF16)
        qikT = dbl("qikT", [128, 3, 128], BF16)
        for i in range(2):
            P.op("pool", lambda e, i=i: e.memset(qik[i][0][:], 0.0), writes=[qik[i][1]])
        for i in range(2):
            P.op("pool", lambda e, i=i: e.memset(va[i][0][:], 1.0), writes=[va[i][1]])

        pb = [(ps("pb%d" % i, [128, 512], F32), Res()) for i in range(6)]
        pt = [(ps("pt%d" % i, [128, 1024], BF16), Res()) for i in range(2)]
        pbi = [0]
        pti = [0]

        def nextpb():
            t = pb[pbi[0] % len(pb)]; pbi[0] += 1; return t

        def nextpt():
            t = pt[pti[0] % len(pt)]; pti[0] += 1; return t

        FM_CHUNKS = [
            (0, "val"), (128, "val"), (256, "glu"), (384, "glu"), (512, "cgate"), (640, "cgate"),
            (768, "pin"), (896, "pin"), (1024, "pgate"), (1152, "pgate")]

        def do_slot(s):
            b = s % 2
            P.dma("sp", hblk[b][0][:], hA[s * 128:(s + 1) * 128, :], writes=[hblk[b][1]])
            P.dma("sp", hhal[b][0][:], hH[s * HALO:(s + 1) * HALO, :], writes=[hhal[b][1]])
            P.dma("sp", cst[b][0][:], cs[s * 128:(s + 1) * 128, :], writes=[cst[b][1]])
            for (h_, sq_, st_, hn_, np_) in ((hblk[b], sq[b], st1[b], hn[b], 128), (hhal[b], sqh[b], st1h[b], hnh[b], HALO)):
                P.op("act", lambda e, h_=h_, sq_=sq_: e.activation(out=sq_[0][:], in_=h_[0][:], func=AF.Square),
                     reads=[h_[1]], writes=[sq_[1]])
                P.op("dve", lambda e, sq_=sq_, st_=st_: e.tensor_reduce(out=st_[0][:, 0:1], in_=sq_[0][:], axis=AX.X, op=ALU.add),
                     reads=[sq_[1]], writes=[st_[1]])
                P.op("dve", lambda e, st_=st_: e.tensor_scalar(out=st_[0][:, 1:2], in0=st_[0][:, 0:1], scalar1=1.0 / D, scalar2=EPS,
                                                               op0=ALU.mult, op1=ALU.add), reads=[st_[1]], writes=[st_[1]])
                P.op("act", lambda e, st_=st_: e.activation(out=st_[0][:, 2:3], in_=st_[0][:, 1:2], func=AF.Sqrt),
                     reads=[st_[1]], writes=[st_[1]])
                P.op("dve", lambda e, st_=st_: e.reciprocal(out=st_[0][:, 3:4], in_=st_[0][:, 2:3]), reads=[st_[1]], writes=[st_[1]])
                P.op("dve", lambda e, h_=h_, st_=st_, hn_=hn_, np_=np_: e.scalar_tensor_tensor(
                    out=hn_[0][:], in0=h_[0][:], scalar=st_[0][:, 3:4], in1=gbc[0:np_, :], op0=ALU.mult, op1=ALU.mult),
                    reads=[h_[1], st_[1], rgbc], writes=[hn_[1]])
            if stage < 2:
                return
            ptt = nextpt()
            for c in range(8):
                P.op("pe", lambda e, c=c, ptt=ptt: e.transpose(out=ptt[0][:, c * 128:(c + 1) * 128], in_=hn[b][0][:, c * 128:(c + 1) * 128],
                                                              identity=idb[:]),
                     reads=[hn[b][1], ridb], writes=[ptt[1]])
            P.op("act", lambda e, ptt=ptt: e.copy(out=nT[b][0][:, :, HALO:NT], in_=ptt[0][:, :].rearrange("p (c t) -> p c t", c=8)),
                 reads=[ptt[1]], writes=[nT[b][1]])
            ptt = nextpt()
            for c in range(8):
                P.op("pe", lambda e, c=c, ptt=ptt: e.transpose(out=ptt[0][:, c * HALO:(c + 1) * HALO], in_=hnh[b][0][:, c * 128:(c + 1) * 128],
                                                              identity=idb[0:HALO, 0:HALO]),
                     reads=[hnh[b][1], ridb], writes=[ptt[1]])
            P.op("dve", lambda e, ptt=ptt: e.tensor_copy(out=nT[b][0][:, :, 0:HALO],
                                                         in_=ptt[0][:, 0:8 * HALO].rearrange("p (c t) -> p c t", c=8)),
                 reads=[ptt[1]], writes=[nT[b][1]])
            if stage < 3:
                return
            for ci_, (c0, kind) in enumerate(FM_CHUNKS):
                cc = ci_ % 2
                nt0 = 0 if kind in ("val", "glu", "pin") else HALO
                nw = NT - nt0
                pz = nextpb()
                for kc in range(8):
                    P.op("pe", lambda e, kc=kc, pz=pz, c0=c0, nt0=nt0, nw=nw: e.matmul(
                        pz[0][:, 0:nw], lhsT=Wb[:, kc, c0:c0 + 128], rhs=nT[b][0][:, kc, nt0:NT], start=(kc == 0), stop=(kc == 7)),
                        reads=[rWb, nT[b][1]], writes=[pz[1]])
                bias = bfm[:, ci_:ci_ + 1]
                if kind == "val":
                    P.op("act", lambda e, pz=pz, cc=cc, bias=bias: e.activation(out=val[b][0][:, cc, :], in_=pz[0][:, 0:NT], func=AF.Identity, bias=bias),
                         reads=[pz[1], rbfm], writes=[val[b][1]])
                elif kind == "glu":
                    P.op("act", lambda e, pz=pz, cc=cc, bias=bias: e.activation(out=sgl[b][0][:, cc, :], in_=pz[0][:, 0:NT], func=AF.Sigmoid, bias=bias),
                         reads=[pz[1], rbfm], writes=[sgl[b][1]])
                elif kind == "pin":
                    P.op("act", lambda e, pz=pz, cc=cc, bias=bias: e.activation(out=pin[b][0][:, cc, :], in_=pz[0][:, 0:NT], func=AF.Identity, bias=bias),
                         reads=[pz[1], rbfm], writes=[pin[b][1]])
                elif kind == "cgate":
                    P.op("act", lambda e, pz=pz, cc=cc, bias=bias: e.activation(out=cgate[b][0][:, cc, :], in_=pz[0][:, 0:128], func=AF.Silu, bias=bias),
                         reads=[pz[1], rbfm], writes=[cgate[b][1]])
                else:
                    P.op("act", lambda e, pz=pz, cc=cc, bias=bias: e.activation(out=pgate[b][0][:, cc, :], in_=pz[0][:, 0:128], func=AF.Silu, bias=bias),
                         reads=[pz[1], rbfm], writes=[pgate[b][1]])
            if stage < 4:
                return
            P.op("dve", lambda e: e.tensor_tensor(out=u[b][0][:], in0=val[b][0][:], in1=sgl[b][0][:], op=ALU.mult),
                 reads=[val[b][1], sgl[b][1]], writes=[u[b][1]])
            P.op("dve", lambda e: e.tensor_scalar(out=u[b][0][:, :, 0:HALO], in0=u[b][0][:, :, 0:HALO], scalar1=hok_t[:, s:s + 1], scalar2=None,
                                                  op0=ALU.mult), reads=[u[b][1], rhok], writes=[u[b][1]])
            P.op("pool", lambda e: e.tensor_scalar(out=pin[b][0][:, :, 0:HALO], in0=pin[b][0][:, :, 0:HALO], scalar1=hok_t[:, s:s + 1], scalar2=None,
                                                   op0=ALU.mult), reads=[pin[b][1], rhok], writes=[pin[b][1]])
            for cc in range(2):
                eng = "dve"
                for k in range(31):
                    o0 = HALO - 30 + k
                    if k == 0:
                        P.op(eng, lambda e, cc=cc, o0=o0, k=k: e.tensor_scalar(
                            out=cacc[b][0][:, cc, :], in0=u[b][0][:, cc, o0:o0 + 128], scalar1=wdw_t[:, cc, k:k + 1], scalar2=cv[:, 0, cc:cc + 1],
                            op0=ALU.mult, op1=ALU.add), reads=[u[b][1], rwdw, rcv], writes=[cacc[b][1]])
                    else:
                        P.op(eng, lambda e, cc=cc, o0=o0, k=k: e.scalar_tensor_tensor(
                            out=cacc[b][0][:, cc, :], in0=u[b][0][:, cc, o0:o0 + 128], scalar=wdw_t[:, cc, k:k + 1], in1=cacc[b][0][:, cc, :],
                            op0=ALU.mult, op1=ALU.add), reads=[u[b][1], rwdw, cacc[b][1]], writes=[cacc[b][1]])
            pm = nextpb()
            for cc in range(2):
                P.op("pe", lambda e, cc=cc, pm=pm: e.matmul(pm[0][:, 0:128], lhsT=onesf[:], rhs=cacc[b][0][:, cc, :], start=(cc == 0), stop=(cc == 1)),
                     reads=[rones, cacc[b][1]], writes=[pm[1]])
            P.op("dve", lambda e, pm=pm: e.tensor_tensor(out=xc[b][0][:], in0=cacc[b][0][:], in1=bc_mid(pm[0][:, 0:128], 2), op=ALU.subtract),
                 reads=[cacc[b][1], pm[1]], writes=[xc[b][1]])
            P.op("act", lambda e: e.activation(out=xsq[b][0][:], in_=xc[b][0][:], func=AF.Square), reads=[xc[b][1]], writes=[xsq[b][1]])
            pv = nextpb()
            for cc in range(2):
                P.op("pe", lambda e, cc=cc, pv=pv: e.matmul(pv[0][:, 0:128], lhsT=onesf[:], rhs=xsq[b][0][:, cc, :], start=(cc == 0), stop=(cc == 1)),
                     reads=[rones, xsq[b][1]], writes=[pv[1]])
            P.op("act", lambda e, pv=pv: e.activation(out=rstd_b[b][0][:], in_=pv[0][:, 0:128], func=AF.Sqrt, bias=EPS),
                 reads=[pv[1]], writes=[rstd_b[b][1]])
            P.op("dve", lambda e: e.reciprocal(out=rstd_b[b][0][:], in_=rstd_b[b][0][:]), reads=[rstd_b[b][1]], writes=[rstd_b[b][1]])
            P.op("dve", lambda e: e.tensor_tensor(out=yn[b][0][:], in0=xc[b][0][:], in1=bc_mid(rstd_b[b][0][:], 2), op=ALU.mult),
                 reads=[xc[b][1], rstd_b[b][1]], writes=[yn[b][1]])
            for cc in range(2):
                P.op("act", lambda e, cc=cc: e.activation(out=sact[b][0][:, cc, :], in_=yn[b][0][:, cc, :], func=AF.Silu,
                                                          scale=cv[:, 1, cc:cc + 1], bias=cv[:, 2, cc:cc + 1]),
                     reads=[yn[b][1], rcv], writes=[sact[b][1]])
            for oc in range(2):
                pz = nextpb()
                for kc in range(2):
                    P.op("pe", lambda e, oc=oc, kc=kc, pz=pz: e.matmul(pz[0][:, 0:128], lhsT=pwb[:, kc, oc * 128:(oc + 1) * 128], rhs=sact[b][0][:, kc, :],
                                                                       start=(kc == 0), stop=(kc == 1)),
                         reads=[rpwb, sact[b][1]], writes=[pz[1]])
                P.op("dve", lambda e, oc=oc, pz=pz: e.scalar_tensor_tensor(out=yab[b][0][:, oc, :], in0=pz[0][:, 0:128], scalar=cv[:, 3, oc:oc + 1],
                                                                           in1=cgate[b][0][:, oc, :], op0=ALU.add, op1=ALU.mult),
                     reads=[pz[1], rcv, cgate[b][1]], writes=[yab[b][1]])
            if stage < 5:
                return
            pe_ = "pool"
            x_ = pin[b]
            P.op(pe_, lambda e: e.tensor_tensor(out=ps2[b][0][:, :, 1:NT], in0=x_[0][:, :, 1:NT], in1=x_[0][:, :, 0:NT - 1], op=ALU.add),
                 reads=[x_[1]], writes=[ps2[b][1]])
            P.op(pe_, lambda e: e.tensor_tensor(out=ps4[b][0][:, :, 3:NT], in0=ps2[b][0][:, :, 3:NT], in1=ps2[b][0][:, :, 1:NT - 2], op=ALU.add),
                 reads=[ps2[b][1]], writes=[ps4[b][1]])
            P.op(pe_, lambda e: e.tensor_tensor(out=ps8[b][0][:, 7:NT], in0=ps4[b][0][:, 1, 7:NT], in1=ps4[b][0][:, 1, 3:NT - 4], op=ALU.add),
                 reads=[ps4[b][1]], writes=[ps8[b][1]])
            P.op(pe_, lambda e: e.tensor_tensor(out=ps16[b][0][:, 15:NT], in0=ps8[b][0][:, 15:NT], in1=ps8[b][0][:, 7:NT - 8], op=ALU.add),
                 reads=[ps8[b][1]], writes=[ps16[b][1]])
            srcs = [(ps2[b], lambda t: t[0][0:64, 0, HALO:NT], 0, 0), (ps4[b], lambda t: t[0][64:128, 0, HALO:NT], 64, 0),
                    (ps8[b], lambda t: t[0][0:64, HALO:NT], 0, 1), (ps16[b], lambda t: t[0][64:128, HALO:NT], 64, 1)]
            for (src, view, p0, cc) in srcs:
                if s == 0:
                    P.op(pe_, lambda e, src=src, view=view, p0=p0, cc=cc: e.tensor_tensor(
                        out=ptmp[b][0][p0:p0 + 64, cc, :], in0=view(src), in1=rc0_t[p0:p0 + 64, cc, :], op=ALU.mult),
                        reads=[src[1], rrc0], writes=[ptmp[b][1]])
                else:
                    P.op(pe_, lambda e, src=src, view=view, p0=p0, cc=cc: e.tensor_scalar(
                        out=ptmp[b][0][p0:p0 + 64, cc, :], in0=view(src), scalar1=cv[p0:p0 + 64, 6, cc:cc + 1], scalar2=None, op0=ALU.mult),
                        reads=[src[1], rcv], writes=[ptmp[b][1]])
            P.op(pe_, lambda e: e.tensor_tensor(out=dpl[b][0][:], in0=ptmp[b][0][:], in1=x_[0][:, :, HALO:NT], op=ALU.subtract),
                 reads=[ptmp[b][1], x_[1]], writes=[dpl[b][1]])
            for cc in range(2):
                pz = nextpb()
                P.op("pe", lambda e, cc=cc, pz=pz: e.matmul(pz[0][:, 0:128], lhsT=plb[:, cc, :], rhs=dpl[b][0][:, cc, :], start=True, stop=True),
                     reads=[rplb, dpl[b][1]], writes=[pz[1]])
                P.op("dve", lambda e, cc=cc, pz=pz: e.tensor_scalar(out=ptmp[b][0][:, cc, :], in0=pz[0][:, 0:128], scalar1=cv[:, 4, cc:cc + 1],
                                                                    scalar2=cv[:, 5, cc:cc + 1], op0=ALU.add, op1=ALU.mult),
                     reads=[pz[1], rcv], writes=[ptmp[b][1]])
            P.op("pool", lambda e: e.tensor_tensor(out=yab[b][0][:, 2:4, :], in0=ptmp[b][0][:], in1=pgate[b][0][:], op=ALU.mult),
                 reads=[ptmp[b][1], pgate[b][1]], writes=[yab[b][1]])
            P.dma("pool", yab_o[s], yab[b][0][:], reads=[yab[b][1]])

            if stage < 6:
                return
            def tm_group(c0, cw, dst):
                pz = nextpb()
                for kc in range(8):
                    P.op("pe", lambda e, kc=kc, pz=pz: e.matmul(pz[0][:, 0:cw], lhsT=nT[b][0][:, kc, HALO:NT], rhs=Wb[:, kc, c0:c0 + cw],
                                                                start=(kc == 0), stop=(kc == 7)),
                         reads=[rWb, nT[b][1]], writes=[pz[1]])
                P.op("dve", lambda e, pz=pz: e.tensor_tensor(out=dst[0][:, 0:cw], in0=pz[0][:, 0:cw], in1=bbc[:, c0:c0 + cw], op=ALU.add),
                     reads=[pz[1], rbbc], writes=[dst[1]])

            def rope(src, nh, dst, eng="pool"):
                xv = src[0].rearrange("p (h t d) -> p h t d", h=nh, t=2)
                ov = dst[0].rearrange("p (h t d) -> p h t d", h=nh, t=2)
                cosb = bc_mid(cst[b][0][:, 0:32], nh)
                sinb = bc_mid(cst[b][0][:, 32:64], nh)
                t1 = rt1[b][0][:, 0:nh * 32].rearrange("p (h d) -> p h d", h=nh)
                t2 = rt2[b][0][:, 0:nh * 32].rearrange("p (h d) -> p h d", h=nh)
                rd = [src[1], cst[b][1]]
                if nh == 1:
                    x0, x1 = src[0][:, 0:32], src[0][:, 32:64]
                    o0_, o1_ = dst[0][:, 0:32], dst[0][:, 32:64]
                    cb, sn = cst[b][0][:, 0:32], cst[b][0][:, 32:64]
                    a1, a2 = rt1[b][0][:, 0:32], rt2[b][0][:, 0:32]
                    P.op(eng, lambda e: e.tensor_tensor(out=a1, in0=x0, in1=cb, op=ALU.mult), reads=rd, writes=[rt1[b][1]])
                    P.op(eng, lambda e: e.tensor_tensor(out=a2, in0=x1, in1=sn, op=ALU.mult), reads=rd, writes=[rt2[b][1]])
                    P.op(eng, lambda e: e.tensor_tensor(out=o0_, in0=a1, in1=a2, op=ALU.subtract), reads=[rt1[b][1], rt2[b][1]], writes=[dst[1]])
                    P.op(eng, lambda e: e.tensor_tensor(out=a1, in0=x1, in1=cb, op=ALU.mult), reads=rd, writes=[rt1[b][1]])
                    P.op(eng, lambda e: e.tensor_tensor(out=a2, in0=x0, in1=sn, op=ALU.mult), reads=rd, writes=[rt2[b][1]])
                    P.op(eng, lambda e: e.tensor_tensor(out=o1_, in0=a1, in1=a2, op=ALU.add), reads=[rt1[b][1], rt2[b][1]], writes=[dst[1]])
                    return
                P.op(eng, lambda e: e.tensor_tensor(out=t1, in0=xv[:, :, 0, :], in1=cosb, op=ALU.mult), reads=rd, writes=[rt1[b][1]])
                P.op(eng, lambda e: e.tensor_tensor(out=t2, in0=xv[:, :, 1, :], in1=sinb, op=ALU.mult), reads=rd, writes=[rt2[b][1]])
                P.op(eng, lambda e: e.tensor_tensor(out=ov[:, :, 0, :], in0=t1, in1=t2, op=ALU.subtract), reads=[rt1[b][1], rt2[b][1]], writes=[dst[1]])
                P.op(eng, lambda e: e.tensor_tensor(out=t1, in0=xv[:, :, 1, :], in1=cosb, op=ALU.mult), reads=rd, writes=[rt1[b][1]])
                P.op(eng, lambda e: e.tensor_tensor(out=t2, in0=xv[:, :, 0, :], in1=sinb, op=ALU.mult), reads=rd, writes=[rt2[b][1]])
                P.op(eng, lambda e: e.tensor_tensor(out=ov[:, :, 1, :], in0=t1, in1=t2, op=ALU.add), reads=[rt1[b][1], rt2[b][1]], writes=[dst[1]])

            tm_group(1280, 512, ztm[b])
            rope((ztm[b][0][:, 0:512], ztm[b][1]), 8, (rot[b][0][:, 0:512], rot[b][1]), eng="pool")
            ptt = nextpt()
            for hp in range(4):
                P.op("pe", lambda e, hp=hp, ptt=ptt: e.transpose(out=ptt[0][:, hp * 128:(hp + 1) * 128], in_=rot[b][0][:, hp * 128:(hp + 1) * 128], identity=idb[:]),
                     reads=[rot[b][1], ridb], writes=[ptt[1]])
            P.op("act", lambda e, ptt=ptt: e.copy(out=qT[b][0][:].rearrange("p a b -> p (a b)"), in_=ptt[0][:, 0:512]), reads=[ptt[1]], writes=[qT[b][1]])
            P.dma("pool", qT_o[s], qT[b][0][:], reads=[qT[b][1]])
            if stage < 7:
                return
            tm_group(1792, 512, zt2[b])
            rope((zt2[b][0][:, 0:512], zt2[b][1]), 8, (rot[b][0][:, 0:512], rot[b][1]), eng="dve")
            ptt = nextpt()
            for hp in range(4):
                P.op("pe", lambda e, hp=hp, ptt=ptt: e.transpose(out=ptt[0][:, hp * 128:(hp + 1) * 128], in_=rot[b][0][:, hp * 128:(hp + 1) * 128], identity=idb[:]),
                     reads=[rot[b][1], ridb], writes=[ptt[1]])
            P.op("act", lambda e, ptt=ptt: e.copy(out=kT[b][0][:].rearrange("p a b -> p (a b)"), in_=ptt[0][:, 0:512]), reads=[ptt[1]], writes=[kT[b][1]])
            P.dma("pool", kT_o[s], kT[b][0][:], reads=[kT[b][1]])
            if stage < 8:
                return
            pz = nextpb()
            for kc in range(8):
                P.op("pe", lambda e, kc=kc, pz=pz: e.matmul(pz[0][:, 0:512], lhsT=nT[b][0][:, kc, HALO:NT], rhs=Wb[:, kc, 2304:2816],
                                                            start=(kc == 0), stop=(kc == 7)), reads=[rWb, nT[b][1]], writes=[pz[1]])
            P.op("dve", lambda e, pz=pz: e.tensor_tensor(out=va[b][0][:, :, 0:64], in0=pz[0][:, 0:512].rearrange("p (h d) -> p h d", h=8),
                                                         in1=bbc[:, 2304:2816].rearrange("p (h d) -> p h d", h=8), op=ALU.add),
                 reads=[pz[1], rbbc], writes=[va[b][1]])
            P.dma("pool", va_o[s], va[b][0][:].rearrange("p h d -> p (h d)"), reads=[va[b][1]])
            if stage < 9:
                return
            tm_group(2816, 512, ag1[b])
            P.op("act", lambda e: e.activation(out=ag[b][0][:], in_=ag1[b][0][:], func=AF.Silu), reads=[ag1[b][1]], writes=[ag[b][1]])
            P.dma("pool", ag_o[s], ag[b][0][:], reads=[ag[b][1]])
            if stage < 10:
                return
            tm_group(3328, 324, zi[b])
            P.op("act", lambda e: e.activation(out=wia[b][0][:, 0:4], in_=zi[b][0][:, 320:324], func=AF.Abs, scale=0.125),
                 reads=[zi[b][1]], writes=[wia[b][1]])
            P.op("act", lambda e: e.activation(out=wia[b][0][:, 4:8], in_=zi[b][0][:, 320:324], func=AF.Sign), reads=[zi[b][1]], writes=[wia[b][1]])
            P.dma("pool", sg_o[s], wia[b][0][:, 4:8], reads=[wia[b][1]])
            if stage < 11:
                return
            for hh in range(4):
                P.op("dve", lambda e, hh=hh: e.tensor_scalar(out=zi[b][0][:, hh * 64:(hh + 1) * 64], in0=zi[b][0][:, hh * 64:(hh + 1) * 64],
                                                             scalar1=wia[b][0][:, hh:hh + 1], scalar2=None, op0=ALU.mult),
                     reads=[zi[b][1], wia[b][1]], writes=[zi[b][1]])
            rope((zi[b][0][:, 0:256], zi[b][1]), 4, (qik[b][0][:, 0:256], qik[b][1]), eng="pool")
            rope((zi[b][0][:, 256:320], zi[b][1]), 1, (qik[b][0][:, 256:320], qik[b][1]), eng="dve")
            ptt = nextpt()
            for hp in range(3):
                P.op("pe", lambda e, hp=hp, ptt=ptt: e.transpose(out=ptt[0][:, hp * 128:(hp + 1) * 128], in_=qik[b][0][:, hp * 128:(hp + 1) * 128], identity=idb[:]),
                     reads=[qik[b][1], ridb], writes=[ptt[1]])
            P.op("act", lambda e, ptt=ptt: e.copy(out=qikT[b][0][:].rearrange("p a b -> p (a b)"), in_=ptt[0][:, 0:384]), reads=[ptt[1]], writes=[qikT[b][1]])
            P.dma("pool", qi_o[s], qikT[b][0][:, 0:2, :], reads=[qikT[b][1]])
            P.dma("pool", ki_o[s], qikT[b][0][0:64, 2, :], reads=[qikT[b][1]])

        for s_ in range(NS if stage >= 1 else 0):
            do_slot(s_)

        fw_ = P.all_dma_tokens()
        P.emit(final_waits={"pool": fw_, "sp": fw_})
    return nc


S = 16384
NIT = 12
NEG = -1.0e30


def bc_mid(ap2d, n):
    a = ap2d.ap
    return AP(ap2d.tensor, ap2d.offset, [list(a[0]), [0, n], list(a[1])])


def build_B(NS, SK=S):
    nc = bass.Bass("TRN2", target_bir_lowering=False)
    dr = lambda n, s, d, k="ExternalInput": nc.dram_tensor(n, list(s), d, kind=k).ap()
    qT_d = dr("qT", [NS, 128, 4, 128], BF16)
    qi_d = dr("qiT", [NS, 128, 2, 128], BF16)
    sg_d = dr("sg", [NS, 128, 4], F32)
    ag_d = dr("ag", [NS, 128, 512], BF16)
    yab_d = dr("yab", [NS, 128, 4, 128], BF16)
    hA = dr("hA", [NS * 128, D], F32)
    pA = dr("pA", [NS * 128, 256], F32)
    KT = dr("KT", [128, 4, SK], BF16)
    VA = dr("VA", [SK // 128, 128, 528], BF16)
    KI = dr("KI", [128, SK], BF16)
    qoff_d = dr("qoff", [128, 2], F32)
    iota_d = dr("iota", [128, 512], F32)
    fl_l = dr("fl_l", [3, 32, 128], BF16)
    fl_r = dr("fl_r", [3, 512], BF16)
    w_out = dr("w_out", [D, D], F32)
    w_g = dr("w_g", [D, D], F32)
    w_p = dr("w_p", [256, D], F32)
    gF = dr("gF", [1, D], F32)
    ident_d = dr("ident", [128, 128], F32)
    h_o = dr("h_o", [NS * 128, D], F32, "ExternalOutput")
    fsel_d = dr("fsel", [128, 1], F32)

    with ExitStack() as st:
        P = Prog(nc, st)
        sb, ps = P.sb, P.ps
        WO = sb("WO", [128, 8, D], BF16); rWO = Res()
        WG = sb("WG", [128, 8, D], BF16); rWG = Res()
        WP = sb("WP", [128, 2, D], BF16); rWP = Res()
        gFb = sb("gFb", [128, D], F32); rgF = Res()
        idf = sb("idf", [128, 128], F32); ridf = Res()
        idb = sb("idb", [128, 128], BF16); ridb = Res()
        qoff = sb("qoff_t", [128, 2], F32); rqoff = Res()
        iota = sb("iota_t", [128, 512], F32); riota = Res()
        fll = sb("fll", [3, 32, 128], BF16); rfll = Res()
        flr = sb("flr", [3, 512], BF16); rflr = Res()
        score = sb("score", [128, S], F32)
        rsc = [Res() for _ in range(32)]
        stg = [score[:, i * 1024:(i + 1) * 1024] for i in range(2)]; rstg = [rsc[0], rsc[2]]
        fsel = sb("fsel_t", [128, 1], F32); rfsel = Res()
        P.dma("sp", fsel[:], fsel_d, writes=[rfsel])
        P.dma("sp", gFb[:], gF.partition_broadcast(128), writes=[rgF])
        P.dma("sp", idf[:], ident_d, writes=[ridf])
        P.dma("sp", qoff[:], qoff_d, writes=[rqoff])
        P.dma("sp", iota[:], iota_d, writes=[riota])
        P.dma("sp", fll[:], fl_l, writes=[rfll])
        P.dma("sp", flr[:], fl_r, writes=[rflr])
        P.op("dve", lambda e: e.tensor_copy(out=idb[:], in_=idf[:]), reads=[ridf], writes=[ridb])
        ci = 0
        for (Wd, Wt, rW, nk) in ((w_out, WO, rWO, 8), (w_g, WG, rWG, 8), (w_p, WP, rWP, 2)):
            for kc in range(nk):
                i = ci % 2
                P.dma("sp", stg[i], Wd[kc * 128:(kc + 1) * 128, :], writes=[rstg[i]])
                eng = ("dve", "pool")[ci % 2]
                P.op(eng, lambda e, i=i, kc=kc, Wt=Wt: e.tensor_copy(out=Wt[:, kc, :], in_=stg[i]), reads=[rstg[i]], writes=[rW])
                ci += 1

        def dbl(name, shape, dt, n=2):
            return [(sb("%s%d" % (name, i), shape, dt), Res()) for i in range(n)]

        class Rot:
            def __init__(self, tiles):
                self.t = tiles; self.i = 0

            def next(self):
                t = self.t[self.i % len(self.t)]; self.i += 1; return t

        qTt = dbl("qTt", [128, 4, 128], BF16)
        qit = dbl("qit", [128, 2, 128], BF16)
        sgt = dbl("sgt", [128, 4], F32)
        agt = dbl("agt", [128, 512], BF16)
        yabt = dbl("yabt", [128, 4, 128], BF16)
        hbl = dbl("hbl", [128, D], F32, 1) * 2
        pbl = dbl("pbl", [128, 256], F32)
        kit = Rot(dbl("kit", [128, 512], BF16, 3))
        ktt = Rot(dbl("ktt", [128, 4, 512], BF16, 2))
        vat = Rot(dbl("vat", [128, 4, 528], BF16, 2))
        Rb = Rot(dbl("Rb", [128, 512], F32, 3))
        Eb = Rot(dbl("Eb", [128, 512], BF16, 3))
        Pb = Rot(dbl("Pb", [128, 512], BF16, 3))
        junk = sb("junk", [128, 1024], BF16); rjunk = Res()
        sm = sb("sm", [128, 512], F32); rsm = Res()
        sm2 = sb("sm2", [128, 512], F32); rsm2 = Res()
        cbt = sm2; rcb = rsm2
        tv = sb("tv", [128, 16], F32)
        rtv = Res()
        cnt8 = sb("cnt8", [128, 16], F32); rcnt8 = Res()
        mk = Rot(dbl("mk", [128, 512], BF16, 2))
        mT = Rot(dbl("mT", [128, 512], BF16, 2))
        oacc = sb("oacc", [128, 8, 66], F32); roacc = Res()
        rec = sb("rec", [128, 8], F32); rrec = Res()
        ycf = score[:, 4096:4608]; rycf = [rsc[8]]
        ycb = sb("ycb", [128, 512], BF16); rycb = Res()
        ycT = sb("ycT", [128, 4, 128], BF16); rycT = Res()
        h1 = score[:, 0:1024]; rh1 = [rsc[0], rsc[1]]
        h1b = sb("h1b", [128, D], BF16); rh1b = Res()
        h1T = sb("h1T", [128, 8, 128], BF16); rh1T = Res()
        gsb = score[:, 1024:2048]; rgsb = [rsc[2], rsc[3]]
        pbb = sb("pbb", [128, 256], BF16); rpbb = Res()
        pT = sb("pT", [128, 2, 128], BF16); rpT = Res()
        h2 = [(score[:, 2048:3072], [rsc[4], rsc[5]])] * 2
        sqt = h1; rsq = rh1
        st1 = sb("st1", [128, 4], F32); rst1 = Res()
        hnt = [(score[:, 3072:4096], [rsc[6], rsc[7]])] * 2

        pa = Rot([(ps("pa%d" % i, [128, 512], F32), Res()) for i in range(4)])
        pf = (ps("pf", [128, 512], F32), Res())
        pm = (ps("pm", [128, 1024], BF16), Res())
        po = [(ps("po%d" % i, [128, 512], F32), Res()) for i in range(2)]

        def do_slot(s):
            b = s % 2
            T = s + 1
            L = 512 * T
            o = s % 2
            qT_, qi_, sg_, ag_, yab_, h_, p_ = qTt[b], qit[b], sgt[b], agt[b], yabt[b], hbl[b], pbl[b]
            P.dma("sp", qi_[0][:], qi_d[s], writes=[qi_[1]])
            P.dma("sp", sg_[0][:], sg_d[s], writes=[sg_[1]])
            P.dma("sp", qT_[0][:], qT_d[s], writes=[qT_[1]])
            P.dma("sp", ag_[0][:], ag_d[s], writes=[ag_[1]])
            P.dma("sp", yab_[0][:], yab_d[s], writes=[yab_[1]])
            P.dma("sp", h_[0][:], hA[s * 128:(s + 1) * 128, :], writes=[h_[1]])
            P.dma("sp", p_[0][:], pA[s * 128:(s + 1) * 128, :], writes=[p_[1]])
            for t in range(T):
                kt = kit.next()
                P.dma("sp", kt[0][:], KI[:, t * 512:(t + 1) * 512], writes=[kt[1]])
                P.op("pe", lambda e, t=t: e.matmul(pf[0][:, :], lhsT=fll[:, t, :], rhs=flr[:, :], start=True, stop=True),
                     reads=[rfll, rflr], writes=[pf[1]])
                sc = score[:, t * 512:(t + 1) * 512]
                for hh in range(4):
                    pz = pa.next()
                    p0 = (hh % 2) * 64
                    P.op("pe", lambda e, pz=pz, p0=p0, hh=hh, kt=kt: e.matmul(pz[0][:, :], lhsT=qi_[0][p0:p0 + 64, hh // 2, :], rhs=kt[0][p0:p0 + 64, :],
                                                                             start=True, stop=True),
                         reads=[qi_[1], kt[1]], writes=[pz[1]])
                    rb = Rb.next()
                    P.op("act", lambda e, pz=pz, rb=rb: e.activation(out=rb[0][:], in_=pz[0][:, :], func=AF.Relu), reads=[pz[1]], writes=[rb[1]])
                    if hh == 0:
                        P.op("dve", lambda e, rb=rb, sc=sc: e.scalar_tensor_tensor(out=sc, in0=rb[0][:], scalar=sg_[0][:, 0:1], in1=pf[0][:, :],
                                                                                   op0=ALU.mult, op1=ALU.add),
                             reads=[rb[1], sg_[1], pf[1]], writes=[rsc[t]])
                    else:
                        P.op("dve", lambda e, rb=rb, sc=sc, hh=hh: e.scalar_tensor_tensor(out=sc, in0=rb[0][:], scalar=sg_[0][:, hh:hh + 1], in1=sc,
                                                                                          op0=ALU.mult, op1=ALU.add),
                             reads=[rb[1], sg_[1], rsc[t]], writes=[rsc[t]])
                if t == T - 1:
                    P.op("dve", lambda e: e.tensor_scalar(out=cbt[:], in0=iota[:], scalar1=qoff[:, o:o + 1], scalar2=NEG, op0=ALU.is_gt, op1=ALU.mult),
                         reads=[riota, rqoff], writes=[rcb])
                    P.op("dve", lambda e, sc=sc: e.tensor_tensor(out=sc, in0=sc, in1=cbt[:], op=ALU.add), reads=[rsc[t], rcb], writes=[rsc[t]])
            rrow = rsc[0:T]
            if T == 1:
                P.op("dve", lambda e: e.tensor_copy(out=sm[:], in_=score[:, 0:512]), reads=rrow, writes=[rsm])
            else:
                P.op("dve", lambda e: e.tensor_reduce(out=sm[:], in_=score[:, 0:L].rearrange("p (g k) -> p g k", k=T), axis=AX.X, op=ALU.max),
                     reads=rrow, writes=[rsm])
            P.op("dve", lambda e: e.tensor_scalar(out=sm2[:], in0=sm[:], scalar1=-1.0e29, scalar2=2.0e30, op0=ALU.is_lt, op1=ALU.mult),
                 reads=[rsm], writes=[rsm2])
            P.op("dve", lambda e: e.tensor_tensor(out=sm2[:], in0=sm2[:], in1=sm[:], op=ALU.add), reads=[rsm, rsm2], writes=[rsm2])
            P.op("dve", lambda e: e.tensor_reduce(out=tv[:, 0:1], in_=sm2[:], axis=AX.X, op=ALU.min), reads=[rsm2], writes=[rtv])
            P.op("dve", lambda e: e.tensor_reduce(out=tv[:, 1:2], in_=sm[:], axis=AX.X, op=ALU.max), reads=[rsm], writes=[rtv])
            P.op("dve", lambda e: e.tensor_tensor(out=tv[:, 2:3], in0=tv[:, 1:2], in1=tv[:, 0:1], op=ALU.subtract), reads=[rtv], writes=[rtv])
            P.op("dve", lambda e: e.tensor_copy(out=tv[:, 3:4], in_=tv[:, 0:1]), reads=[rtv], writes=[rtv])
            CH = 1024
            nch = (L + CH - 1) // CH
            for k in range(1, NIT + 1):
                P.op("dve", lambda e, k=k: e.tensor_scalar(out=tv[:, 4:5], in0=tv[:, 2:3], scalar1=float(2.0 ** -k), scalar2=None, op0=ALU.mult),
                     reads=[rtv], writes=[rtv])
                P.op("dve", lambda e: e.tensor_tensor(out=tv[:, 5:6], in0=tv[:, 3:4], in1=tv[:, 4:5], op=ALU.add), reads=[rtv], writes=[rtv])
                for c in range(nch):
                    c0 = c * CH
                    w = min(CH, L - c0)
                    P.op("dve", lambda e, c=c, c0=c0, w=w: e.tensor_scalar(out=junk[:, 0:w], in0=score[:, c0:c0 + w], scalar1=tv[:, 5:6], scalar2=0.0,
                                                                           op0=ALU.is_ge, op1=ALU.add, accum_out=cnt8[:, c:c + 1]),
                         reads=rrow + [rtv], writes=[rjunk, rcnt8])
                if nch > 1:
                    P.op("dve", lambda e: e.tensor_reduce(out=tv[:, 6:7], in_=cnt8[:, 0:nch], axis=AX.X, op=ALU.add), reads=[rcnt8], writes=[rtv])
                else:
                    P.op("dve", lambda e: e.tensor_copy(out=tv[:, 6:7], in_=cnt8[:, 0:1]), reads=[rcnt8], writes=[rtv])
                P.op("dve", lambda e: e.scalar_tensor_tensor(out=tv[:, 7:8], in0=tv[:, 6:7], scalar=255.5, in1=tv[:, 4:5], op0=ALU.is_ge, op1=ALU.mult),
                     reads=[rtv], writes=[rtv])
                P.op("dve", lambda e: e.tensor_tensor(out=tv[:, 3:4], in0=tv[:, 3:4], in1=tv[:, 7:8], op=ALU.add), reads=[rtv], writes=[rtv])
            for t in range(T):
                ktile = ktt.next()
                vtile = vat.next()
                P.dma("sp", ktile[0][:], KT[:, :, t * 512:(t + 1) * 512], writes=[ktile[1]])
                P.dma("sp", vtile[0][:], VA[4 * t:4 * t + 4].rearrange("c p f -> p c f"), writes=[vtile[1]])
                m_ = mk.next()
                P.op("dve", lambda e, m_=m_, t=t: e.tensor_scalar(out=m_[0][:], in0=score[:, t * 512:(t + 1) * 512], scalar1=tv[:, 3:4], scalar2=None,
                                                                  op0=ALU.is_ge), reads=[rsc[t], rtv], writes=[m_[1]])
                for c in range(4):
                    P.op("pe", lambda e, c=c, m_=m_: e.transpose(out=pm[0][:, c * 128:(c + 1) * 128], in_=m_[0][:, c * 128:(c + 1) * 128], identity=idb[:]),
                         reads=[m_[1], ridb], writes=[pm[1]])
                mt = mT.next()
                P.op("act", lambda e, mt=mt: e.copy(out=mt[0][:], in_=pm[0][:, 0:512]), reads=[pm[1]], writes=[mt[1]])
                for hh in range(8):
                    p0 = (hh % 2) * 64
                    pz = pa.next()
                    for c in range(4):
                        P.op("pe", lambda e, c=c, pz=pz, p0=p0, hh=hh, ktile=ktile: e.matmul(
                            pz[0][:, c * 128:(c + 1) * 128], lhsT=ktile[0][p0:p0 + 64, hh // 2, c * 128:(c + 1) * 128], rhs=qT_[0][p0:p0 + 64, hh // 2, :],
                            start=True, stop=True), reads=[ktile[1], qT_[1]], writes=[pz[1]])
                    eb = Eb.next()
                    P.op("act", lambda e, pz=pz, eb=eb: e.activation(out=eb[0][:], in_=pz[0][:, :], func=AF.Exp, scale=0.125), reads=[pz[1]], writes=[eb[1]])
                    pb_ = Pb.next()
                    eng = "dve" if hh % 2 == 0 else "pool"
                    P.op(eng, lambda e, eb=eb, pb_=pb_, mt=mt: e.tensor_tensor(out=pb_[0][:], in0=eb[0][:], in1=mt[0][:], op=ALU.mult),
                         reads=[eb[1], mt[1]], writes=[pb_[1]])
                    pob = po[hh // 4]
                    for c in range(4):
                        P.op("pe", lambda e, c=c, pb_=pb_, vtile=vtile, hh=hh, pob=pob: e.matmul(
                            pob[0][:, (hh % 4) * 66:(hh % 4 + 1) * 66], lhsT=pb_[0][:, c * 128:(c + 1) * 128], rhs=vtile[0][:, c, hh * 66:(hh + 1) * 66],
                            start=(c == 0), stop=(c == 3)), reads=[pb_[1], vtile[1]], writes=[pob[1]])
                for g in range(2):
                    ov = oacc[:, 4 * g:4 * g + 4, :].rearrange("p h d -> p (h d)")
                    if t == 0:
                        P.op("act", lambda e, g=g, ov=ov: e.copy(out=ov, in_=po[g][0][:, 0:264]), reads=[po[g][1]], writes=[roacc])
                    else:
                        P.op("dve", lambda e, g=g, ov=ov: e.tensor_tensor(out=ov, in0=ov, in1=po[g][0][:, 0:264], op=ALU.add),
                             reads=[po[g][1], roacc], writes=[roacc])
            P.op("dve", lambda e: e.reciprocal(out=rec[:], in_=oacc[:, :, 64]), reads=[roacc], writes=[rrec])
            for hh in range(8):
                P.op("dve", lambda e, hh=hh: e.tensor_scalar(out=ycf[:, hh * 64:(hh + 1) * 64], in0=oacc[:, hh, 0:64], scalar1=rec[:, hh:hh + 1], scalar2=None,
                                                             op0=ALU.mult), reads=[roacc, rrec], writes=[rycf])
            P.op("dve", lambda e: e.tensor_tensor(out=ycb[:], in0=ycf[:], in1=ag_[0][:], op=ALU.mult), reads=[rycf, ag_[1]], writes=[rycb])
            for c in range(4):
                P.op("pe", lambda e, c=c: e.transpose(out=pm[0][:, c * 128:(c + 1) * 128], in_=ycb[:, c * 128:(c + 1) * 128], identity=idb[:]),
                     reads=[rycb, ridb], writes=[pm[1]])
            P.op("act", lambda e: e.copy(out=ycT[:].rearrange("p a b -> p (a b)"), in_=pm[0][:, 0:512]), reads=[pm[1]], writes=[rycT])
            for n in range(2):
                pz = pa.next()
                for kc in range(8):
                    lt = yab_[0][:, kc, :] if kc < 4 else ycT[:, kc - 4, :]
                    P.op("pe", lambda e, kc=kc, pz=pz, lt=lt, n=n: e.matmul(pz[0][:, :], lhsT=lt, rhs=WO[:, kc, n * 512:(n + 1) * 512],
                                                                           start=(kc == 0), stop=(kc == 7)),
                         reads=[yab_[1], rycT, rWO], writes=[pz[1]])
                P.op("dve", lambda e, pz=pz, n=n: e.tensor_tensor(out=h1[:, n * 512:(n + 1) * 512], in0=pz[0][:, :], in1=h_[0][:, n * 512:(n + 1) * 512], op=ALU.add),
                     reads=[pz[1], h_[1]], writes=[rh1])
            P.op("act", lambda e: e.copy(out=h1b[:], in_=h1[:]), reads=[rh1], writes=[rh1b])
            for c in range(8):
                P.op("pe", lambda e, c=c: e.transpose(out=pm[0][:, c * 128:(c + 1) * 128], in_=h1b[:, c * 128:(c + 1) * 128], identity=idb[:]),
                     reads=[rh1b, ridb], writes=[pm[1]])
            P.op("act", lambda e: e.copy(out=h1T[:].rearrange("p a b -> p (a b)"), in_=pm[0][:, :]), reads=[pm[1]], writes=[rh1T])
            for n in range(2):
                pz = pa.next()
                for kc in range(8):
                    P.op("pe", lambda e, kc=kc, pz=pz, n=n: e.matmul(pz[0][:, :], lhsT=h1T[:, kc, :], rhs=WG[:, kc, n * 512:(n + 1) * 512],
                                                                    start=(kc == 0), stop=(kc == 7)), reads=[rh1T, rWG], writes=[pz[1]])
                P.op("act", lambda e, pz=pz, n=n: e.activation(out=gsb[:, n * 512:(n + 1) * 512], in_=pz[0][:, :], func=AF.Sigmoid), reads=[pz[1]], writes=[rgsb])
            P.op("pool", lambda e: e.tensor_copy(out=pbb[:], in_=p_[0][:]), reads=[p_[1]], writes=[rpbb])
            for c in range(2):
                P.op("pe", lambda e, c=c: e.transpose(out=pm[0][:, c * 128:(c + 1) * 128], in_=pbb[:, c * 128:(c + 1) * 128], identity=idb[:]),
                     reads=[rpbb, ridb], writes=[pm[1]])
            P.op("act", lambda e: e.copy(out=pT[:].rearrange("p a b -> p (a b)"), in_=pm[0][:, 0:256]), reads=[pm[1]], writes=[rpT])
            h2_ = h2[b]
            for n in range(2):
                pz = pa.next()
                for kc in range(2):
                    P.op("pe", lambda e, kc=kc, pz=pz, n=n: e.matmul(pz[0][:, :], lhsT=pT[:, kc, :], rhs=WP[:, kc, n * 512:(n + 1) * 512],
                                                                    start=(kc == 0), stop=(kc == 1)), reads=[rpT, rWP], writes=[pz[1]])
                P.op("dve", lambda e, pz=pz, n=n: e.tensor_tensor(out=gsb[:, n * 512:(n + 1) * 512], in0=pz[0][:, :], in1=gsb[:, n * 512:(n + 1) * 512], op=ALU.mult),
                     reads=[pz[1], rgsb], writes=[rgsb])
            P.op("pool", lambda e: e.tensor_tensor(out=h2_[0][:], in0=h1[:], in1=gsb[:], op=ALU.add), reads=[rh1, rgsb], writes=[h2_[1]])
            hn_ = hnt[b]
            P.op("act", lambda e: e.activation(out=sqt[:], in_=h2_[0][:], func=AF.Square), reads=[h2_[1]], writes=[rsq])
            P.op("dve", lambda e: e.tensor_reduce(out=st1[:, 0:1], in_=sqt[:], axis=AX.X, op=ALU.add), reads=[rsq], writes=[rst1])
            P.op("dve", lambda e: e.tensor_scalar(out=st1[:, 1:2], in0=st1[:, 0:1], scalar1=1.0 / D, scalar2=EPS, op0=ALU.mult, op1=ALU.add),
                 reads=[rst1], writes=[rst1])
            P.op("act", lambda e: e.activation(out=st1[:, 2:3], in_=st1[:, 1:2], func=AF.Sqrt), reads=[rst1], writes=[rst1])
            P.op("dve", lambda e: e.reciprocal(out=st1[:, 3:4], in_=st1[:, 2:3]), reads=[rst1], writes=[rst1])
            P.op("dve", lambda e: e.scalar_tensor_tensor(out=hn_[0][:], in0=h2_[0][:], scalar=st1[:, 3:4], in1=gFb[:], op0=ALU.mult, op1=ALU.mult),
                 reads=[h2_[1], rst1, rgF], writes=[hn_[1]])
            P.op("pool", lambda e: e.tensor_tensor(out=hn_[0][:], in0=hn_[0][:], in1=h2_[0][:], op=ALU.subtract), reads=[hn_[1], h2_[1]], writes=[hn_[1]])
            P.op("dve", lambda e: e.scalar_tensor_tensor(out=hn_[0][:], in0=hn_[0][:], scalar=fsel[:, 0:1], in1=h2_[0][:], op0=ALU.mult, op1=ALU.add),
                 reads=[hn_[1], h2_[1], rfsel], writes=[hn_[1]])
            P.dma("pool", h_o[s * 128:(s + 1) * 128, :], hn_[0][:], reads=[hn_[1]])

        for s_ in range(NS):
            do_slot(s_)
        fw_ = P.all_dma_tokens()
        P.emit(final_waits={"pool": fw_, "sp": fw_})
    return nc


BF = ml_dtypes.bfloat16
S = 16384
HALO = 32
NQB = S // 128


def slot_qb(core, s):
    j = core % 4
    return 8 * (s // 2) + (j if s % 2 == 0 else 7 - j)


def rope_table():
    half = 32
    inv = (np.float32(10000.0) ** (-np.arange(half, dtype=np.float32) / np.float32(half))).astype(np.float32)
    ang = np.arange(S, dtype=np.float32)[:, None] * inv[None, :]
    return np.concatenate([np.cos(ang), np.sin(ang)], axis=1).astype(np.float32)


def prep_A_weights(i, inp):
    w = {}
    w["w_in"] = np.ascontiguousarray(inp["w_in"][i])
    b = inp["b_in"][i]
    w["b_bc"] = np.ascontiguousarray(b[None, :])
    w["b_fm"] = np.ascontiguousarray(b[:1280].reshape(10, 128).T)
    w["g_bc"] = np.ascontiguousarray(inp["norm_g"][i][None, :])
    w["wdw"] = np.ascontiguousarray(inp["conv_dw_w"][i].T.reshape(2, 128, 31).transpose(1, 0, 2))
    cv = np.zeros((128, 7, 2), np.float32)
    for k, name in enumerate(["conv_dw_b", "conv_ln_g", "conv_ln_b", "conv_pw_b", "pool_b", "pool_scale"]):
        cv[:, k, :] = inp[name][i].reshape(2, 128).T
    cv[:64, 6, 0] = 1 / 2; cv[64:, 6, 0] = 1 / 4; cv[:64, 6, 1] = 1 / 8; cv[64:, 6, 1] = 1 / 16
    w["cvec"] = cv
    w["pw_w"] = np.ascontiguousarray(inp["conv_pw_w"][i].reshape(2, 128, 256).transpose(1, 0, 2))
    pl = np.zeros((128, 2, 128), np.float32)
    pw = inp["pool_w"][i]
    for g in range(4):
        cc, p0 = g // 2, (g % 2) * 64
        pl[p0:p0 + 64, cc, p0:p0 + 64] = pw[g]
    w["plw"] = pl
    w["ident"] = np.eye(128, dtype=np.float32)
    return w


def prep_A_core(core, h, NS, cs_tab):
    bt = core // 4
    hA = np.empty((NS * 128, 1024), np.float32)
    hH = np.zeros((NS * HALO, 1024), np.float32)
    hok = np.ones((128, NS), np.float32)
    cs = np.empty((NS * 128, 64), np.float32)
    rc0 = np.empty((128, 2, 128), np.float32)
    wins = [2, 4, 8, 16]
    for s in range(NS):
        qb = slot_qb(core, s)
        t0 = qb * 128
        hA[s * 128:(s + 1) * 128] = h[bt, t0:t0 + 128]
        cs[s * 128:(s + 1) * 128] = cs_tab[t0:t0 + 128]
        if qb == 0:
            hok[:, s] = 0.0
        else:
            hH[s * HALO:(s + 1) * HALO] = h[bt, t0 - HALO:t0]
    qb0 = slot_qb(core, 0)
    t = qb0 * 128 + np.arange(128)
    for g in range(4):
        cc, p0 = g // 2, (g % 2) * 64
        rc0[p0:p0 + 64, cc, :] = (1.0 / np.minimum(t + 1, wins[g]).astype(np.float32))[None, :]
    return {"hA": hA, "hH": hH, "hok": hok, "cs": cs, "rc0": rc0}


def prep_B_consts(core):
    j = core % 4
    pidx = np.arange(128, dtype=np.float32)
    qoff = np.stack([128.0 * j + pidx, 128.0 * (3 - j) + pidx], 1).astype(np.float32)
    iota = np.tile(np.arange(512, dtype=np.float32)[None, :], (128, 1))
    eps = 2.0 ** -30
    fl_l = np.zeros((3, 32, 128), np.float32)
    fl_l[0] = -eps * 16
    fl_l[1] = -eps
    fl_l[2] = (-eps * 512 * np.arange(32, dtype=np.float32))[:, None]
    kk = np.arange(512)
    fl_r = np.stack([kk // 16, kk % 16, np.ones(512)], 0).astype(np.float32)
    return {"qoff": qoff, "iota": iota, "fl_l": fl_l.astype(BF), "fl_r": fl_r.astype(BF), "ident": np.eye(128, dtype=np.float32)}


def prep_B_weights(i, inp):
    return {"w_out": np.ascontiguousarray(inp["w_out"][i]), "w_g": np.ascontiguousarray(inp["ple_gate_w"][i]),
            "w_p": np.ascontiguousarray(inp["ple_w"][i]), "gF": np.ascontiguousarray(inp["final_norm_g"][None, :])}

AP = bass.AP
NSLOT = 32
_CACHE = {}


def _progs():
    if "A" not in _CACHE:
        _CACHE["A"] = build_A(NSLOT)
        _CACHE["B"] = build_B(NSLOT)
    return _CACHE["A"], _CACHE["B"]


def kernel(**inputs):
    inp = {k: np.asarray(v) for k, v in inputs.items()}
    ncA, ncB = _progs()
    h = np.array(inp["x"], dtype=np.float32, copy=True)
    cs_tab = rope_table()
    cores = list(range(8))
    for i in range(4):
        wA = prep_A_weights(i, inp)
        mapsA = []
        for c in cores:
            m = dict(wA)
            m.update(prep_A_core(c, h, NSLOT, cs_tab))
            mapsA.append(m)
        rA = run_bass_kernel_spmd(ncA, mapsA, core_ids=cores).results
        KT = [np.empty((128, 4, S), BF) for _ in range(2)]
        VA = [np.empty((S // 128, 128, 528), BF) for _ in range(2)]
        KI = [np.empty((128, S), BF) for _ in range(2)]
        for c in cores:
            bt = c // 4
            kT_o, va_o, ki_o = np.asarray(rA[c]["kT_o"]), np.asarray(rA[c]["va_o"]), np.asarray(rA[c]["ki_o"])
            for s in range(NSLOT):
                qb = slot_qb(c, s)
                KT[bt][:, :, qb * 128:(qb + 1) * 128] = kT_o[s]
                VA[bt][qb] = va_o[s]
                KI[bt][0:64, qb * 128:(qb + 1) * 128] = ki_o[s]
                KI[bt][64:128, qb * 128:(qb + 1) * 128] = ki_o[s]
        wB = prep_B_weights(i, inp)
        mapsB = []
        for c in cores:
            bt = c // 4
            m = dict(wB)
            m.update(prep_B_consts(c))
            toks = np.concatenate([np.arange(128) + 128 * slot_qb(c, s) for s in range(NSLOT)])
            m["qT"] = np.asarray(rA[c]["qT_o"]); m["qiT"] = np.asarray(rA[c]["qi_o"]); m["sg"] = np.asarray(rA[c]["sg_o"])
            m["ag"] = np.asarray(rA[c]["ag_o"]); m["yab"] = np.asarray(rA[c]["yab_o"])
            m["hA"] = np.ascontiguousarray(h[bt][toks]); m["pA"] = np.ascontiguousarray(inp["p"][i, bt][toks])
            m["KT"] = KT[bt]; m["VA"] = VA[bt]; m["KI"] = KI[bt]
            m["fsel"] = np.full((128, 1), 1.0 if i == 3 else 0.0, np.float32)
            mapsB.append(m)
        rB = run_bass_kernel_spmd(ncB, mapsB, core_ids=cores).results
        for c in cores:
            bt = c // 4
            toks = np.concatenate([np.arange(128) + 128 * slot_qb(c, s) for s in range(NSLOT)])
            h[bt][toks] = np.asarray(rB[c]["h_o"])
    return h.astype(np.float32)
```

```python
from contextlib import ExitStack
import numpy as np
import ml_dtypes
import concourse.bass as bass
import concourse.mybir as mybir
from concourse.bass_utils import run_bass_kernel_spmd


F32 = mybir.dt.float32
BF16 = mybir.dt.bfloat16
ALU = mybir.AluOpType
AF = mybir.ActivationFunctionType
AX = mybir.AxisListType


class Res:
    __slots__ = ("w", "r")

    def __init__(self):
        self.w = None
        self.r = {}


class Prog:
    ENGS = ("pe", "act", "dve", "pool", "sp")
    NDSEM = 24

    def __init__(self, nc, stack):
        self.nc = nc
        self.q = {e: [] for e in self.ENGS}
        self.cnt = {e: 0 for e in self.ENGS}
        self.sems = {}
        for e in ("pe", "act", "dve", "pool"):
            self.sems[e] = stack.enter_context(nc.semaphore("s_" + e))
        self.dsem = {}
        self.dcnt = {}
        self.drr = {}
        for qn in ("sp", "pool"):
            for i in range(self.NDSEM):
                k = "d_%s_%d" % (qn, i)
                self.sems[k] = stack.enter_context(nc.semaphore(k))
                self.dcnt[k] = 0
            self.drr[qn] = 0
        self.stack = stack

    def sb(self, name, shape, dt):
        h = self.stack.enter_context(self.nc.sbuf_tensor(name, list(shape), dt))
        return h

    def ps(self, name, shape, dt):
        h = self.stack.enter_context(self.nc.psum_tensor(name, list(shape), dt))
        return h

    def _deps(self, eng, reads, writes):
        deps = {}

        def add(tok):
            if tok is None:
                return
            k, v = tok
            if eng == "pe" and k == "pe":
                return
            if deps.get(k, 0) < v:
                deps[k] = v

        for r in reads:
            add(r.w)
        for w in writes:
            add(w.w)
            for k, v in w.r.items():
                add((k, v))
        return deps

    def _commit(self, tok, reads, writes):
        k, v = tok
        for r in reads:
            if r.r.get(k, 0) < v:
                r.r[k] = v
        for w in writes:
            w.w = tok
            w.r = {}

    @staticmethod
    def _flat(xs):
        out = []
        for x in xs:
            if isinstance(x, (list, tuple)):
                out.extend(Prog._flat(x))
            else:
                out.append(x)
        return out

    def op(self, eng, fn, reads=(), writes=()):
        reads = self._flat(reads); writes = self._flat(writes)
        deps = self._deps(eng, reads, writes)
        self.cnt[eng] += 1
        tok = (eng, self.cnt[eng])
        self.q[eng].append((deps, fn, tok, 1))
        self._commit(tok, reads, writes)
        return tok

    def dma(self, qn, out, in_, reads=(), writes=(), **kw):
        reads = self._flat(reads); writes = self._flat(writes)
        deps = self._deps(qn, reads, writes)
        i = self.drr[qn]
        self.drr[qn] = (i + 1) % self.NDSEM
        k = "d_%s_%d" % (qn, i)
        if self.dcnt[k] > 0:
            if deps.get(k, 0) < self.dcnt[k]:
                deps[k] = self.dcnt[k]
        self.dcnt[k] += 16
        tok = (k, self.dcnt[k])
        if qn == "pool":
            self.cnt["pool"] += 0
        self.q[qn].append((deps, lambda e: e.dma_start(out=out, in_=in_, **kw), tok, 16))
        self._commit(tok, reads, writes)
        return tok

    def emit(self, final_waits=None):
        nc = self.nc
        engobj = {"pe": "tensor", "act": "scalar", "dve": "vector", "pool": "gpsimd", "sp": "sync"}
        ce = ("pe", "act", "dve", "pool")
        needed = {e: set() for e in ce}
        for en in self.ENGS:
            known = {}
            for deps, fn, tok, inc in self.q[en]:
                for k, v in deps.items():
                    if known.get(k, 0) < v:
                        known[k] = v
                        if k in needed:
                            needed[k].add(v)
            if final_waits and en in final_waits:
                for k, v in final_waits[en].items():
                    if k in needed and known.get(k, 0) < v:
                        needed[k].add(v)
        rank = {e: {v: i + 1 for i, v in enumerate(sorted(needed[e]))} for e in ce}
        with nc.Block() as block:
            for en in self.ENGS:
                q = self.q[en]

                def body(e, q=q, en=en):
                    known = {}

                    def wait(k, v):
                        if known.get(k, 0) < v:
                            known[k] = v
                            e.wait_ge(self.sems[k], rank[k][v] if k in rank else v)

                    for deps, fn, tok, inc in q:
                        for k, v in deps.items():
                            wait(k, v)
                        ins = fn(e)
                        if tok[0] in rank:
                            if tok[1] in rank[tok[0]]:
                                ins.then_inc(self.sems[tok[0]], 1)
                        else:
                            ins.then_inc(self.sems[tok[0]], inc)
                    if final_waits and en in final_waits:
                        for k, v in final_waits[en].items():
                            wait(k, v)

                getattr(block, engobj[en])(body)

    def all_dma_tokens(self):
        return {k: v for k, v in self.dcnt.items() if v > 0}


D = 1024
DIN = 3652
EPS = 1e-6
HALO = 32
NT = 128 + HALO


def bc_mid(ap2d, n):
    a = ap2d.ap
    return AP(ap2d.tensor, ap2d.offset, [list(a[0]), [0, n], list(a[1])])


def bc_last(ap2d, n):
    a = ap2d.ap
    return AP(ap2d.tensor, ap2d.offset, [list(a[0]), list(a[1]), [0, n]])


def build_A(NS, stage=99):
    nc = bass.Bass("TRN2", target_bir_lowering=False)
    dr = lambda n, s, d, k="ExternalInput": nc.dram_tensor(n, list(s), d, kind=k).ap()
    hA = dr("hA", [NS * 128, D], F32)
    hH = dr("hH", [NS * HALO, D], F32)
    hok = dr("hok", [128, NS], F32)
    cs = dr("cs", [NS * 128, 64], F32)
    w_in = dr("w_in", [D, DIN], F32)
    b_bc = dr("b_bc", [1, DIN], F32)
    b_fm = dr("b_fm", [128, 10], F32)
    g_bc = dr("g_bc", [1, D], F32)
    wdw = dr("wdw", [128, 2, 31], F32)
    cvec = dr("cvec", [128, 7, 2], F32)
    pw_w = dr("pw_w", [128, 2, 256], F32)
    plw = dr("plw", [128, 2, 128], F32)
    rc0 = dr("rc0", [128, 2, 128], F32)
    ident_d = dr("ident", [128, 128], F32)
    O = "ExternalOutput"
    kT_o = dr("kT_o", [NS, 128, 4, 128], BF16, O)
    va_o = dr("va_o", [NS, 128, 528], BF16, O)
    ki_o = dr("ki_o", [NS, 64, 128], BF16, O)
    qT_o = dr("qT_o", [NS, 128, 4, 128], BF16, O)
    qi_o = dr("qi_o", [NS, 128, 2, 128], BF16, O)
    sg_o = dr("sg_o", [NS, 128, 4], F32, O)
    ag_o = dr("ag_o", [NS, 128, 512], BF16, O)
    yab_o = dr("yab_o", [NS, 128, 4, 128], BF16, O)

    with ExitStack() as st:
        P = Prog(nc, st)
        sb, ps = P.sb, P.ps
        Wb = sb("Wb", [128, 8, DIN], BF16); rWb = Res()
        bbc = sb("bbc", [128, DIN], F32); rbbc = Res()
        bfm = sb("bfm", [128, 10], F32); rbfm = Res()
        gbc = sb("gbc", [128, D], F32); rgbc = Res()
        wdw_t = sb("wdw_t", [128, 2, 31], F32); rwdw = Res()
        cv = sb("cv", [128, 7, 2], F32); rcv = Res()
        pwb = sb("pwb", [128, 2, 256], BF16); rpwb = Res()
        plb = sb("plb", [128, 2, 128], BF16); rplb = Res()
        rc0_t = sb("rc0_t", [128, 2, 128], F32); rrc0 = Res()
        hok_t = sb("hok_t", [128, NS], F32); rhok = Res()
        idf = sb("idf", [128, 128], F32); ridf = Res()
        idb = sb("idb", [128, 128], BF16); ridb = Res()
        onesf = sb("onesf", [128, 128], F32); rones = Res()
        stg = [sb("stg%d" % i, [128, 1024], F32) for i in range(2)]; rstg = [Res(), Res()]

        P.dma("sp", bbc[:], b_bc.partition_broadcast(128), writes=[rbbc])
        P.dma("sp", gbc[:], g_bc.partition_broadcast(128), writes=[rgbc])
        P.dma("sp", bfm[:], b_fm, writes=[rbfm])
        P.dma("sp", wdw_t[:], wdw, writes=[rwdw])
        P.dma("sp", cv[:], cvec, writes=[rcv])
        P.dma("sp", rc0_t[:], rc0, writes=[rrc0])
        P.dma("sp", hok_t[:], hok, writes=[rhok])
        P.dma("sp", idf[:], ident_d, writes=[ridf])
        P.op("dve", lambda e: e.tensor_copy(out=idb[:], in_=idf[:]), reads=[ridf], writes=[ridb])
        P.op("pool", lambda e: e.memset(onesf[:], 1.0 / 256.0), writes=[rones])
        i = 0
        P.dma("sp", stg[i][:, 0:512], pw_w.rearrange("p a b -> p (a b)"), writes=[rstg[i]])
        P.op("dve", lambda e: e.tensor_copy(out=pwb[:].rearrange("p a b -> p (a b)"), in_=stg[0][:, 0:512]),
             reads=[rstg[0]], writes=[rpwb])
        P.dma("sp", stg[1][:, 0:256], plw.rearrange("p a b -> p (a b)"), writes=[rstg[1]])
        P.op("dve", lambda e: e.tensor_copy(out=plb[:].rearrange("p a b -> p (a b)"), in_=stg[1][:, 0:256]),
             reads=[rstg[1]], writes=[rplb])
        ci = 0
        for kc in range(8):
            for c0 in range(0, DIN, 1024):
                cw = min(1024, DIN - c0)
                i = ci % 2
                P.dma("sp", stg[i][:, 0:cw], w_in[kc * 128:(kc + 1) * 128, c0:c0 + cw], writes=[rstg[i]])
                eng = ("dve", "act", "pool")[ci % 3]
                if eng == "act":
                    P.op("act", lambda e, i=i, kc=kc, c0=c0, cw=cw: e.copy(out=Wb[:, kc, c0:c0 + cw], in_=stg[i][:, 0:cw]),
                         reads=[rstg[i]], writes=[rWb])
                else:
                    P.op(eng, lambda e, i=i, kc=kc, c0=c0, cw=cw: e.tensor_copy(out=Wb[:, kc, c0:c0 + cw], in_=stg[i][:, 0:cw]),
                         reads=[rstg[i]], writes=[rWb])
                ci += 1

        def dbl(name, shape, dt):
            return [(sb("%s%d" % (name, i), shape, dt), Res()) for i in range(2)]

        hblk = dbl("hblk", [128, D], F32)
        hhal = dbl("hhal", [HALO, D], F32)
        cst = dbl("cst", [128, 64], F32)
        sq = dbl("sq", [128, D], F32)
        sqh = dbl("sqh", [HALO, D], F32)
        st1 = dbl("st1", [128, 4], F32)
        st1h = dbl("st1h", [HALO, 4], F32)
        hn = dbl("hn", [128, D], BF16)
        hnh = dbl("hnh", [HALO, D], BF16)
        nT = dbl("nT", [128, 8, NT], BF16)
        val = dbl("val", [128, 2, NT], F32)
        sgl = dbl("sgl", [128, 2, NT], F32)
        u = dbl("u", [128, 2, NT], F32)
        pin = dbl("pin", [128, 2, NT], F32)
        cgate = dbl("cgate", [128, 2, 128], F32)
        pgate = dbl("pgate", [128, 2, 128], F32)
        cacc = dbl("cacc", [128, 2, 128], F32)
        xc = dbl("xc", [128, 2, 128], F32)
        xsq = dbl("xsq", [128, 2, 128], F32)
        rstd_b = dbl("rstd_b", [128, 128], F32)
        yn = dbl("yn", [128, 2, 128], F32)
        sact = dbl("sact", [128, 2, 128], BF16)
        yab = dbl("yab", [128, 4, 128], BF16)
        ps2 = dbl("ps2", [128, 2, NT], F32)
        ps4 = dbl("ps4", [128, 2, NT], F32)
        ps8 = dbl("ps8", [128, NT], F32)
        ps16 = dbl("ps16", [128, NT], F32)
        dpl = dbl("dpl", [128, 2, 128], BF16)
        ptmp = dbl("ptmp", [128, 2, 128], F32)
        ztm = dbl("ztm", [128, 512], F32)
        zt2 = dbl("zt2", [128, 512], F32)
        rt1 = dbl("rt1", [128, 256], F32)
        rt2 = dbl("rt2", [128, 256], F32)
        rot = dbl("rot", [128, 512], BF16)
        qT = dbl("qT", [128, 4, 128], BF16)
        kT = dbl("kT", [128, 4, 128], BF16)
        va = dbl("va", [128, 8, 66], BF16)
        ag1 = dbl("ag1", [128, 512], F32)
        ag = dbl("ag", [128, 512], BF16)
        zi = dbl("zi", [128, 324], F32)
        wia = dbl("wia", [128, 8], F32)
        qik = dbl("qik", [128, 384], BF16)
        qikT = dbl("qikT", [128, 3, 128], BF16)
        for i in range(2):
            P.op("pool", lambda e, i=i: e.memset(qik[i][0][:], 0.0), writes=[qik[i][1]])
        for i in range(2):
            P.op("pool", lambda e, i=i: e.memset(va[i][0][:], 1.0), writes=[va[i][1]])

        pb = [(ps("pb%d" % i, [128, 512], F32), Res()) for i in range(6)]
        pt = [(ps("pt%d" % i, [128, 1024], BF16), Res()) for i in range(2)]
        pbi = [0]
        pti = [0]

        def nextpb():
            t = pb[pbi[0] % len(pb)]; pbi[0] += 1; return t

        def nextpt():
            t = pt[pti[0] % len(pt)]; pti[0] += 1; return t

        FM_CHUNKS = [
            (0, "val"), (128, "val"), (256, "glu"), (384, "glu"), (512, "cgate"), (640, "cgate"),
            (768, "pin"), (896, "pin"), (1024, "pgate"), (1152, "pgate")]

        def do_slot(s):
            b = s % 2
            P.dma("sp", hblk[b][0][:], hA[s * 128:(s + 1) * 128, :], writes=[hblk[b][1]])
            P.dma("sp", hhal[b][0][:], hH[s * HALO:(s + 1) * HALO, :], writes=[hhal[b][1]])
            P.dma("sp", cst[b][0][:], cs[s * 128:(s + 1) * 128, :], writes=[cst[b][1]])
            for (h_, sq_, st_, hn_, np_) in ((hblk[b], sq[b], st1[b], hn[b], 128), (hhal[b], sqh[b], st1h[b], hnh[b], HALO)):
                P.op("act", lambda e, h_=h_, sq_=sq_: e.activation(out=sq_[0][:], in_=h_[0][:], func=AF.Square),
                     reads=[h_[1]], writes=[sq_[1]])
                P.op("dve", lambda e, sq_=sq_, st_=st_: e.tensor_reduce(out=st_[0][:, 0:1], in_=sq_[0][:], axis=AX.X, op=ALU.add),
                     reads=[sq_[1]], writes=[st_[1]])
                P.op("dve", lambda e, st_=st_: e.tensor_scalar(out=st_[0][:, 1:2], in0=st_[0][:, 0:1], scalar1=1.0 / D, scalar2=EPS,
                                                               op0=ALU.mult, op1=ALU.add), reads=[st_[1]], writes=[st_[1]])
                P.op("act", lambda e, st_=st_: e.activation(out=st_[0][:, 2:3], in_=st_[0][:, 1:2], func=AF.Sqrt),
                     reads=[st_[1]], writes=[st_[1]])
                P.op("dve", lambda e, st_=st_: e.reciprocal(out=st_[0][:, 3:4], in_=st_[0][:, 2:3]), reads=[st_[1]], writes=[st_[1]])
                P.op("dve", lambda e, h_=h_, st_=st_, hn_=hn_, np_=np_: e.scalar_tensor_tensor(
                    out=hn_[0][:], in0=h_[0][:], scalar=st_[0][:, 3:4], in1=gbc[0:np_, :], op0=ALU.mult, op1=ALU.mult),
                    reads=[h_[1], st_[1], rgbc], writes=[hn_[1]])
            if stage < 2:
                return
            ptt = nextpt()
            for c in range(8):
                P.op("pe", lambda e, c=c, ptt=ptt: e.transpose(out=ptt[0][:, c * 128:(c + 1) * 128], in_=hn[b][0][:, c * 128:(c + 1) * 128],
                                                              identity=idb[:]),
                     reads=[hn[b][1], ridb], writes=[ptt[1]])
            P.op("act", lambda e, ptt=ptt: e.copy(out=nT[b][0][:, :, HALO:NT], in_=ptt[0][:, :].rearrange("p (c t) -> p c t", c=8)),
                 reads=[ptt[1]], writes=[nT[b][1]])
            ptt = nextpt()
            for c in range(8):
                P.op("pe", lambda e, c=c, ptt=ptt: e.transpose(out=ptt[0][:, c * HALO:(c + 1) * HALO], in_=hnh[b][0][:, c * 128:(c + 1) * 128],
                                                              identity=idb[0:HALO, 0:HALO]),
                     reads=[hnh[b][1], ridb], writes=[ptt[1]])
            P.op("dve", lambda e, ptt=ptt: e.tensor_copy(out=nT[b][0][:, :, 0:HALO],
                                                         in_=ptt[0][:, 0:8 * HALO].rearrange("p (c t) -> p c t", c=8)),
                 reads=[ptt[1]], writes=[nT[b][1]])
            if stage < 3:
                return
            for ci_, (c0, kind) in enumerate(FM_CHUNKS):
                cc = ci_ % 2
                nt0 = 0 if kind in ("val", "glu", "pin") else HALO
                nw = NT - nt0
                pz = nextpb()
                for kc in range(8):
                    P.op("pe", lambda e, kc=kc, pz=pz, c0=c0, nt0=nt0, nw=nw: e.matmul(
                        pz[0][:, 0:nw], lhsT=Wb[:, kc, c0:c0 + 128], rhs=nT[b][0][:, kc, nt0:NT], start=(kc == 0), stop=(kc == 7)),
                        reads=[rWb, nT[b][1]], writes=[pz[1]])
                bias = bfm[:, ci_:ci_ + 1]
                if kind == "val":
                    P.op("act", lambda e, pz=pz, cc=cc, bias=bias: e.activation(out=val[b][0][:, cc, :], in_=pz[0][:, 0:NT], func=AF.Identity, bias=bias),
                         reads=[pz[1], rbfm], writes=[val[b][1]])
                elif kind == "glu":
                    P.op("act", lambda e, pz=pz, cc=cc, bias=bias: e.activation(out=sgl[b][0][:, cc, :], in_=pz[0][:, 0:NT], func=AF.Sigmoid, bias=bias),
                         reads=[pz[1], rbfm], writes=[sgl[b][1]])
                elif kind == "pin":
                    P.op("act", lambda e, pz=pz, cc=cc, bias=bias: e.activation(out=pin[b][0][:, cc, :], in_=pz[0][:, 0:NT], func=AF.Identity, bias=bias),
                         reads=[pz[1], rbfm], writes=[pin[b][1]])
                elif kind == "cgate":
                    P.op("act", lambda e, pz=pz, cc=cc, bias=bias: e.activation(out=cgate[b][0][:, cc, :], in_=pz[0][:, 0:128], func=AF.Silu, bias=bias),
                         reads=[pz[1], rbfm], writes=[cgate[b][1]])
                else:
                    P.op("act", lambda e, pz=pz, cc=cc, bias=bias: e.activation(out=pgate[b][0][:, cc, :], in_=pz[0][:, 0:128], func=AF.Silu, bias=bias),
                         reads=[pz[1], rbfm], writes=[pgate[b][1]])
            if stage < 4:
                return
            P.op("dve", lambda e: e.tensor_tensor(out=u[b][0][:], in0=val[b][0][:], in1=sgl[b][0][:], op=ALU.mult),
                 reads=[val[b][1], sgl[b][1]], writes=[u[b][1]])
            P.op("dve", lambda e: e.tensor_scalar(out=u[b][0][:, :, 0:HALO], in0=u[b][0][:, :, 0:HALO], scalar1=hok_t[:, s:s + 1], scalar2=None,
                                                  op0=ALU.mult), reads=[u[b][1], rhok], writes=[u[b][1]])
            P.op("pool", lambda e: e.tensor_scalar(out=pin[b][0][:, :, 0:HALO], in0=pin[b][0][:, :, 0:HALO], scalar1=hok_t[:, s:s + 1], scalar2=None,
                                                   op0=ALU.mult), reads=[pin[b][1], rhok], writes=[pin[b][1]])
            for cc in range(2):
                eng = "dve"
                for k in range(31):
                    o0 = HALO - 30 + k
                    if k == 0:
                        P.op(eng, lambda e, cc=cc, o0=o0, k=k: e.tensor_scalar(
                            out=cacc[b][0][:, cc, :], in0=u[b][0][:, cc, o0:o0 + 128], scalar1=wdw_t[:, cc, k:k + 1], scalar2=cv[:, 0, cc:cc + 1],
                            op0=ALU.mult, op1=ALU.add), reads=[u[b][1], rwdw, rcv], writes=[cacc[b][1]])
                    else:
                        P.op(eng, lambda e, cc=cc, o0=o0, k=k: e.scalar_tensor_tensor(
                            out=cacc[b][0][:, cc, :], in0=u[b][0][:, cc, o0:o0 + 128], scalar=wdw_t[:, cc, k:k + 1], in1=cacc[b][0][:, cc, :],
                            op0=ALU.mult, op1=ALU.add), reads=[u[b][1], rwdw, cacc[b][1]], writes=[cacc[b][1]])
            pm = nextpb()
            for cc in range(2):
                P.op("pe", lambda e, cc=cc, pm=pm: e.matmul(pm[0][:, 0:128], lhsT=onesf[:], rhs=cacc[b][0][:, cc, :], start=(cc == 0), stop=(cc == 1)),
                     reads=[rones, cacc[b][1]], writes=[pm[1]])
            P.op("dve", lambda e, pm=pm: e.tensor_tensor(out=xc[b][0][:], in0=cacc[b][0][:], in1=bc_mid(pm[0][:, 0:128], 2), op=ALU.subtract),
                 reads=[cacc[b][1], pm[1]], writes=[xc[b][1]])
            P.op("act", lambda e: e.activation(out=xsq[b][0][:], in_=xc[b][0][:], func=AF.Square), reads=[xc[b][1]], writes=[xsq[b][1]])
            pv = nextpb()
            for cc in range(2):
                P.op("pe", lambda e, cc=cc, pv=pv: e.matmul(pv[0][:, 0:128], lhsT=onesf[:], rhs=xsq[b][0][:, cc, :], start=(cc == 0), stop=(cc == 1)),
                     reads=[rones, xsq[b][1]], writes=[pv[1]])
            P.op("act", lambda e, pv=pv: e.activation(out=rstd_b[b][0][:], in_=pv[0][:, 0:128], func=AF.Sqrt, bias=EPS),
                 reads=[pv[1]], writes=[rstd_b[b][1]])
            P.op("dve", lambda e: e.reciprocal(out=rstd_b[b][0][:], in_=rstd_b[b][0][:]), reads=[rstd_b[b][1]], writes=[rstd_b[b][1]])
            P.op("dve", lambda e: e.tensor_tensor(out=yn[b][0][:], in0=xc[b][0][:], in1=bc_mid(rstd_b[b][0][:], 2), op=ALU.mult),
                 reads=[xc[b][1], rstd_b[b][1]], writes=[yn[b][1]])
            for cc in range(2):
                P.op("act", lambda e, cc=cc: e.activation(out=sact[b][0][:, cc, :], in_=yn[b][0][:, cc, :], func=AF.Silu,
                                                          scale=cv[:, 1, cc:cc + 1], bias=cv[:, 2, cc:cc + 1]),
                     reads=[yn[b][1], rcv], writes=[sact[b][1]])
            for oc in range(2):
                pz = nextpb()
                for kc in range(2):
                    P.op("pe", lambda e, oc=oc, kc=kc, pz=pz: e.matmul(pz[0][:, 0:128], lhsT=pwb[:, kc, oc * 128:(oc + 1) * 128], rhs=sact[b][0][:, kc, :],
                                                                       start=(kc == 0), stop=(kc == 1)),
                         reads=[rpwb, sact[b][1]], writes=[pz[1]])
                P.op("dve", lambda e, oc=oc, pz=pz: e.scalar_tensor_tensor(out=yab[b][0][:, oc, :], in0=pz[0][:, 0:128], scalar=cv[:, 3, oc:oc + 1],
                                                                           in1=cgate[b][0][:, oc, :], op0=ALU.add, op1=ALU.mult),
                     reads=[pz[1], rcv, cgate[b][1]], writes=[yab[b][1]])
            if stage < 5:
                return
            pe_ = "pool"
            x_ = pin[b]
            P.op(pe_, lambda e: e.tensor_tensor(out=ps2[b][0][:, :, 1:NT], in0=x_[0][:, :, 1:NT], in1=x_[0][:, :, 0:NT - 1], op=ALU.add),
                 reads=[x_[1]], writes=[ps2[b][1]])
            P.op(pe_, lambda e: e.tensor_tensor(out=ps4[b][0][:, :, 3:NT], in0=ps2[b][0][:, :, 3:NT], in1=ps2[b][0][:, :, 1:NT - 2], op=ALU.add),
                 reads=[ps2[b][1]], writes=[ps4[b][1]])
            P.op(pe_, lambda e: e.tensor_tensor(out=ps8[b][0][:, 7:NT], in0=ps4[b][0][:, 1, 7:NT], in1=ps4[b][0][:, 1, 3:NT - 4], op=ALU.add),
                 reads=[ps4[b][1]], writes=[ps8[b][1]])
            P.op(pe_, lambda e: e.tensor_tensor(out=ps16[b][0][:, 15:NT], in0=ps8[b][0][:, 15:NT], in1=ps8[b][0][:, 7:NT - 8], op=ALU.add),
                 reads=[ps8[b][1]], writes=[ps16[b][1]])
            srcs = [(ps2[b], lambda t: t[0][0:64, 0, HALO:NT], 0, 0), (ps4[b], lambda t: t[0][64:128, 0, HALO:NT], 64, 0),
                    (ps8[b], lambda t: t[0][0:64, HALO:NT], 0, 1), (ps16[b], lambda t: t[0][64:128, HALO:NT], 64, 1)]
            for (src, view, p0, cc) in srcs:
                if s == 0:
                    P.op(pe_, lambda e, src=src, view=view, p0=p0, cc=cc: e.tensor_tensor(
                        out=ptmp[b][0][p0:p0 + 64, cc, :], in0=view(src), in1=rc0_t[p0:p0 + 64, cc, :], op=ALU.mult),
                        reads=[src[1], rrc0], writes=[ptmp[b][1]])
                else:
                    P.op(pe_, lambda e, src=src, view=view, p0=p0, cc=cc: e.tensor_scalar(
                        out=ptmp[b][0][p0:p0 + 64, cc, :], in0=view(src), scalar1=cv[p0:p0 + 64, 6, cc:cc + 1], scalar2=None, op0=ALU.mult),
                        reads=[src[1], rcv], writes=[ptmp[b][1]])
            P.op(pe_, lambda e: e.tensor_tensor(out=dpl[b][0][:], in0=ptmp[b][0][:], in1=x_[0][:, :, HALO:NT], op=ALU.subtract),
                 reads=[ptmp[b][1], x_[1]], writes=[dpl[b][1]])
            for cc in range(2):
                pz = nextpb()
                P.op("pe", lambda e, cc=cc, pz=pz: e.matmul(pz[0][:, 0:128], lhsT=plb[:, cc, :], rhs=dpl[b][0][:, cc, :], start=True, stop=True),
                     reads=[rplb, dpl[b][1]], writes=[pz[1]])
                P.op("dve", lambda e, cc=cc, pz=pz: e.tensor_scalar(out=ptmp[b][0][:, cc, :], in0=pz[0][:, 0:128], scalar1=cv[:, 4, cc:cc + 1],
                                                                    scalar2=cv[:, 5, cc:cc + 1], op0=ALU.add, op1=ALU.mult),
                     reads=[pz[1], rcv], writes=[ptmp[b][1]])
            P.op("pool", lambda e: e.tensor_tensor(out=yab[b][0][:, 2:4, :], in0=ptmp[b][0][:], in1=pgate[b][0][:], op=ALU.mult),
                 reads=[ptmp[b][1], pgate[b][1]], writes=[yab[b][1]])
            P.dma("pool", yab_o[s], yab[b][0][:], reads=[yab[b][1]])

            if stage < 6:
                return
            def tm_group(c0, cw, dst):
                pz = nextpb()
                for kc in range(8):
                    P.op("pe", lambda e, kc=kc, pz=pz: e.matmul(pz[0][:, 0:cw], lhsT=nT[b][0][:, kc, HALO:NT], rhs=Wb[:, kc, c0:c0 + cw],
                                                                start=(kc == 0), stop=(kc == 7)),
                         reads=[rWb, nT[b][1]], writes=[pz[1]])
                P.op("dve", lambda e, pz=pz: e.tensor_tensor(out=dst[0][:, 0:cw], in0=pz[0][:, 0:cw], in1=bbc[:, c0:c0 + cw], op=ALU.add),
                     reads=[pz[1], rbbc], writes=[dst[1]])

            def rope(src, nh, dst, eng="pool"):
                xv = src[0].rearrange("p (h t d) -> p h t d", h=nh, t=2)
                ov = dst[0].rearrange("p (h t d) -> p h t d", h=nh, t=2)
                cosb = bc_mid(cst[b][0][:, 0:32], nh)
                sinb = bc_mid(cst[b][0][:, 32:64], nh)
                t1 = rt1[b][0][:, 0:nh * 32].rearrange("p (h d) -> p h d", h=nh)
                t2 = rt2[b][0][:, 0:nh * 32].rearrange("p (h d) -> p h d", h=nh)
                rd = [src[1], cst[b][1]]
                if nh == 1:
                    x0, x1 = src[0][:, 0:32], src[0][:, 32:64]
                    o0_, o1_ = dst[0][:, 0:32], dst[0][:, 32:64]
                    cb, sn = cst[b][0][:, 0:32], cst[b][0][:, 32:64]
                    a1, a2 = rt1[b][0][:, 0:32], rt2[b][0][:, 0:32]
                    P.op(eng, lambda e: e.tensor_tensor(out=a1, in0=x0, in1=cb, op=ALU.mult), reads=rd, writes=[rt1[b][1]])
                    P.op(eng, lambda e: e.tensor_tensor(out=a2, in0=x1, in1=sn, op=ALU.mult), reads=rd, writes=[rt2[b][1]])
                    P.op(eng, lambda e: e.tensor_tensor(out=o0_, in0=a1, in1=a2, op=ALU.subtract), reads=[rt1[b][1], rt2[b][1]], writes=[dst[1]])
                    P.op(eng, lambda e: e.tensor_tensor(out=a1, in0=x1, in1=cb, op=ALU.mult), reads=rd, writes=[rt1[b][1]])
                    P.op(eng, lambda e: e.tensor_tensor(out=a2, in0=x0, in1=sn, op=ALU.mult), reads=rd, writes=[rt2[b][1]])
                    P.op(eng, lambda e: e.tensor_tensor(out=o1_, in0=a1, in1=a2, op=ALU.add), reads=[rt1[b][1], rt2[b][1]], writes=[dst[1]])
                    return
                P.op(eng, lambda e: e.tensor_tensor(out=t1, in0=xv[:, :, 0, :], in1=cosb, op=ALU.mult), reads=rd, writes=[rt1[b][1]])
                P.op(eng, lambda e: e.tensor_tensor(out=t2, in0=xv[:, :, 1, :], in1=sinb, op=ALU.mult), reads=rd, writes=[rt2[b][1]])
                P.op(eng, lambda e: e.tensor_tensor(out=ov[:, :, 0, :], in0=t1, in1=t2, op=ALU.subtract), reads=[rt1[b][1], rt2[b][1]], writes=[dst[1]])
                P.op(eng, lambda e: e.tensor_tensor(out=t1, in0=xv[:, :, 1, :], in1=cosb, op=ALU.mult), reads=rd, writes=[rt1[b][1]])
                P.op(eng, lambda e: e.tensor_tensor(out=t2, in0=xv[:, :, 0, :], in1=sinb, op=ALU.mult), reads=rd, writes=[rt2[b][1]])
                P.op(eng, lambda e: e.tensor_tensor(out=ov[:, :, 1, :], in0=t1, in1=t2, op=ALU.add), reads=[rt1[b][1], rt2[b][1]], writes=[dst[1]])

            tm_group(1280, 512, ztm[b])
            rope((ztm[b][0][:, 0:512], ztm[b][1]), 8, (rot[b][0][:, 0:512], rot[b][1]), eng="pool")
            ptt = nextpt()
            for hp in range(4):
                P.op("pe", lambda e, hp=hp, ptt=ptt: e.transpose(out=ptt[0][:, hp * 128:(hp + 1) * 128], in_=rot[b][0][:, hp * 128:(hp + 1) * 128], identity=idb[:]),
                     reads=[rot[b][1], ridb], writes=[ptt[1]])
            P.op("act", lambda e, ptt=ptt: e.copy(out=qT[b][0][:].rearrange("p a b -> p (a b)"), in_=ptt[0][:, 0:512]), reads=[ptt[1]], writes=[qT[b][1]])
            P.dma("pool", qT_o[s], qT[b][0][:], reads=[qT[b][1]])
            if stage < 7:
                return
            tm_group(1792, 512, zt2[b])
            rope((zt2[b][0][:, 0:512], zt2[b][1]), 8, (rot[b][0][:, 0:512], rot[b][1]), eng="dve")
            ptt = nextpt()
            for hp in range(4):
                P.op("pe", lambda e, hp=hp, ptt=ptt: e.transpose(out=ptt[0][:, hp * 128:(hp + 1) * 128], in_=rot[b][0][:, hp * 128:(hp + 1) * 128], identity=idb[:]),
                     reads=[rot[b][1], ridb], writes=[ptt[1]])
            P.op("act", lambda e, ptt=ptt: e.copy(out=kT[b][0][:].rearrange("p a b -> p (a b)"), in_=ptt[0][:, 0:512]), reads=[ptt[1]], writes=[kT[b][1]])
            P.dma("pool", kT_o[s], kT[b][0][:], reads=[kT[b][1]])
            if stage < 8:
                return
            pz = nextpb()
            for kc in range(8):
                P.op("pe", lambda e, kc=kc, pz=pz: e.matmul(pz[0][:, 0:512], lhsT=nT[b][0][:, kc, HALO:NT], rhs=Wb[:, kc, 2304:2816],
                                                            start=(kc == 0), stop=(kc == 7)), reads=[rWb, nT[b][1]], writes=[pz[1]])
            P.op("dve", lambda e, pz=pz: e.tensor_tensor(out=va[b][0][:, :, 0:64], in0=pz[0][:, 0:512].rearrange("p (h d) -> p h d", h=8),
                                                         in1=bbc[:, 2304:2816].rearrange("p (h d) -> p h d", h=8), op=ALU.add),
                 reads=[pz[1], rbbc], writes=[va[b][1]])
            P.dma("pool", va_o[s], va[b][0][:].rearrange("p h d -> p (h d)"), reads=[va[b][1]])
            if stage < 9:
                return
            tm_group(2816, 512, ag1[b])
            P.op("act", lambda e: e.activation(out=ag[b][0][:], in_=ag1[b][0][:], func=AF.Silu), reads=[ag1[b][1]], writes=[ag[b][1]])
            P.dma("pool", ag_o[s], ag[b][0][:], reads=[ag[b][1]])
            if stage < 10:
                return
            tm_group(3328, 324, zi[b])
            P.op("act", lambda e: e.activation(out=wia[b][0][:, 0:4], in_=zi[b][0][:, 320:324], func=AF.Abs, scale=0.125),
                 reads=[zi[b][1]], writes=[wia[b][1]])
            P.op("act", lambda e: e.activation(out=wia[b][0][:, 4:8], in_=zi[b][0][:, 320:324], func=AF.Sign), reads=[zi[b][1]], writes=[wia[b][1]])
            P.dma("pool", sg_o[s], wia[b][0][:, 4:8], reads=[wia[b][1]])
            if stage < 11:
                return
            for hh in range(4):
                P.op("dve", lambda e, hh=hh: e.tensor_scalar(out=zi[b][0][:, hh * 64:(hh + 1) * 64], in0=zi[b][0][:, hh * 64:(hh + 1) * 64],
                                                             scalar1=wia[b][0][:, hh:hh + 1], scalar2=None, op0=ALU.mult),
                     reads=[zi[b][1], wia[b][1]], writes=[zi[b][1]])
            rope((zi[b][0][:, 0:256], zi[b][1]), 4, (qik[b][0][:, 0:256], qik[b][1]), eng="pool")
            rope((zi[b][0][:, 256:320], zi[b][1]), 1, (qik[b][0][:, 256:320], qik[b][1]), eng="dve")
            ptt = nextpt()
            for hp in range(3):
                P.op("pe", lambda e, hp=hp, ptt=ptt: e.transpose(out=ptt[0][:, hp * 128:(hp + 1) * 128], in_=qik[b][0][:, hp * 128:(hp + 1) * 128], identity=idb[:]),
                     reads=[qik[b][1], ridb], writes=[ptt[1]])
            P.op("act", lambda e, ptt=ptt: e.copy(out=qikT[b][0][:].rearrange("p a b -> p (a b)"), in_=ptt[0][:, 0:384]), reads=[ptt[1]], writes=[qikT[b][1]])
            P.dma("pool", qi_o[s], qikT[b][0][:, 0:2, :], reads=[qikT[b][1]])
            P.dma("pool", ki_o[s], qikT[b][0][0:64, 2, :], reads=[qikT[b][1]])

        for s_ in range(NS if stage >= 1 else 0):
            do_slot(s_)

        fw_ = P.all_dma_tokens()
        P.emit(final_waits={"pool": fw_, "sp": fw_})
    return nc


S = 16384
NIT = 12
NEG = -1.0e30


def bc_mid(ap2d, n):
    a = ap2d.ap
    return AP(ap2d.tensor, ap2d.offset, [list(a[0]), [0, n], list(a[1])])


def build_B(NS, SK=S):
    nc = bass.Bass("TRN2", target_bir_lowering=False)
    dr = lambda n, s, d, k="ExternalInput": nc.dram_tensor(n, list(s), d, kind=k).ap()
    qT_d = dr("qT", [NS, 128, 4, 128], BF16)
    qi_d = dr("qiT", [NS, 128, 2, 128], BF16)
    sg_d = dr("sg", [NS, 128, 4], F32)
    ag_d = dr("ag", [NS, 128, 512], BF16)
    yab_d = dr("yab", [NS, 128, 4, 128], BF16)
    hA = dr("hA", [NS * 128, D], F32)
    pA = dr("pA", [NS * 128, 256], F32)
    KT = dr("KT", [128, 4, SK], BF16)
    VA = dr("VA", [SK // 128, 128, 528], BF16)
    KI = dr("KI", [128, SK], BF16)
    qoff_d = dr("qoff", [128, 2], F32)
    iota_d = dr("iota", [128, 512], F32)
    fl_l = dr("fl_l", [3, 32, 128], BF16)
    fl_r = dr("fl_r", [3, 512], BF16)
    w_out = dr("w_out", [D, D], F32)
    w_g = dr("w_g", [D, D], F32)
    w_p = dr("w_p", [256, D], F32)
    gF = dr("gF", [1, D], F32)
    ident_d = dr("ident", [128, 128], F32)
    h_o = dr("h_o", [NS * 128, D], F32, "ExternalOutput")
    fsel_d = dr("fsel", [128, 1], F32)

    with ExitStack() as st:
        P = Prog(nc, st)
        sb, ps = P.sb, P.ps
        WO = sb("WO", [128, 8, D], BF16); rWO = Res()
        WG = sb("WG", [128, 8, D], BF16); rWG = Res()
        WP = sb("WP", [128, 2, D], BF16); rWP = Res()
        gFb = sb("gFb", [128, D], F32); rgF = Res()
        idf = sb("idf", [128, 128], F32); ridf = Res()
        idb = sb("idb", [128, 128], BF16); ridb = Res()
        qoff = sb("qoff_t", [128, 2], F32); rqoff = Res()
        iota = sb("iota_t", [128, 512], F32); riota = Res()
        fll = sb("fll", [3, 32, 128], BF16); rfll = Res()
        flr = sb("flr", [3, 512], BF16); rflr = Res()
        score = sb("score", [128, S], F32)
        rsc = [Res() for _ in range(32)]
        stg = [score[:, i * 1024:(i + 1) * 1024] for i in range(2)]; rstg = [rsc[0], rsc[2]]
        fsel = sb("fsel_t", [128, 1], F32); rfsel = Res()
        P.dma("sp", fsel[:], fsel_d, writes=[rfsel])
        P.dma("sp", gFb[:], gF.partition_broadcast(128), writes=[rgF])
        P.dma("sp", idf[:], ident_d, writes=[ridf])
        P.dma("sp", qoff[:], qoff_d, writes=[rqoff])
        P.dma("sp", iota[:], iota_d, writes=[riota])
        P.dma("sp", fll[:], fl_l, writes=[rfll])
        P.dma("sp", flr[:], fl_r, writes=[rflr])
        P.op("dve", lambda e: e.tensor_copy(out=idb[:], in_=idf[:]), reads=[ridf], writes=[ridb])
        ci = 0
        for (Wd, Wt, rW, nk) in ((w_out, WO, rWO, 8), (w_g, WG, rWG, 8), (w_p, WP, rWP, 2)):
            for kc in range(nk):
                i = ci % 2
                P.dma("sp", stg[i], Wd[kc * 128:(kc + 1) * 128, :], writes=[rstg[i]])
                eng = ("dve", "pool")[ci % 2]
                P.op(eng, lambda e, i=i, kc=kc, Wt=Wt: e.tensor_copy(out=Wt[:, kc, :], in_=stg[i]), reads=[rstg[i]], writes=[rW])
                ci += 1

        def dbl(name, shape, dt, n=2):
            return [(sb("%s%d" % (name, i), shape, dt), Res()) for i in range(n)]

        class Rot:
            def __init__(self, tiles):
                self.t = tiles; self.i = 0

            def next(self):
                t = self.t[self.i % len(self.t)]; self.i += 1; return t

        qTt = dbl("qTt", [128, 4, 128], BF16)
        qit = dbl("qit", [128, 2, 128], BF16)
        sgt = dbl("sgt", [128, 4], F32)
        agt = dbl("agt", [128, 512], BF16)
        yabt = dbl("yabt", [128, 4, 128], BF16)
        hbl = dbl("hbl", [128, D], F32, 1) * 2
        pbl = dbl("pbl", [128, 256], F32)
        kit = Rot(dbl("kit", [128, 512], BF16, 3))
        ktt = Rot(dbl("ktt", [128, 4, 512], BF16, 2))
        vat = Rot(dbl("vat", [128, 4, 528], BF16, 2))
        Rb = Rot(dbl("Rb", [128, 512], F32, 3))
        Eb = Rot(dbl("Eb", [128, 512], BF16, 3))
        Pb = Rot(dbl("Pb", [128, 512], BF16, 3))
        junk = sb("junk", [128, 1024], BF16); rjunk = Res()
        sm = sb("sm", [128, 512], F32); rsm = Res()
        sm2 = sb("sm2", [128, 512], F32); rsm2 = Res()
        cbt = sm2; rcb = rsm2
        tv = sb("tv", [128, 16], F32)
        rtv = Res()
        cnt8 = sb("cnt8", [128, 16], F32); rcnt8 = Res()
        mk = Rot(dbl("mk", [128, 512], BF16, 2))
        mT = Rot(dbl("mT", [128, 512], BF16, 2))
        oacc = sb("oacc", [128, 8, 66], F32); roacc = Res()
        rec = sb("rec", [128, 8], F32); rrec = Res()
        ycf = score[:, 4096:4608]; rycf = [rsc[8]]
        ycb = sb("ycb", [128, 512], BF16); rycb = Res()
        ycT = sb("ycT", [128, 4, 128], BF16); rycT = Res()
        h1 = score[:, 0:1024]; rh1 = [rsc[0], rsc[1]]
        h1b = sb("h1b", [128, D], BF16); rh1b = Res()
        h1T = sb("h1T", [128, 8, 128], BF16); rh1T = Res()
        gsb = score[:, 1024:2048]; rgsb = [rsc[2], rsc[3]]
        pbb = sb("pbb", [128, 256], BF16); rpbb = Res()
        pT = sb("pT", [128, 2, 128], BF16); rpT = Res()
        h2 = [(score[:, 2048:3072], [rsc[4], rsc[5]])] * 2
        sqt = h1; rsq = rh1
        st1 = sb("st1", [128, 4], F32); rst1 = Res()
        hnt = [(score[:, 3072:4096], [rsc[6], rsc[7]])] * 2

        pa = Rot([(ps("pa%d" % i, [128, 512], F32), Res()) for i in range(4)])
        pf = (ps("pf", [128, 512], F32), Res())
        pm = (ps("pm", [128, 1024], BF16), Res())
        po = [(ps("po%d" % i, [128, 512], F32), Res()) for i in range(2)]

        def do_slot(s):
            b = s % 2
            T = s + 1
            L = 512 * T
            o = s % 2
            qT_, qi_, sg_, ag_, yab_, h_, p_ = qTt[b], qit[b], sgt[b], agt[b], yabt[b], hbl[b], pbl[b]
            P.dma("sp", qi_[0][:], qi_d[s], writes=[qi_[1]])
            P.dma("sp", sg_[0][:], sg_d[s], writes=[sg_[1]])
            P.dma("sp", qT_[0][:], qT_d[s], writes=[qT_[1]])
            P.dma("sp", ag_[0][:], ag_d[s], writes=[ag_[1]])
            P.dma("sp", yab_[0][:], yab_d[s], writes=[yab_[1]])
            P.dma("sp", h_[0][:], hA[s * 128:(s + 1) * 128, :], writes=[h_[1]])
            P.dma("sp", p_[0][:], pA[s * 128:(s + 1) * 128, :], writes=[p_[1]])
            for t in range(T):
                kt = kit.next()
                P.dma("sp", kt[0][:], KI[:, t * 512:(t + 1) * 512], writes=[kt[1]])
                P.op("pe", lambda e, t=t: e.matmul(pf[0][:, :], lhsT=fll[:, t, :], rhs=flr[:, :], start=True, stop=True),
                     reads=[rfll, rflr], writes=[pf[1]])
                sc = score[:, t * 512:(t + 1) * 512]
                for hh in range(4):
                    pz = pa.next()
                    p0 = (hh % 2) * 64
                    P.op("pe", lambda e, pz=pz, p0=p0, hh=hh, kt=kt: e.matmul(pz[0][:, :], lhsT=qi_[0][p0:p0 + 64, hh // 2, :], rhs=kt[0][p0:p0 + 64, :],
                                                                             start=True, stop=True),
                         reads=[qi_[1], kt[1]], writes=[pz[1]])
                    rb = Rb.next()
                    P.op("act", lambda e, pz=pz, rb=rb: e.activation(out=rb[0][:], in_=pz[0][:, :], func=AF.Relu), reads=[pz[1]], writes=[rb[1]])
                    if hh == 0:
                        P.op("dve", lambda e, rb=rb, sc=sc: e.scalar_tensor_tensor(out=sc, in0=rb[0][:], scalar=sg_[0][:, 0:1], in1=pf[0][:, :],
                                                                                   op0=ALU.mult, op1=ALU.add),
                             reads=[rb[1], sg_[1], pf[1]], writes=[rsc[t]])
                    else:
                        P.op("dve", lambda e, rb=rb, sc=sc, hh=hh: e.scalar_tensor_tensor(out=sc, in0=rb[0][:], scalar=sg_[0][:, hh:hh + 1], in1=sc,
                                                                                          op0=ALU.mult, op1=ALU.add),
                             reads=[rb[1], sg_[1], rsc[t]], writes=[rsc[t]])
                if t == T - 1:
                    P.op("dve", lambda e: e.tensor_scalar(out=cbt[:], in0=iota[:], scalar1=qoff[:, o:o + 1], scalar2=NEG, op0=ALU.is_gt, op1=ALU.mult),
                         reads=[riota, rqoff], writes=[rcb])
                    P.op("dve", lambda e, sc=sc: e.tensor_tensor(out=sc, in0=sc, in1=cbt[:], op=ALU.add), reads=[rsc[t], rcb], writes=[rsc[t]])
            rrow = rsc[0:T]
            if T == 1:
                P.op("dve", lambda e: e.tensor_copy(out=sm[:], in_=score[:, 0:512]), reads=rrow, writes=[rsm])
            else:
                P.op("dve", lambda e: e.tensor_reduce(out=sm[:], in_=score[:, 0:L].rearrange("p (g k) -> p g k", k=T), axis=AX.X, op=ALU.max),
                     reads=rrow, writes=[rsm])
            P.op("dve", lambda e: e.tensor_scalar(out=sm2[:], in0=sm[:], scalar1=-1.0e29, scalar2=2.0e30, op0=ALU.is_lt, op1=ALU.mult),
                 reads=[rsm], writes=[rsm2])
            P.op("dve", lambda e: e.tensor_tensor(out=sm2[:], in0=sm2[:], in1=sm[:], op=ALU.add), reads=[rsm, rsm2], writes=[rsm2])
            P.op("dve", lambda e: e.tensor_reduce(out=tv[:, 0:1], in_=sm2[:], axis=AX.X, op=ALU.min), reads=[rsm2], writes=[rtv])
            P.op("dve", lambda e: e.tensor_reduce(out=tv[:, 1:2], in_=sm[:], axis=AX.X, op=ALU.max), reads=[rsm], writes=[rtv])
            P.op("dve", lambda e: e.tensor_tensor(out=tv[:, 2:3], in0=tv[:, 1:2], in1=tv[:, 0:1], op=ALU.subtract), reads=[rtv], writes=[rtv])
            P.op("dve", lambda e: e.tensor_copy(out=tv[:, 3:4], in_=tv[:, 0:1]), reads=[rtv], writes=[rtv])
            CH = 1024
            nch = (L + CH - 1) // CH
            for k in range(1, NIT + 1):
                P.op("dve", lambda e, k=k: e.tensor_scalar(out=tv[:, 4:5], in0=tv[:, 2:3], scalar1=float(2.0 ** -k), scalar2=None, op0=ALU.mult),
                     reads=[rtv], writes=[rtv])
                P.op("dve", lambda e: e.tensor_tensor(out=tv[:, 5:6], in0=tv[:, 3:4], in1=tv[:, 4:5], op=ALU.add), reads=[rtv], writes=[rtv])
                for c in range(nch):
                    c0 = c * CH
                    w = min(CH, L - c0)
                    P.op("dve", lambda e, c=c, c0=c0, w=w: e.tensor_scalar(out=junk[:, 0:w], in0=score[:, c0:c0 + w], scalar1=tv[:, 5:6], scalar2=0.0,
                                                                           op0=ALU.is_ge, op1=ALU.add, accum_out=cnt8[:, c:c + 1]),
                         reads=rrow + [rtv], writes=[rjunk, rcnt8])
                if nch > 1:
                    P.op("dve", lambda e: e.tensor_reduce(out=tv[:, 6:7], in_=cnt8[:, 0:nch], axis=AX.X, op=ALU.add), reads=[rcnt8], writes=[rtv])
                else:
                    P.op("dve", lambda e: e.tensor_copy(out=tv[:, 6:7], in_=cnt8[:, 0:1]), reads=[rcnt8], writes=[rtv])
                P.op("dve", lambda e: e.scalar_tensor_tensor(out=tv[:, 7:8], in0=tv[:, 6:7], scalar=255.5, in1=tv[:, 4:5], op0=ALU.is_ge, op1=ALU.mult),
                     reads=[rtv], writes=[rtv])
                P.op("dve", lambda e: e.tensor_tensor(out=tv[:, 3:4], in0=tv[:, 3:4], in1=tv[:, 7:8], op=ALU.add), reads=[rtv], writes=[rtv])
            for t in range(T):
                ktile = ktt.next()
                vtile = vat.next()
                P.dma("sp", ktile[0][:], KT[:, :, t * 512:(t + 1) * 512], writes=[ktile[1]])
                P.dma("sp", vtile[0][:], VA[4 * t:4 * t + 4].rearrange("c p f -> p c f"), writes=[vtile[1]])
                m_ = mk.next()
                P.op("dve", lambda e, m_=m_, t=t: e.tensor_scalar(out=m_[0][:], in0=score[:, t * 512:(t + 1) * 512], scalar1=tv[:, 3:4], scalar2=None,
                                                                  op0=ALU.is_ge), reads=[rsc[t], rtv], writes=[m_[1]])
                def emit_qk(hh, ktile=ktile):
                    p0 = (hh % 2) * 64
                    pz = pa.next()
                    for c in range(4):
                        P.op("pe", lambda e, c=c, pz=pz, p0=p0, hh=hh, ktile=ktile: e.matmul(
                            pz[0][:, c * 128:(c + 1) * 128], lhsT=ktile[0][p0:p0 + 64, hh // 2, c * 128:(c + 1) * 128], rhs=qT_[0][p0:p0 + 64, hh // 2, :],
                            start=True, stop=True), reads=[ktile[1], qT_[1]], writes=[pz[1]])
                    return pz

                def emit_rest(hh, pz, mt, vtile=vtile):
                    eb = Eb.next()
                    P.op("act", lambda e, pz=pz, eb=eb: e.activation(out=eb[0][:], in_=pz[0][:, :], func=AF.Exp, scale=0.125), reads=[pz[1]], writes=[eb[1]])
                    pb_ = Pb.next()
                    eng = "dve" if hh % 2 == 0 else "pool"
                    P.op(eng, lambda e, eb=eb, pb_=pb_, mt=mt: e.tensor_tensor(out=pb_[0][:], in0=eb[0][:], in1=mt[0][:], op=ALU.mult),
                         reads=[eb[1], mt[1]], writes=[pb_[1]])
                    pob = po[hh // 4]
                    for c in range(4):
                        P.op("pe", lambda e, c=c, pb_=pb_, vtile=vtile, hh=hh, pob=pob: e.matmul(
                            pob[0][:, (hh % 4) * 66:(hh % 4 + 1) * 66], lhsT=pb_[0][:, c * 128:(c + 1) * 128], rhs=vtile[0][:, c, hh * 66:(hh + 1) * 66],
                            start=(c == 0), stop=(c == 3)), reads=[pb_[1], vtile[1]], writes=[pob[1]])

                LAG = 2
                pzs = {}
                for hh in range(LAG):
                    pzs[hh] = emit_qk(hh)
                for c in range(4):
                    P.op("pe", lambda e, c=c, m_=m_: e.transpose(out=pm[0][:, c * 128:(c + 1) * 128], in_=m_[0][:, c * 128:(c + 1) * 128], identity=idb[:]),
                         reads=[m_[1], ridb], writes=[pm[1]])
                mt = mT.next()
                P.op("act", lambda e, mt=mt: e.copy(out=mt[0][:], in_=pm[0][:, 0:512]), reads=[pm[1]], writes=[mt[1]])
                for hh in range(8):
                    if hh + LAG < 8:
                        pzs[hh + LAG] = emit_qk(hh + LAG)
                    emit_rest(hh, pzs[hh], mt)
                for g in range(2):
                    ov = oacc[:, 4 * g:4 * g + 4, :].rearrange("p h d -> p (h d)")
                    if t == 0:
                        P.op("act", lambda e, g=g, ov=ov: e.copy(out=ov, in_=po[g][0][:, 0:264]), reads=[po[g][1]], writes=[roacc])
                    else:
                        P.op("dve", lambda e, g=g, ov=ov: e.tensor_tensor(out=ov, in0=ov, in1=po[g][0][:, 0:264], op=ALU.add),
                             reads=[po[g][1], roacc], writes=[roacc])
            P.op("dve", lambda e: e.reciprocal(out=rec[:], in_=oacc[:, :, 64]), reads=[roacc], writes=[rrec])
            for hh in range(8):
                P.op("dve", lambda e, hh=hh: e.tensor_scalar(out=ycf[:, hh * 64:(hh + 1) * 64], in0=oacc[:, hh, 0:64], scalar1=rec[:, hh:hh + 1], scalar2=None,
                                                             op0=ALU.mult), reads=[roacc, rrec], writes=[rycf])
            P.op("dve", lambda e: e.tensor_tensor(out=ycb[:], in0=ycf[:], in1=ag_[0][:], op=ALU.mult), reads=[rycf, ag_[1]], writes=[rycb])
            for c in range(4):
                P.op("pe", lambda e, c=c: e.transpose(out=pm[0][:, c * 128:(c + 1) * 128], in_=ycb[:, c * 128:(c + 1) * 128], identity=idb[:]),
                     reads=[rycb, ridb], writes=[pm[1]])
            P.op("act", lambda e: e.copy(out=ycT[:].rearrange("p a b -> p (a b)"), in_=pm[0][:, 0:512]), reads=[pm[1]], writes=[rycT])
            for n in range(2):
                pz = pa.next()
                for kc in range(8):
                    lt = yab_[0][:, kc, :] if kc < 4 else ycT[:, kc - 4, :]
                    P.op("pe", lambda e, kc=kc, pz=pz, lt=lt, n=n: e.matmul(pz[0][:, :], lhsT=lt, rhs=WO[:, kc, n * 512:(n + 1) * 512],
                                                                           start=(kc == 0), stop=(kc == 7)),
                         reads=[yab_[1], rycT, rWO], writes=[pz[1]])
                P.op("dve", lambda e, pz=pz, n=n: e.tensor_tensor(out=h1[:, n * 512:(n + 1) * 512], in0=pz[0][:, :], in1=h_[0][:, n * 512:(n + 1) * 512], op=ALU.add),
                     reads=[pz[1], h_[1]], writes=[rh1])
            P.op("act", lambda e: e.copy(out=h1b[:], in_=h1[:]), reads=[rh1], writes=[rh1b])
            for c in range(8):
                P.op("pe", lambda e, c=c: e.transpose(out=pm[0][:, c * 128:(c + 1) * 128], in_=h1b[:, c * 128:(c + 1) * 128], identity=idb[:]),
                     reads=[rh1b, ridb], writes=[pm[1]])
            P.op("act", lambda e: e.copy(out=h1T[:].rearrange("p a b -> p (a b)"), in_=pm[0][:, :]), reads=[pm[1]], writes=[rh1T])
            for n in range(2):
                pz = pa.next()
                for kc in range(8):
                    P.op("pe", lambda e, kc=kc, pz=pz, n=n: e.matmul(pz[0][:, :], lhsT=h1T[:, kc, :], rhs=WG[:, kc, n * 512:(n + 1) * 512],
                                                                    start=(kc == 0), stop=(kc == 7)), reads=[rh1T, rWG], writes=[pz[1]])
                P.op("act", lambda e, pz=pz, n=n: e.activation(out=gsb[:, n * 512:(n + 1) * 512], in_=pz[0][:, :], func=AF.Sigmoid), reads=[pz[1]], writes=[rgsb])
            P.op("pool", lambda e: e.tensor_copy(out=pbb[:], in_=p_[0][:]), reads=[p_[1]], writes=[rpbb])
            for c in range(2):
                P.op("pe", lambda e, c=c: e.transpose(out=pm[0][:, c * 128:(c + 1) * 128], in_=pbb[:, c * 128:(c + 1) * 128], identity=idb[:]),
                     reads=[rpbb, ridb], writes=[pm[1]])
            P.op("act", lambda e: e.copy(out=pT[:].rearrange("p a b -> p (a b)"), in_=pm[0][:, 0:256]), reads=[pm[1]], writes=[rpT])
            h2_ = h2[b]
            for n in range(2):
                pz = pa.next()
                for kc in range(2):
                    P.op("pe", lambda e, kc=kc, pz=pz, n=n: e.matmul(pz[0][:, :], lhsT=pT[:, kc, :], rhs=WP[:, kc, n * 512:(n + 1) * 512],
                                                                    start=(kc == 0), stop=(kc == 1)), reads=[rpT, rWP], writes=[pz[1]])
                P.op("dve", lambda e, pz=pz, n=n: e.tensor_tensor(out=gsb[:, n * 512:(n + 1) * 512], in0=pz[0][:, :], in1=gsb[:, n * 512:(n + 1) * 512], op=ALU.mult),
                     reads=[pz[1], rgsb], writes=[rgsb])
            P.op("pool", lambda e: e.tensor_tensor(out=h2_[0][:], in0=h1[:], in1=gsb[:], op=ALU.add), reads=[rh1, rgsb], writes=[h2_[1]])
            hn_ = hnt[b]
            P.op("act", lambda e: e.activation(out=sqt[:], in_=h2_[0][:], func=AF.Square), reads=[h2_[1]], writes=[rsq])
            P.op("dve", lambda e: e.tensor_reduce(out=st1[:, 0:1], in_=sqt[:], axis=AX.X, op=ALU.add), reads=[rsq], writes=[rst1])
            P.op("dve", lambda e: e.tensor_scalar(out=st1[:, 1:2], in0=st1[:, 0:1], scalar1=1.0 / D, scalar2=EPS, op0=ALU.mult, op1=ALU.add),
                 reads=[rst1], writes=[rst1])
            P.op("act", lambda e: e.activation(out=st1[:, 2:3], in_=st1[:, 1:2], func=AF.Sqrt), reads=[rst1], writes=[rst1])
            P.op("dve", lambda e: e.reciprocal(out=st1[:, 3:4], in_=st1[:, 2:3]), reads=[rst1], writes=[rst1])
            P.op("dve", lambda e: e.scalar_tensor_tensor(out=hn_[0][:], in0=h2_[0][:], scalar=st1[:, 3:4], in1=gFb[:], op0=ALU.mult, op1=ALU.mult),
                 reads=[h2_[1], rst1, rgF], writes=[hn_[1]])
            P.op("pool", lambda e: e.tensor_tensor(out=hn_[0][:], in0=hn_[0][:], in1=h2_[0][:], op=ALU.subtract), reads=[hn_[1], h2_[1]], writes=[hn_[1]])
            P.op("dve", lambda e: e.scalar_tensor_tensor(out=hn_[0][:], in0=hn_[0][:], scalar=fsel[:, 0:1], in1=h2_[0][:], op0=ALU.mult, op1=ALU.add),
                 reads=[hn_[1], h2_[1], rfsel], writes=[hn_[1]])
            P.dma("pool", h_o[s * 128:(s + 1) * 128, :], hn_[0][:], reads=[hn_[1]])

        for s_ in range(NS):
            do_slot(s_)
        fw_ = P.all_dma_tokens()
        P.emit(final_waits={"pool": fw_, "sp": fw_})
    return nc


BF = ml_dtypes.bfloat16
S = 16384
HALO = 32
NQB = S // 128


def slot_qb(core, s):
    j = core % 4
    return 8 * (s // 2) + (j if s % 2 == 0 else 7 - j)


def rope_table():
    half = 32
    inv = (np.float32(10000.0) ** (-np.arange(half, dtype=np.float32) / np.float32(half))).astype(np.float32)
    ang = np.arange(S, dtype=np.float32)[:, None] * inv[None, :]
    return np.concatenate([np.cos(ang), np.sin(ang)], axis=1).astype(np.float32)


def prep_A_weights(i, inp):
    w = {}
    w["w_in"] = np.ascontiguousarray(inp["w_in"][i])
    b = inp["b_in"][i]
    w["b_bc"] = np.ascontiguousarray(b[None, :])
    w["b_fm"] = np.ascontiguousarray(b[:1280].reshape(10, 128).T)
    w["g_bc"] = np.ascontiguousarray(inp["norm_g"][i][None, :])
    w["wdw"] = np.ascontiguousarray(inp["conv_dw_w"][i].T.reshape(2, 128, 31).transpose(1, 0, 2))
    cv = np.zeros((128, 7, 2), np.float32)
    for k, name in enumerate(["conv_dw_b", "conv_ln_g", "conv_ln_b", "conv_pw_b", "pool_b", "pool_scale"]):
        cv[:, k, :] = inp[name][i].reshape(2, 128).T
    cv[:64, 6, 0] = 1 / 2; cv[64:, 6, 0] = 1 / 4; cv[:64, 6, 1] = 1 / 8; cv[64:, 6, 1] = 1 / 16
    w["cvec"] = cv
    w["pw_w"] = np.ascontiguousarray(inp["conv_pw_w"][i].reshape(2, 128, 256).transpose(1, 0, 2))
    pl = np.zeros((128, 2, 128), np.float32)
    pw = inp["pool_w"][i]
    for g in range(4):
        cc, p0 = g // 2, (g % 2) * 64
        pl[p0:p0 + 64, cc, p0:p0 + 64] = pw[g]
    w["plw"] = pl
    w["ident"] = np.eye(128, dtype=np.float32)
    return w


def prep_A_core(core, h, NS, cs_tab):
    bt = core // 4
    hA = np.empty((NS * 128, 1024), np.float32)
    hH = np.zeros((NS * HALO, 1024), np.float32)
    hok = np.ones((128, NS), np.float32)
    cs = np.empty((NS * 128, 64), np.float32)
    rc0 = np.empty((128, 2, 128), np.float32)
    wins = [2, 4, 8, 16]
    for s in range(NS):
        qb = slot_qb(core, s)
        t0 = qb * 128
        hA[s * 128:(s + 1) * 128] = h[bt, t0:t0 + 128]
        cs[s * 128:(s + 1) * 128] = cs_tab[t0:t0 + 128]
        if qb == 0:
            hok[:, s] = 0.0
        else:
            hH[s * HALO:(s + 1) * HALO] = h[bt, t0 - HALO:t0]
    qb0 = slot_qb(core, 0)
    t = qb0 * 128 + np.arange(128)
    for g in range(4):
        cc, p0 = g // 2, (g % 2) * 64
        rc0[p0:p0 + 64, cc, :] = (1.0 / np.minimum(t + 1, wins[g]).astype(np.float32))[None, :]
    return {"hA": hA, "hH": hH, "hok": hok, "cs": cs, "rc0": rc0}


def prep_B_consts(core):
    j = core % 4
    pidx = np.arange(128, dtype=np.float32)
    qoff = np.stack([128.0 * j + pidx, 128.0 * (3 - j) + pidx], 1).astype(np.float32)
    iota = np.tile(np.arange(512, dtype=np.float32)[None, :], (128, 1))
    eps = 2.0 ** -30
    fl_l = np.zeros((3, 32, 128), np.float32)
    fl_l[0] = -eps * 16
    fl_l[1] = -eps
    fl_l[2] = (-eps * 512 * np.arange(32, dtype=np.float32))[:, None]
    kk = np.arange(512)
    fl_r = np.stack([kk // 16, kk % 16, np.ones(512)], 0).astype(np.float32)
    return {"qoff": qoff, "iota": iota, "fl_l": fl_l.astype(BF), "fl_r": fl_r.astype(BF), "ident": np.eye(128, dtype=np.float32)}


def prep_B_weights(i, inp):
    return {"w_out": np.ascontiguousarray(inp["w_out"][i]), "w_g": np.ascontiguousarray(inp["ple_gate_w"][i]),
            "w_p": np.ascontiguousarray(inp["ple_w"][i]), "gF": np.ascontiguousarray(inp["final_norm_g"][None, :])}

AP = bass.AP
NSLOT = 32
_CACHE = {}


def _progs():
    if "A" not in _CACHE:
        _CACHE["A"] = build_A(NSLOT)
        _CACHE["B"] = build_B(NSLOT)
    return _CACHE["A"], _CACHE["B"]


def kernel(**inputs):
    inp = {k: np.asarray(v) for k, v in inputs.items()}
    ncA, ncB = _progs()
    h = np.array(inp["x"], dtype=np.float32, copy=True)
    cs_tab = rope_table()
    cores = list(range(8))
    for i in range(4):
        wA = prep_A_weights(i, inp)
        mapsA = []
        for c in cores:
            m = dict(wA)
            m.update(prep_A_core(c, h, NSLOT, cs_tab))
            mapsA.append(m)
        rA = run_bass_kernel_spmd(ncA, mapsA, core_ids=cores).results
        KT = [np.empty((128, 4, S), BF) for _ in range(2)]
        VA = [np.empty((S // 128, 128, 528), BF) for _ in range(2)]
        KI = [np.empty((128, S), BF) for _ in range(2)]
        for c in cores:
            bt = c // 4
            kT_o, va_o, ki_o = np.asarray(rA[c]["kT_o"]), np.asarray(rA[c]["va_o"]), np.asarray(rA[c]["ki_o"])
            for s in range(NSLOT):
                qb = slot_qb(c, s)
                KT[bt][:, :, qb * 128:(qb + 1) * 128] = kT_o[s]
                VA[bt][qb] = va_o[s]
                KI[bt][0:64, qb * 128:(qb + 1) * 128] = ki_o[s]
                KI[bt][64:128, qb * 128:(qb + 1) * 128] = ki_o[s]
        wB = prep_B_weights(i, inp)
        mapsB = []
        for c in cores:
            bt = c // 4
            m = dict(wB)
            m.update(prep_B_consts(c))
            toks = np.concatenate([np.arange(128) + 128 * slot_qb(c, s) for s in range(NSLOT)])
            m["qT"] = np.asarray(rA[c]["qT_o"]); m["qiT"] = np.asarray(rA[c]["qi_o"]); m["sg"] = np.asarray(rA[c]["sg_o"])
            m["ag"] = np.asarray(rA[c]["ag_o"]); m["yab"] = np.asarray(rA[c]["yab_o"])
            m["hA"] = np.ascontiguousarray(h[bt][toks]); m["pA"] = np.ascontiguousarray(inp["p"][i, bt][toks])
            m["KT"] = KT[bt]; m["VA"] = VA[bt]; m["KI"] = KI[bt]
            m["fsel"] = np.full((128, 1), 1.0 if i == 3 else 0.0, np.float32)
            mapsB.append(m)
        rB = run_bass_kernel_spmd(ncB, mapsB, core_ids=cores).results
        for c in cores:
            bt = c // 4
            toks = np.concatenate([np.arange(128) + 128 * slot_qb(c, s) for s in range(NSLOT)])
            h[bt][toks] = np.asarray(rB[c]["h_o"])
    return h.astype(np.float32)
```

```python
from contextlib import ExitStack
import numpy as np
import ml_dtypes
import concourse.bass as bass
import concourse.mybir as mybir
from concourse.bass_utils import run_bass_kernel_spmd


F32 = mybir.dt.float32
BF16 = mybir.dt.bfloat16
ALU = mybir.AluOpType
AF = mybir.ActivationFunctionType
AX = mybir.AxisListType


class Res:
    __slots__ = ("w", "r")

    def __init__(self):
        self.w = None
        self.r = {}


class Prog:
    ENGS = ("pe", "act", "dve", "pool", "sp")
    NDSEM = 24

    def __init__(self, nc, stack):
        self.nc = nc
        self.q = {e: [] for e in self.ENGS}
        self.cnt = {e: 0 for e in self.ENGS}
        self.sems = {}
        for e in ("pe", "act", "dve", "pool"):
            self.sems[e] = stack.enter_context(nc.semaphore("s_" + e))
        self.dsem = {}
        self.dcnt = {}
        self.drr = {}
        for qn in ("sp", "pool"):
            for i in range(self.NDSEM):
                k = "d_%s_%d" % (qn, i)
                self.sems[k] = stack.enter_context(nc.semaphore(k))
                self.dcnt[k] = 0
            self.drr[qn] = 0
        self.stack = stack

    def sb(self, name, shape, dt):
        h = self.stack.enter_context(self.nc.sbuf_tensor(name, list(shape), dt))
        return h

    def ps(self, name, shape, dt):
        h = self.stack.enter_context(self.nc.psum_tensor(name, list(shape), dt))
        return h

    def _deps(self, eng, reads, writes):
        deps = {}

        def add(tok):
            if tok is None:
                return
            k, v = tok
            if eng == "pe" and k == "pe":
                return
            if deps.get(k, 0) < v:
                deps[k] = v

        for r in reads:
            add(r.w)
        for w in writes:
            add(w.w)
            for k, v in w.r.items():
                add((k, v))
        return deps

    def _commit(self, tok, reads, writes):
        k, v = tok
        for r in reads:
            if r.r.get(k, 0) < v:
                r.r[k] = v
        for w in writes:
            w.w = tok
            w.r = {}

    @staticmethod
    def _flat(xs):
        out = []
        for x in xs:
            if isinstance(x, (list, tuple)):
                out.extend(Prog._flat(x))
            else:
                out.append(x)
        return out

    def op(self, eng, fn, reads=(), writes=()):
        reads = self._flat(reads); writes = self._flat(writes)
        deps = self._deps(eng, reads, writes)
        self.cnt[eng] += 1
        tok = (eng, self.cnt[eng])
        self.q[eng].append((deps, fn, tok, 1))
        self._commit(tok, reads, writes)
        return tok

    def dma(self, qn, out, in_, reads=(), writes=(), **kw):
        reads = self._flat(reads); writes = self._flat(writes)
        deps = self._deps(qn, reads, writes)
        i = self.drr[qn]
        self.drr[qn] = (i + 1) % self.NDSEM
        k = "d_%s_%d" % (qn, i)
        if self.dcnt[k] > 0:
            if deps.get(k, 0) < self.dcnt[k]:
                deps[k] = self.dcnt[k]
        self.dcnt[k] += 16
        tok = (k, self.dcnt[k])
        if qn == "pool":
            self.cnt["pool"] += 0
        self.q[qn].append((deps, lambda e: e.dma_start(out=out, in_=in_, **kw), tok, 16))
        self._commit(tok, reads, writes)
        return tok

    def emit(self, final_waits=None):
        nc = self.nc
        engobj = {"pe": "tensor", "act": "scalar", "dve": "vector", "pool": "gpsimd", "sp": "sync"}
        ce = ("pe", "act", "dve", "pool")
        needed = {e: set() for e in ce}
        for en in self.ENGS:
            known = {}
            for deps, fn, tok, inc in self.q[en]:
                for k, v in deps.items():
                    if known.get(k, 0) < v:
                        known[k] = v
                        if k in needed:
                            needed[k].add(v)
            if final_waits and en in final_waits:
                for k, v in final_waits[en].items():
                    if k in needed and known.get(k, 0) < v:
                        needed[k].add(v)
        rank = {e: {v: i + 1 for i, v in enumerate(sorted(needed[e]))} for e in ce}
        with nc.Block() as block:
            for en in self.ENGS:
                q = self.q[en]

                def body(e, q=q, en=en):
                    known = {}

                    def wait(k, v):
                        if known.get(k, 0) < v:
                            known[k] = v
                            e.wait_ge(self.sems[k], rank[k][v] if k in rank else v)

                    for deps, fn, tok, inc in q:
                        for k, v in deps.items():
                            wait(k, v)
                        ins = fn(e)
                        if tok[0] in rank:
                            if tok[1] in rank[tok[0]]:
                                ins.then_inc(self.sems[tok[0]], 1)
                        else:
                            ins.then_inc(self.sems[tok[0]], inc)
                    if final_waits and en in final_waits:
                        for k, v in final_waits[en].items():
                            wait(k, v)

                getattr(block, engobj[en])(body)

    def all_dma_tokens(self):
        return {k: v for k, v in self.dcnt.items() if v > 0}


D = 1024
DIN = 3652
EPS = 1e-6
HALO = 32
NT = 128 + HALO


def bc_mid(ap2d, n):
    a = ap2d.ap
    return AP(ap2d.tensor, ap2d.offset, [list(a[0]), [0, n], list(a[1])])


def bc_last(ap2d, n):
    a = ap2d.ap
    return AP(ap2d.tensor, ap2d.offset, [list(a[0]), list(a[1]), [0, n]])


def build_A(NS, stage=99):
    nc = bass.Bass("TRN2", target_bir_lowering=False)
    dr = lambda n, s, d, k="ExternalInput": nc.dram_tensor(n, list(s), d, kind=k).ap()
    hA = dr("hA", [NS * 128, D], F32)
    hH = dr("hH", [NS * HALO, D], F32)
    hok = dr("hok", [128, NS], F32)
    cs = dr("cs", [NS * 128, 64], F32)
    w_in = dr("w_in", [D, DIN], F32)
    b_bc = dr("b_bc", [1, DIN], F32)
    b_fm = dr("b_fm", [128, 10], F32)
    g_bc = dr("g_bc", [1, D], F32)
    wdw = dr("wdw", [128, 2, 31], F32)
    cvec = dr("cvec", [128, 7, 2], F32)
    pw_w = dr("pw_w", [128, 2, 256], F32)
    plw = dr("plw", [128, 2, 128], F32)
    rc0 = dr("rc0", [128, 2, 128], F32)
    ident_d = dr("ident", [128, 128], F32)
    O = "ExternalOutput"
    kT_o = dr("kT_o", [NS, 128, 4, 128], BF16, O)
    va_o = dr("va_o", [NS, 128, 528], BF16, O)
    ki_o = dr("ki_o", [NS, 64, 128], BF16, O)
    qT_o = dr("qT_o", [NS, 128, 4, 128], BF16, O)
    qi_o = dr("qi_o", [NS, 128, 2, 128], BF16, O)
    sg_o = dr("sg_o", [NS, 128, 4], F32, O)
    ag_o = dr("ag_o", [NS, 128, 512], BF16, O)
    yab_o = dr("yab_o", [NS, 128, 4, 128], BF16, O)

    with ExitStack() as st:
        P = Prog(nc, st)
        sb, ps = P.sb, P.ps
        Wb = sb("Wb", [128, 8, DIN], BF16); rWb = Res()
        bbc = sb("bbc", [128, DIN], F32); rbbc = Res()
        bfm = sb("bfm", [128, 10], F32); rbfm = Res()
        gbc = sb("gbc", [128, D], F32); rgbc = Res()
        wdw_t = sb("wdw_t", [128, 2, 31], F32); rwdw = Res()
        cv = sb("cv", [128, 7, 2], F32); rcv = Res()
        pwb = sb("pwb", [128, 2, 256], BF16); rpwb = Res()
        plb = sb("plb", [128, 2, 128], BF16); rplb = Res()
        rc0_t = sb("rc0_t", [128, 2, 128], F32); rrc0 = Res()
        hok_t = sb("hok_t", [128, NS], F32); rhok = Res()
        idf = sb("idf", [128, 128], F32); ridf = Res()
        idb = sb("idb", [128, 128], BF16); ridb = Res()
        onesf = sb("onesf", [128, 128], F32); rones = Res()
        stg = [sb("stg%d" % i, [128, 1024], F32) for i in range(2)]; rstg = [Res(), Res()]

        P.dma("sp", bbc[:], b_bc.partition_broadcast(128), writes=[rbbc])
        P.dma("sp", gbc[:], g_bc.partition_broadcast(128), writes=[rgbc])
        P.dma("sp", bfm[:], b_fm, writes=[rbfm])
        P.dma("sp", wdw_t[:], wdw, writes=[rwdw])
        P.dma("sp", cv[:], cvec, writes=[rcv])
        P.dma("sp", rc0_t[:], rc0, writes=[rrc0])
        P.dma("sp", hok_t[:], hok, writes=[rhok])
        P.dma("sp", idf[:], ident_d, writes=[ridf])
        P.op("dve", lambda e: e.tensor_copy(out=idb[:], in_=idf[:]), reads=[ridf], writes=[ridb])
        P.op("pool", lambda e: e.memset(onesf[:], 1.0 / 256.0), writes=[rones])
        i = 0
        P.dma("sp", stg[i][:, 0:512], pw_w.rearrange("p a b -> p (a b)"), writes=[rstg[i]])
        P.op("dve", lambda e: e.tensor_copy(out=pwb[:].rearrange("p a b -> p (a b)"), in_=stg[0][:, 0:512]),
             reads=[rstg[0]], writes=[rpwb])
        P.dma("sp", stg[1][:, 0:256], plw.rearrange("p a b -> p (a b)"), writes=[rstg[1]])
        P.op("dve", lambda e: e.tensor_copy(out=plb[:].rearrange("p a b -> p (a b)"), in_=stg[1][:, 0:256]),
             reads=[rstg[1]], writes=[rplb])
        ci = 0
        for kc in range(8):
            for c0 in range(0, DIN, 1024):
                cw = min(1024, DIN - c0)
                i = ci % 2
                P.dma("sp", stg[i][:, 0:cw], w_in[kc * 128:(kc + 1) * 128, c0:c0 + cw], writes=[rstg[i]])
                eng = ("dve", "act", "pool")[ci % 3]
                if eng == "act":
                    P.op("act", lambda e, i=i, kc=kc, c0=c0, cw=cw: e.copy(out=Wb[:, kc, c0:c0 + cw], in_=stg[i][:, 0:cw]),
                         reads=[rstg[i]], writes=[rWb])
                else:
                    P.op(eng, lambda e, i=i, kc=kc, c0=c0, cw=cw: e.tensor_copy(out=Wb[:, kc, c0:c0 + cw], in_=stg[i][:, 0:cw]),
                         reads=[rstg[i]], writes=[rWb])
                ci += 1

        def dbl(name, shape, dt):
            return [(sb("%s%d" % (name, i), shape, dt), Res()) for i in range(2)]

        hblk = dbl("hblk", [128, D], F32)
        hhal = dbl("hhal", [HALO, D], F32)
        cst = dbl("cst", [128, 64], F32)
        sq = dbl("sq", [128, D], F32)
        sqh = dbl("sqh", [HALO, D], F32)
        st1 = dbl("st1", [128, 4], F32)
        st1h = dbl("st1h", [HALO, 4], F32)
        hn = dbl("hn", [128, D], BF16)
        hnh = dbl("hnh", [HALO, D], BF16)
        nT = dbl("nT", [128, 8, NT], BF16)
        val = dbl("val", [128, 2, NT], F32)
        sgl = dbl("sgl", [128, 2, NT], F32)
        u = dbl("u", [128, 2, NT], F32)
        pin = dbl("pin", [128, 2, NT], F32)
        cgate = dbl("cgate", [128, 2, 128], F32)
        pgate = dbl("pgate", [128, 2, 128], F32)
        cacc = dbl("cacc", [128, 2, 128], F32)
        xc = dbl("xc", [128, 2, 128], F32)
        xsq = dbl("xsq", [128, 2, 128], F32)
        rstd_b = dbl("rstd_b", [128, 128], F32)
        yn = dbl("yn", [128, 2, 128], F32)
        sact = dbl("sact", [128, 2, 128], BF16)
        yab = dbl("yab", [128, 4, 128], BF16)
        ps2 = dbl("ps2", [128, 2, NT], F32)
        ps4 = dbl("ps4", [128, 2, NT], F32)
        ps8 = dbl("ps8", [128, NT], F32)
        ps16 = dbl("ps16", [128, NT], F32)
        dpl = dbl("dpl", [128, 2, 128], BF16)
        ptmp = dbl("ptmp", [128, 2, 128], F32)
        ztm = dbl("ztm", [128, 512], F32)
        zt2 = dbl("zt2", [128, 512], F32)
        rt1 = dbl("rt1", [128, 256], F32)
        rt2 = dbl("rt2", [128, 256], F32)
        rot = dbl("rot", [128, 512], BF16)
        qT = dbl("qT", [128, 4, 128], BF16)
        kT = dbl("kT", [128, 4, 128], BF16)
        va = dbl("va", [128, 8, 66], BF16)
        ag1 = dbl("ag1", [128, 512], F32)
        ag = dbl("ag", [128, 512], BF16)
        zi = dbl("zi", [128, 324], F32)
        wia = dbl("wia", [128, 8], F32)
        qik = dbl("qik", [128, 384], BF16)
        qikT = dbl("qikT", [128, 3, 128], BF16)
        for i in range(2):
            P.op("pool", lambda e, i=i: e.memset(qik[i][0][:], 0.0), writes=[qik[i][1]])
        for i in range(2):
            P.op("pool", lambda e, i=i: e.memset(va[i][0][:], 1.0), writes=[va[i][1]])

        pb = [(ps("pb%d" % i, [128, 512], F32), Res()) for i in range(6)]
        pt = [(ps("pt%d" % i, [128, 1024], BF16), Res()) for i in range(2)]
        pbi = [0]
        pti = [0]

        def nextpb():
            t = pb[pbi[0] % len(pb)]; pbi[0] += 1; return t

        def nextpt():
            t = pt[pti[0] % len(pt)]; pti[0] += 1; return t

        FM_CHUNKS = [
            (0, "val"), (128, "val"), (256, "glu"), (384, "glu"), (512, "cgate"), (640, "cgate"),
            (768, "pin"), (896, "pin"), (1024, "pgate"), (1152, "pgate")]

        def do_slot(s):
            b = s % 2
            P.dma("sp", hblk[b][0][:], hA[s * 128:(s + 1) * 128, :], writes=[hblk[b][1]])
            P.dma("sp", hhal[b][0][:], hH[s * HALO:(s + 1) * HALO, :], writes=[hhal[b][1]])
            P.dma("sp", cst[b][0][:], cs[s * 128:(s + 1) * 128, :], writes=[cst[b][1]])
            for (h_, sq_, st_, hn_, np_) in ((hblk[b], sq[b], st1[b], hn[b], 128), (hhal[b], sqh[b], st1h[b], hnh[b], HALO)):
                P.op("act", lambda e, h_=h_, sq_=sq_: e.activation(out=sq_[0][:], in_=h_[0][:], func=AF.Square),
                     reads=[h_[1]], writes=[sq_[1]])
                P.op("dve", lambda e, sq_=sq_, st_=st_: e.tensor_reduce(out=st_[0][:, 0:1], in_=sq_[0][:], axis=AX.X, op=ALU.add),
                     reads=[sq_[1]], writes=[st_[1]])
                P.op("dve", lambda e, st_=st_: e.tensor_scalar(out=st_[0][:, 1:2], in0=st_[0][:, 0:1], scalar1=1.0 / D, scalar2=EPS,
                                                               op0=ALU.mult, op1=ALU.add), reads=[st_[1]], writes=[st_[1]])
                P.op("act", lambda e, st_=st_: e.activation(out=st_[0][:, 2:3], in_=st_[0][:, 1:2], func=AF.Sqrt),
                     reads=[st_[1]], writes=[st_[1]])
                P.op("dve", lambda e, st_=st_: e.reciprocal(out=st_[0][:, 3:4], in_=st_[0][:, 2:3]), reads=[st_[1]], writes=[st_[1]])
                P.op("dve", lambda e, h_=h_, st_=st_, hn_=hn_, np_=np_: e.scalar_tensor_tensor(
                    out=hn_[0][:], in0=h_[0][:], scalar=st_[0][:, 3:4], in1=gbc[0:np_, :], op0=ALU.mult, op1=ALU.mult),
                    reads=[h_[1], st_[1], rgbc], writes=[hn_[1]])
            if stage < 2:
                return
            ptt = nextpt()
            for c in range(8):
                P.op("pe", lambda e, c=c, ptt=ptt: e.transpose(out=ptt[0][:, c * 128:(c + 1) * 128], in_=hn[b][0][:, c * 128:(c + 1) * 128],
                                                              identity=idb[:]),
                     reads=[hn[b][1], ridb], writes=[ptt[1]])
            P.op("act", lambda e, ptt=ptt: e.copy(out=nT[b][0][:, :, HALO:NT], in_=ptt[0][:, :].rearrange("p (c t) -> p c t", c=8)),
                 reads=[ptt[1]], writes=[nT[b][1]])
            ptt = nextpt()
            for c in range(8):
                P.op("pe", lambda e, c=c, ptt=ptt: e.transpose(out=ptt[0][:, c * HALO:(c + 1) * HALO], in_=hnh[b][0][:, c * 128:(c + 1) * 128],
                                                              identity=idb[0:HALO, 0:HALO]),
                     reads=[hnh[b][1], ridb], writes=[ptt[1]])
            P.op("dve", lambda e, ptt=ptt: e.tensor_copy(out=nT[b][0][:, :, 0:HALO],
                                                         in_=ptt[0][:, 0:8 * HALO].rearrange("p (c t) -> p c t", c=8)),
                 reads=[ptt[1]], writes=[nT[b][1]])
            if stage < 3:
                return
            for ci_, (c0, kind) in enumerate(FM_CHUNKS):
                cc = ci_ % 2
                nt0 = 0 if kind in ("val", "glu", "pin") else HALO
                nw = NT - nt0
                pz = nextpb()
                for kc in range(8):
                    P.op("pe", lambda e, kc=kc, pz=pz, c0=c0, nt0=nt0, nw=nw: e.matmul(
                        pz[0][:, 0:nw], lhsT=Wb[:, kc, c0:c0 + 128], rhs=nT[b][0][:, kc, nt0:NT], start=(kc == 0), stop=(kc == 7)),
                        reads=[rWb, nT[b][1]], writes=[pz[1]])
                bias = bfm[:, ci_:ci_ + 1]
                if kind == "val":
                    P.op("act", lambda e, pz=pz, cc=cc, bias=bias: e.activation(out=val[b][0][:, cc, :], in_=pz[0][:, 0:NT], func=AF.Identity, bias=bias),
                         reads=[pz[1], rbfm], writes=[val[b][1]])
                elif kind == "glu":
                    P.op("act", lambda e, pz=pz, cc=cc, bias=bias: e.activation(out=sgl[b][0][:, cc, :], in_=pz[0][:, 0:NT], func=AF.Sigmoid, bias=bias),
                         reads=[pz[1], rbfm], writes=[sgl[b][1]])
                elif kind == "pin":
                    P.op("act", lambda e, pz=pz, cc=cc, bias=bias: e.activation(out=pin[b][0][:, cc, :], in_=pz[0][:, 0:NT], func=AF.Identity, bias=bias),
                         reads=[pz[1], rbfm], writes=[pin[b][1]])
                elif kind == "cgate":
                    P.op("act", lambda e, pz=pz, cc=cc, bias=bias: e.activation(out=cgate[b][0][:, cc, :], in_=pz[0][:, 0:128], func=AF.Silu, bias=bias),
                         reads=[pz[1], rbfm], writes=[cgate[b][1]])
                else:
                    P.op("act", lambda e, pz=pz, cc=cc, bias=bias: e.activation(out=pgate[b][0][:, cc, :], in_=pz[0][:, 0:128], func=AF.Silu, bias=bias),
                         reads=[pz[1], rbfm], writes=[pgate[b][1]])
            if stage < 4:
                return
            P.op("dve", lambda e: e.tensor_tensor(out=u[b][0][:], in0=val[b][0][:], in1=sgl[b][0][:], op=ALU.mult),
                 reads=[val[b][1], sgl[b][1]], writes=[u[b][1]])
            P.op("dve", lambda e: e.tensor_scalar(out=u[b][0][:, :, 0:HALO], in0=u[b][0][:, :, 0:HALO], scalar1=hok_t[:, s:s + 1], scalar2=None,
                                                  op0=ALU.mult), reads=[u[b][1], rhok], writes=[u[b][1]])
            P.op("pool", lambda e: e.tensor_scalar(out=pin[b][0][:, :, 0:HALO], in0=pin[b][0][:, :, 0:HALO], scalar1=hok_t[:, s:s + 1], scalar2=None,
                                                   op0=ALU.mult), reads=[pin[b][1], rhok], writes=[pin[b][1]])
            for cc in range(2):
                eng = "dve"
                for k in range(31):
                    o0 = HALO - 30 + k
                    if k == 0:
                        P.op(eng, lambda e, cc=cc, o0=o0, k=k: e.tensor_scalar(
                            out=cacc[b][0][:, cc, :], in0=u[b][0][:, cc, o0:o0 + 128], scalar1=wdw_t[:, cc, k:k + 1], scalar2=cv[:, 0, cc:cc + 1],
                            op0=ALU.mult, op1=ALU.add), reads=[u[b][1], rwdw, rcv], writes=[cacc[b][1]])
                    else:
                        P.op(eng, lambda e, cc=cc, o0=o0, k=k: e.scalar_tensor_tensor(
                            out=cacc[b][0][:, cc, :], in0=u[b][0][:, cc, o0:o0 + 128], scalar=wdw_t[:, cc, k:k + 1], in1=cacc[b][0][:, cc, :],
                            op0=ALU.mult, op1=ALU.add), reads=[u[b][1], rwdw, cacc[b][1]], writes=[cacc[b][1]])
            pm = nextpb()
            for cc in range(2):
                P.op("pe", lambda e, cc=cc, pm=pm: e.matmul(pm[0][:, 0:128], lhsT=onesf[:], rhs=cacc[b][0][:, cc, :], start=(cc == 0), stop=(cc == 1)),
                     reads=[rones, cacc[b][1]], writes=[pm[1]])
            P.op("dve", lambda e, pm=pm: e.tensor_tensor(out=xc[b][0][:], in0=cacc[b][0][:], in1=bc_mid(pm[0][:, 0:128], 2), op=ALU.subtract),
                 reads=[cacc[b][1], pm[1]], writes=[xc[b][1]])
            P.op("act", lambda e: e.activation(out=xsq[b][0][:], in_=xc[b][0][:], func=AF.Square), reads=[xc[b][1]], writes=[xsq[b][1]])
            pv = nextpb()
            for cc in range(2):
                P.op("pe", lambda e, cc=cc, pv=pv: e.matmul(pv[0][:, 0:128], lhsT=onesf[:], rhs=xsq[b][0][:, cc, :], start=(cc == 0), stop=(cc == 1)),
                     reads=[rones, xsq[b][1]], writes=[pv[1]])
            P.op("act", lambda e, pv=pv: e.activation(out=rstd_b[b][0][:], in_=pv[0][:, 0:128], func=AF.Sqrt, bias=EPS),
                 reads=[pv[1]], writes=[rstd_b[b][1]])
            P.op("dve", lambda e: e.reciprocal(out=rstd_b[b][0][:], in_=rstd_b[b][0][:]), reads=[rstd_b[b][1]], writes=[rstd_b[b][1]])
            P.op("dve", lambda e: e.tensor_tensor(out=yn[b][0][:], in0=xc[b][0][:], in1=bc_mid(rstd_b[b][0][:], 2), op=ALU.mult),
                 reads=[xc[b][1], rstd_b[b][1]], writes=[yn[b][1]])
            for cc in range(2):
                P.op("act", lambda e, cc=cc: e.activation(out=sact[b][0][:, cc, :], in_=yn[b][0][:, cc, :], func=AF.Silu,
                                                          scale=cv[:, 1, cc:cc + 1], bias=cv[:, 2, cc:cc + 1]),
                     reads=[yn[b][1], rcv], writes=[sact[b][1]])
            for oc in range(2):
                pz = nextpb()
                for kc in range(2):
                    P.op("pe", lambda e, oc=oc, kc=kc, pz=pz: e.matmul(pz[0][:, 0:128], lhsT=pwb[:, kc, oc * 128:(oc + 1) * 128], rhs=sact[b][0][:, kc, :],
                                                                       start=(kc == 0), stop=(kc == 1)),
                         reads=[rpwb, sact[b][1]], writes=[pz[1]])
                P.op("dve", lambda e, oc=oc, pz=pz: e.scalar_tensor_tensor(out=yab[b][0][:, oc, :], in0=pz[0][:, 0:128], scalar=cv[:, 3, oc:oc + 1],
                                                                           in1=cgate[b][0][:, oc, :], op0=ALU.add, op1=ALU.mult),
                     reads=[pz[1], rcv, cgate[b][1]], writes=[yab[b][1]])
            if stage < 5:
                return
            pe_ = "pool"
            x_ = pin[b]
            P.op(pe_, lambda e: e.tensor_tensor(out=ps2[b][0][:, :, 1:NT], in0=x_[0][:, :, 1:NT], in1=x_[0][:, :, 0:NT - 1], op=ALU.add),
                 reads=[x_[1]], writes=[ps2[b][1]])
            P.op(pe_, lambda e: e.tensor_tensor(out=ps4[b][0][:, :, 3:NT], in0=ps2[b][0][:, :, 3:NT], in1=ps2[b][0][:, :, 1:NT - 2], op=ALU.add),
                 reads=[ps2[b][1]], writes=[ps4[b][1]])
            P.op(pe_, lambda e: e.tensor_tensor(out=ps8[b][0][:, 7:NT], in0=ps4[b][0][:, 1, 7:NT], in1=ps4[b][0][:, 1, 3:NT - 4], op=ALU.add),
                 reads=[ps4[b][1]], writes=[ps8[b][1]])
            P.op(pe_, lambda e: e.tensor_tensor(out=ps16[b][0][:, 15:NT], in0=ps8[b][0][:, 15:NT], in1=ps8[b][0][:, 7:NT - 8], op=ALU.add),
                 reads=[ps8[b][1]], writes=[ps16[b][1]])
            srcs = [(ps2[b], lambda t: t[0][0:64, 0, HALO:NT], 0, 0), (ps4[b], lambda t: t[0][64:128, 0, HALO:NT], 64, 0),
                    (ps8[b], lambda t: t[0][0:64, HALO:NT], 0, 1), (ps16[b], lambda t: t[0][64:128, HALO:NT], 64, 1)]
            for (src, view, p0, cc) in srcs:
                if s == 0:
                    P.op(pe_, lambda e, src=src, view=view, p0=p0, cc=cc: e.tensor_tensor(
                        out=ptmp[b][0][p0:p0 + 64, cc, :], in0=view(src), in1=rc0_t[p0:p0 + 64, cc, :], op=ALU.mult),
                        reads=[src[1], rrc0], writes=[ptmp[b][1]])
                else:
                    P.op(pe_, lambda e, src=src, view=view, p0=p0, cc=cc: e.tensor_scalar(
                        out=ptmp[b][0][p0:p0 + 64, cc, :], in0=view(src), scalar1=cv[p0:p0 + 64, 6, cc:cc + 1], scalar2=None, op0=ALU.mult),
                        reads=[src[1], rcv], writes=[ptmp[b][1]])
            P.op(pe_, lambda e: e.tensor_tensor(out=dpl[b][0][:], in0=ptmp[b][0][:], in1=x_[0][:, :, HALO:NT], op=ALU.subtract),
                 reads=[ptmp[b][1], x_[1]], writes=[dpl[b][1]])
            for cc in range(2):
                pz = nextpb()
                P.op("pe", lambda e, cc=cc, pz=pz: e.matmul(pz[0][:, 0:128], lhsT=plb[:, cc, :], rhs=dpl[b][0][:, cc, :], start=True, stop=True),
                     reads=[rplb, dpl[b][1]], writes=[pz[1]])
                P.op("dve", lambda e, cc=cc, pz=pz: e.tensor_scalar(out=ptmp[b][0][:, cc, :], in0=pz[0][:, 0:128], scalar1=cv[:, 4, cc:cc + 1],
                                                                    scalar2=cv[:, 5, cc:cc + 1], op0=ALU.add, op1=ALU.mult),
                     reads=[pz[1], rcv], writes=[ptmp[b][1]])
            P.op("pool", lambda e: e.tensor_tensor(out=yab[b][0][:, 2:4, :], in0=ptmp[b][0][:], in1=pgate[b][0][:], op=ALU.mult),
                 reads=[ptmp[b][1], pgate[b][1]], writes=[yab[b][1]])
            P.dma("pool", yab_o[s], yab[b][0][:], reads=[yab[b][1]])

            if stage < 6:
                return
            def tm_group(c0, cw, dst):
                pz = nextpb()
                for kc in range(8):
                    P.op("pe", lambda e, kc=kc, pz=pz: e.matmul(pz[0][:, 0:cw], lhsT=nT[b][0][:, kc, HALO:NT], rhs=Wb[:, kc, c0:c0 + cw],
                                                                start=(kc == 0), stop=(kc == 7)),
                         reads=[rWb, nT[b][1]], writes=[pz[1]])
                P.op("dve", lambda e, pz=pz: e.tensor_tensor(out=dst[0][:, 0:cw], in0=pz[0][:, 0:cw], in1=bbc[:, c0:c0 + cw], op=ALU.add),
                     reads=[pz[1], rbbc], writes=[dst[1]])

            def rope(src, nh, dst, eng="pool"):
                xv = src[0].rearrange("p (h t d) -> p h t d", h=nh, t=2)
                ov = dst[0].rearrange("p (h t d) -> p h t d", h=nh, t=2)
                cosb = bc_mid(cst[b][0][:, 0:32], nh)
                sinb = bc_mid(cst[b][0][:, 32:64], nh)
                t1 = rt1[b][0][:, 0:nh * 32].rearrange("p (h d) -> p h d", h=nh)
                t2 = rt2[b][0][:, 0:nh * 32].rearrange("p (h d) -> p h d", h=nh)
                rd = [src[1], cst[b][1]]
                if nh == 1:
                    x0, x1 = src[0][:, 0:32], src[0][:, 32:64]
                    o0_, o1_ = dst[0][:, 0:32], dst[0][:, 32:64]
                    cb, sn = cst[b][0][:, 0:32], cst[b][0][:, 32:64]
                    a1, a2 = rt1[b][0][:, 0:32], rt2[b][0][:, 0:32]
                    P.op(eng, lambda e: e.tensor_tensor(out=a1, in0=x0, in1=cb, op=ALU.mult), reads=rd, writes=[rt1[b][1]])
                    P.op(eng, lambda e: e.tensor_tensor(out=a2, in0=x1, in1=sn, op=ALU.mult), reads=rd, writes=[rt2[b][1]])
                    P.op(eng, lambda e: e.tensor_tensor(out=o0_, in0=a1, in1=a2, op=ALU.subtract), reads=[rt1[b][1], rt2[b][1]], writes=[dst[1]])
                    P.op(eng, lambda e: e.tensor_tensor(out=a1, in0=x1, in1=cb, op=ALU.mult), reads=rd, writes=[rt1[b][1]])
                    P.op(eng, lambda e: e.tensor_tensor(out=a2, in0=x0, in1=sn, op=ALU.mult), reads=rd, writes=[rt2[b][1]])
                    P.op(eng, lambda e: e.tensor_tensor(out=o1_, in0=a1, in1=a2, op=ALU.add), reads=[rt1[b][1], rt2[b][1]], writes=[dst[1]])
                    return
                P.op(eng, lambda e: e.tensor_tensor(out=t1, in0=xv[:, :, 0, :], in1=cosb, op=ALU.mult), reads=rd, writes=[rt1[b][1]])
                P.op(eng, lambda e: e.tensor_tensor(out=t2, in0=xv[:, :, 1, :], in1=sinb, op=ALU.mult), reads=rd, writes=[rt2[b][1]])
                P.op(eng, lambda e: e.tensor_tensor(out=ov[:, :, 0, :], in0=t1, in1=t2, op=ALU.subtract), reads=[rt1[b][1], rt2[b][1]], writes=[dst[1]])
                P.op(eng, lambda e: e.tensor_tensor(out=t1, in0=xv[:, :, 1, :], in1=cosb, op=ALU.mult), reads=rd, writes=[rt1[b][1]])
                P.op(eng, lambda e: e.tensor_tensor(out=t2, in0=xv[:, :, 0, :], in1=sinb, op=ALU.mult), reads=rd, writes=[rt2[b][1]])
                P.op(eng, lambda e: e.tensor_tensor(out=ov[:, :, 1, :], in0=t1, in1=t2, op=ALU.add), reads=[rt1[b][1], rt2[b][1]], writes=[dst[1]])

            tm_group(1280, 512, ztm[b])
            rope((ztm[b][0][:, 0:512], ztm[b][1]), 8, (rot[b][0][:, 0:512], rot[b][1]), eng="pool")
            ptt = nextpt()
            for hp in range(4):
                P.op("pe", lambda e, hp=hp, ptt=ptt: e.transpose(out=ptt[0][:, hp * 128:(hp + 1) * 128], in_=rot[b][0][:, hp * 128:(hp + 1) * 128], identity=idb[:]),
                     reads=[rot[b][1], ridb], writes=[ptt[1]])
            P.op("act", lambda e, ptt=ptt: e.copy(out=qT[b][0][:].rearrange("p a b -> p (a b)"), in_=ptt[0][:, 0:512]), reads=[ptt[1]], writes=[qT[b][1]])
            P.dma("pool", qT_o[s], qT[b][0][:], reads=[qT[b][1]])
            if stage < 7:
                return
            tm_group(1792, 512, zt2[b])
            rope((zt2[b][0][:, 0:512], zt2[b][1]), 8, (rot[b][0][:, 0:512], rot[b][1]), eng="dve")
            ptt = nextpt()
            for hp in range(4):
                P.op("pe", lambda e, hp=hp, ptt=ptt: e.transpose(out=ptt[0][:, hp * 128:(hp + 1) * 128], in_=rot[b][0][:, hp * 128:(hp + 1) * 128], identity=idb[:]),
                     reads=[rot[b][1], ridb], writes=[ptt[1]])
            P.op("act", lambda e, ptt=ptt: e.copy(out=kT[b][0][:].rearrange("p a b -> p (a b)"), in_=ptt[0][:, 0:512]), reads=[ptt[1]], writes=[kT[b][1]])
            P.dma("pool", kT_o[s], kT[b][0][:], reads=[kT[b][1]])
            if stage < 8:
                return
            pz = nextpb()
            for kc in range(8):
                P.op("pe", lambda e, kc=kc, pz=pz: e.matmul(pz[0][:, 0:512], lhsT=nT[b][0][:, kc, HALO:NT], rhs=Wb[:, kc, 2304:2816],
                                                            start=(kc == 0), stop=(kc == 7)), reads=[rWb, nT[b][1]], writes=[pz[1]])
            P.op("dve", lambda e, pz=pz: e.tensor_tensor(out=va[b][0][:, :, 0:64], in0=pz[0][:, 0:512].rearrange("p (h d) -> p h d", h=8),
                                                         in1=bbc[:, 2304:2816].rearrange("p (h d) -> p h d", h=8), op=ALU.add),
                 reads=[pz[1], rbbc], writes=[va[b][1]])
            P.dma("pool", va_o[s], va[b][0][:].rearrange("p h d -> p (h d)"), reads=[va[b][1]])
            if stage < 9:
                return
            tm_group(2816, 512, ag1[b])
            P.op("act", lambda e: e.activation(out=ag[b][0][:], in_=ag1[b][0][:], func=AF.Silu), reads=[ag1[b][1]], writes=[ag[b][1]])
            P.dma("pool", ag_o[s], ag[b][0][:], reads=[ag[b][1]])
            if stage < 10:
                return
            tm_group(3328, 324, zi[b])
            P.op("act", lambda e: e.activation(out=wia[b][0][:, 0:4], in_=zi[b][0][:, 320:324], func=AF.Abs, scale=0.125),
                 reads=[zi[b][1]], writes=[wia[b][1]])
            P.op("act", lambda e: e.activation(out=wia[b][0][:, 4:8], in_=zi[b][0][:, 320:324], func=AF.Sign), reads=[zi[b][1]], writes=[wia[b][1]])
            P.dma("pool", sg_o[s], wia[b][0][:, 4:8], reads=[wia[b][1]])
            if stage < 11:
                return
            for hh in range(4):
                P.op("dve", lambda e, hh=hh: e.tensor_scalar(out=zi[b][0][:, hh * 64:(hh + 1) * 64], in0=zi[b][0][:, hh * 64:(hh + 1) * 64],
                                                             scalar1=wia[b][0][:, hh:hh + 1], scalar2=None, op0=ALU.mult),
                     reads=[zi[b][1], wia[b][1]], writes=[zi[b][1]])
            rope((zi[b][0][:, 0:256], zi[b][1]), 4, (qik[b][0][:, 0:256], qik[b][1]), eng="pool")
            rope((zi[b][0][:, 256:320], zi[b][1]), 1, (qik[b][0][:, 256:320], qik[b][1]), eng="dve")
            ptt = nextpt()
            for hp in range(3):
                P.op("pe", lambda e, hp=hp, ptt=ptt: e.transpose(out=ptt[0][:, hp * 128:(hp + 1) * 128], in_=qik[b][0][:, hp * 128:(hp + 1) * 128], identity=idb[:]),
                     reads=[qik[b][1], ridb], writes=[ptt[1]])
            P.op("act", lambda e, ptt=ptt: e.copy(out=qikT[b][0][:].rearrange("p a b -> p (a b)"), in_=ptt[0][:, 0:384]), reads=[ptt[1]], writes=[qikT[b][1]])
            P.dma("pool", qi_o[s], qikT[b][0][:, 0:2, :], reads=[qikT[b][1]])
            P.dma("pool", ki_o[s], qikT[b][0][0:64, 2, :], reads=[qikT[b][1]])

        for s_ in range(NS if stage >= 1 else 0):
            do_slot(s_)

        fw_ = P.all_dma_tokens()
        P.emit(final_waits={"pool": fw_, "sp": fw_})
    return nc


S = 16384
NIT = 12
NEG = -1.0e30


def bc_mid(ap2d, n):
    a = ap2d.ap
    return AP(ap2d.tensor, ap2d.offset, [list(a[0]), [0, n], list(a[1])])


def build_B(NS, SK=S):
    nc = bass.Bass("TRN2", target_bir_lowering=False)
    dr = lambda n, s, d, k="ExternalInput": nc.dram_tensor(n, list(s), d, kind=k).ap()
    qT_d = dr("qT", [NS, 128, 4, 128], BF16)
    qi_d = dr("qiT", [NS, 128, 2, 128], BF16)
    sg_d = dr("sg", [NS, 128, 4], F32)
    ag_d = dr("ag", [NS, 128, 512], BF16)
    yab_d = dr("yab", [NS, 128, 4, 128], BF16)
    hA = dr("hA", [NS * 128, D], F32)
    pA = dr("pA", [NS * 128, 256], F32)
    KT = dr("KT", [128, 4, SK], BF16)
    VA = dr("VA", [SK // 128, 128, 528], BF16)
    KI = dr("KI", [128, SK], BF16)
    qoff_d = dr("qoff", [128, 2], F32)
    iota_d = dr("iota", [128, 512], F32)
    fl_l = dr("fl_l", [3, 32, 128], BF16)
    fl_r = dr("fl_r", [3, 512], BF16)
    w_out = dr("w_out", [D, D], F32)
    w_g = dr("w_g", [D, D], F32)
    w_p = dr("w_p", [256, D], F32)
    gF = dr("gF", [1, D], F32)
    ident_d = dr("ident", [128, 128], F32)
    h_o = dr("h_o", [NS * 128, D], F32, "ExternalOutput")
    fsel_d = dr("fsel", [128, 1], F32)

    with ExitStack() as st:
        P = Prog(nc, st)
        sb, ps = P.sb, P.ps
        WO = sb("WO", [128, 8, D], BF16); rWO = Res()
        WG = sb("WG", [128, 8, D], BF16); rWG = Res()
        WP = sb("WP", [128, 2, D], BF16); rWP = Res()
        gFb = sb("gFb", [128, D], F32); rgF = Res()
        idf = sb("idf", [128, 128], F32); ridf = Res()
        idb = sb("idb", [128, 128], BF16); ridb = Res()
        qoff = sb("qoff_t", [128, 2], F32); rqoff = Res()
        iota = sb("iota_t", [128, 512], F32); riota = Res()
        fll = sb("fll", [3, 32, 128], BF16); rfll = Res()
        flr = sb("flr", [3, 512], BF16); rflr = Res()
        score = sb("score", [128, S], F32)
        rsc = [Res() for _ in range(32)]
        stg = [score[:, i * 1024:(i + 1) * 1024] for i in range(2)]; rstg = [rsc[0], rsc[2]]
        fsel = sb("fsel_t", [128, 1], F32); rfsel = Res()
        P.dma("sp", fsel[:], fsel_d, writes=[rfsel])
        P.dma("sp", gFb[:], gF.partition_broadcast(128), writes=[rgF])
        P.dma("sp", idf[:], ident_d, writes=[ridf])
        P.dma("sp", qoff[:], qoff_d, writes=[rqoff])
        P.dma("sp", iota[:], iota_d, writes=[riota])
        P.dma("sp", fll[:], fl_l, writes=[rfll])
        P.dma("sp", flr[:], fl_r, writes=[rflr])
        P.op("dve", lambda e: e.tensor_copy(out=idb[:], in_=idf[:]), reads=[ridf], writes=[ridb])
        ci = 0
        for (Wd, Wt, rW, nk) in ((w_out, WO, rWO, 8), (w_g, WG, rWG, 8), (w_p, WP, rWP, 2)):
            for kc in range(nk):
                i = ci % 2
                P.dma("sp", stg[i], Wd[kc * 128:(kc + 1) * 128, :], writes=[rstg[i]])
                eng = ("dve", "pool")[ci % 2]
                P.op(eng, lambda e, i=i, kc=kc, Wt=Wt: e.tensor_copy(out=Wt[:, kc, :], in_=stg[i]), reads=[rstg[i]], writes=[rW])
                ci += 1

        def dbl(name, shape, dt, n=2):
            return [(sb("%s%d" % (name, i), shape, dt), Res()) for i in range(n)]

        class Rot:
            def __init__(self, tiles):
                self.t = tiles; self.i = 0

            def next(self):
                t = self.t[self.i % len(self.t)]; self.i += 1; return t

        qTt = dbl("qTt", [128, 4, 128], BF16)
        qit = dbl("qit", [128, 2, 128], BF16)
        sgt = dbl("sgt", [128, 4], F32)
        agt = dbl("agt", [128, 512], BF16)
        yabt = dbl("yabt", [128, 4, 128], BF16)
        hbl = dbl("hbl", [128, D], F32, 1) * 2
        pbl = dbl("pbl", [128, 256], F32)
        kit = Rot(dbl("kit", [128, 512], BF16, 3))
        ktt = Rot(dbl("ktt", [128, 4, 512], BF16, 2))
        vat = Rot(dbl("vat", [128, 4, 528], BF16, 2))
        Rb = Rot(dbl("Rb", [128, 512], F32, 3))
        Eb = Rot(dbl("Eb", [128, 512], BF16, 3))
        Pb = Rot(dbl("Pb", [128, 512], BF16, 3))
        junk = sb("junk", [128, 1024], BF16); rjunk = Res()
        sm = sb("sm", [128, 512], F32); rsm = Res()
        sm2 = sb("sm2", [128, 512], F32); rsm2 = Res()
        cbt = sm2; rcb = rsm2
        tv = sb("tv", [128, 16], F32)
        rtv = Res()
        cnt8 = sb("cnt8", [128, 16], F32); rcnt8 = Res()
        mk = Rot(dbl("mk", [128, 512], BF16, 2))
        mT = Rot(dbl("mT", [128, 512], BF16, 2))
        oacc = sb("oacc", [128, 8, 66], F32); roacc = Res()
        rec = sb("rec", [128, 8], F32); rrec = Res()
        ycf = score[:, 4096:4608]; rycf = [rsc[8]]
        ycb = sb("ycb", [128, 512], BF16); rycb = Res()
        ycT = sb("ycT", [128, 4, 128], BF16); rycT = Res()
        h1 = score[:, 0:1024]; rh1 = [rsc[0], rsc[1]]
        h1b = sb("h1b", [128, D], BF16); rh1b = Res()
        h1T = sb("h1T", [128, 8, 128], BF16); rh1T = Res()
        gsb = score[:, 1024:2048]; rgsb = [rsc[2], rsc[3]]
        pbb = sb("pbb", [128, 256], BF16); rpbb = Res()
        pT = sb("pT", [128, 2, 128], BF16); rpT = Res()
        h2 = [(score[:, 2048:3072], [rsc[4], rsc[5]])] * 2
        sqt = h1; rsq = rh1
        st1 = sb("st1", [128, 4], F32); rst1 = Res()
        hnt = [(score[:, 3072:4096], [rsc[6], rsc[7]])] * 2

        pa = Rot([(ps("pa%d" % i, [128, 512], F32), Res()) for i in range(4)])
        pf = (ps("pf", [128, 512], F32), Res())
        pm = (ps("pm", [128, 1024], BF16), Res())
        po = [(ps("po%d" % i, [128, 512], F32), Res()) for i in range(2)]

        def do_slot(s):
            b = s % 2
            T = s + 1
            L = 512 * T
            o = s % 2
            qT_, qi_, sg_, ag_, yab_, h_, p_ = qTt[b], qit[b], sgt[b], agt[b], yabt[b], hbl[b], pbl[b]
            P.dma("sp", qi_[0][:], qi_d[s], writes=[qi_[1]])
            P.dma("sp", sg_[0][:], sg_d[s], writes=[sg_[1]])
            P.dma("sp", qT_[0][:], qT_d[s], writes=[qT_[1]])
            P.dma("sp", ag_[0][:], ag_d[s], writes=[ag_[1]])
            P.dma("sp", yab_[0][:], yab_d[s], writes=[yab_[1]])
            P.dma("sp", h_[0][:], hA[s * 128:(s + 1) * 128, :], writes=[h_[1]])
            P.dma("sp", p_[0][:], pA[s * 128:(s + 1) * 128, :], writes=[p_[1]])
            for t in range(T):
                kt = kit.next()
                P.dma("sp", kt[0][:], KI[:, t * 512:(t + 1) * 512], writes=[kt[1]])
                P.op("pe", lambda e, t=t: e.matmul(pf[0][:, :], lhsT=fll[:, t, :], rhs=flr[:, :], start=True, stop=True),
                     reads=[rfll, rflr], writes=[pf[1]])
                sc = score[:, t * 512:(t + 1) * 512]
                for hh in range(4):
                    pz = pa.next()
                    p0 = (hh % 2) * 64
                    P.op("pe", lambda e, pz=pz, p0=p0, hh=hh, kt=kt: e.matmul(pz[0][:, :], lhsT=qi_[0][p0:p0 + 64, hh // 2, :], rhs=kt[0][p0:p0 + 64, :],
                                                                             start=True, stop=True),
                         reads=[qi_[1], kt[1]], writes=[pz[1]])
                    rb = Rb.next()
                    P.op("act", lambda e, pz=pz, rb=rb: e.activation(out=rb[0][:], in_=pz[0][:, :], func=AF.Relu), reads=[pz[1]], writes=[rb[1]])
                    if hh == 0:
                        P.op("dve", lambda e, rb=rb, sc=sc: e.scalar_tensor_tensor(out=sc, in0=rb[0][:], scalar=sg_[0][:, 0:1], in1=pf[0][:, :],
                                                                                   op0=ALU.mult, op1=ALU.add),
                             reads=[rb[1], sg_[1], pf[1]], writes=[rsc[t]])
                    else:
                        P.op("dve", lambda e, rb=rb, sc=sc, hh=hh: e.scalar_tensor_tensor(out=sc, in0=rb[0][:], scalar=sg_[0][:, hh:hh + 1], in1=sc,
                                                                                          op0=ALU.mult, op1=ALU.add),
                             reads=[rb[1], sg_[1], rsc[t]], writes=[rsc[t]])
                if t == T - 1:
                    P.op("dve", lambda e: e.tensor_scalar(out=cbt[:], in0=iota[:], scalar1=qoff[:, o:o + 1], scalar2=NEG, op0=ALU.is_gt, op1=ALU.mult),
                         reads=[riota, rqoff], writes=[rcb])
                    P.op("dve", lambda e, sc=sc: e.tensor_tensor(out=sc, in0=sc, in1=cbt[:], op=ALU.add), reads=[rsc[t], rcb], writes=[rsc[t]])
            rrow = rsc[0:T]
            if T == 1:
                P.op("dve", lambda e: e.tensor_copy(out=sm[:], in_=score[:, 0:512]), reads=rrow, writes=[rsm])
            else:
                P.op("dve", lambda e: e.tensor_reduce(out=sm[:], in_=score[:, 0:L].rearrange("p (g k) -> p g k", k=T), axis=AX.X, op=ALU.max),
                     reads=rrow, writes=[rsm])
            P.op("dve", lambda e: e.tensor_scalar(out=sm2[:], in0=sm[:], scalar1=-1.0e29, scalar2=2.0e30, op0=ALU.is_lt, op1=ALU.mult),
                 reads=[rsm], writes=[rsm2])
            P.op("dve", lambda e: e.tensor_tensor(out=sm2[:], in0=sm2[:], in1=sm[:], op=ALU.add), reads=[rsm, rsm2], writes=[rsm2])
            P.op("dve", lambda e: e.tensor_reduce(out=tv[:, 0:1], in_=sm2[:], axis=AX.X, op=ALU.min), reads=[rsm2], writes=[rtv])
            P.op("dve", lambda e: e.tensor_reduce(out=tv[:, 1:2], in_=sm[:], axis=AX.X, op=ALU.max), reads=[rsm], writes=[rtv])
            P.op("dve", lambda e: e.tensor_tensor(out=tv[:, 2:3], in0=tv[:, 1:2], in1=tv[:, 0:1], op=ALU.subtract), reads=[rtv], writes=[rtv])
            P.op("dve", lambda e: e.tensor_copy(out=tv[:, 3:4], in_=tv[:, 0:1]), reads=[rtv], writes=[rtv])
            CH = 1024
            nch = (L + CH - 1) // CH
            for k in range(1, NIT + 1):
                P.op("dve", lambda e, k=k: e.tensor_scalar(out=tv[:, 4:5], in0=tv[:, 2:3], scalar1=float(2.0 ** -k), scalar2=None, op0=ALU.mult),
                     reads=[rtv], writes=[rtv])
                P.op("dve", lambda e: e.tensor_tensor(out=tv[:, 5:6], in0=tv[:, 3:4], in1=tv[:, 4:5], op=ALU.add), reads=[rtv], writes=[rtv])
                for c in range(nch):
                    c0 = c * CH
                    w = min(CH, L - c0)
                    P.op("dve", lambda e, c=c, c0=c0, w=w: e.tensor_scalar(out=junk[:, 0:w], in0=score[:, c0:c0 + w], scalar1=tv[:, 5:6], scalar2=0.0,
                                                                           op0=ALU.is_ge, op1=ALU.add, accum_out=cnt8[:, c:c + 1]),
                         reads=rrow + [rtv], writes=[rjunk, rcnt8])
                if nch > 1:
                    P.op("dve", lambda e: e.tensor_reduce(out=tv[:, 6:7], in_=cnt8[:, 0:nch], axis=AX.X, op=ALU.add), reads=[rcnt8], writes=[rtv])
                else:
                    P.op("dve", lambda e: e.tensor_copy(out=tv[:, 6:7], in_=cnt8[:, 0:1]), reads=[rcnt8], writes=[rtv])
                P.op("dve", lambda e: e.scalar_tensor_tensor(out=tv[:, 7:8], in0=tv[:, 6:7], scalar=255.5, in1=tv[:, 4:5], op0=ALU.is_ge, op1=ALU.mult),
                     reads=[rtv], writes=[rtv])
                P.op("dve", lambda e: e.tensor_tensor(out=tv[:, 3:4], in0=tv[:, 3:4], in1=tv[:, 7:8], op=ALU.add), reads=[rtv], writes=[rtv])
            for t in range(T):
                ktile = ktt.next()
                vtile = vat.next()
                P.dma("sp", ktile[0][:], KT[:, :, t * 512:(t + 1) * 512], writes=[ktile[1]])
                P.dma("sp", vtile[0][:], VA[4 * t:4 * t + 4].rearrange("c p f -> p c f"), writes=[vtile[1]])
                m_ = mk.next()
                P.op("dve", lambda e, m_=m_, t=t: e.tensor_scalar(out=m_[0][:], in0=score[:, t * 512:(t + 1) * 512], scalar1=tv[:, 3:4], scalar2=None,
                                                                  op0=ALU.is_ge), reads=[rsc[t], rtv], writes=[m_[1]])
                def emit_qk(hh, ktile=ktile):
                    p0 = (hh % 2) * 64
                    pz = pa.next()
                    for c in range(4):
                        P.op("pe", lambda e, c=c, pz=pz, p0=p0, hh=hh, ktile=ktile: e.matmul(
                            pz[0][:, c * 128:(c + 1) * 128], lhsT=ktile[0][p0:p0 + 64, hh // 2, c * 128:(c + 1) * 128], rhs=qT_[0][p0:p0 + 64, hh // 2, :],
                            start=True, stop=True), reads=[ktile[1], qT_[1]], writes=[pz[1]])
                    return pz

                def emit_rest(hh, pz, mt, vtile=vtile):
                    eb = Eb.next()
                    P.op("act", lambda e, pz=pz, eb=eb: e.activation(out=eb[0][:], in_=pz[0][:, :], func=AF.Exp, scale=0.125), reads=[pz[1]], writes=[eb[1]])
                    pb_ = Pb.next()
                    eng = "dve" if hh % 2 == 0 else "pool"
                    P.op(eng, lambda e, eb=eb, pb_=pb_, mt=mt: e.tensor_tensor(out=pb_[0][:], in0=eb[0][:], in1=mt[0][:], op=ALU.mult),
                         reads=[eb[1], mt[1]], writes=[pb_[1]])
                    pob = po[hh // 4]
                    for c in range(4):
                        P.op("pe", lambda e, c=c, pb_=pb_, vtile=vtile, hh=hh, pob=pob: e.matmul(
                            pob[0][:, (hh % 4) * 66:(hh % 4 + 1) * 66], lhsT=pb_[0][:, c * 128:(c + 1) * 128], rhs=vtile[0][:, c, hh * 66:(hh + 1) * 66],
                            start=(c == 0), stop=(c == 3)), reads=[pb_[1], vtile[1]], writes=[pob[1]])

                LAG = 3
                pzs = {}
                for hh in range(LAG):
                    pzs[hh] = emit_qk(hh)
                for c in range(4):
                    P.op("pe", lambda e, c=c, m_=m_: e.transpose(out=pm[0][:, c * 128:(c + 1) * 128], in_=m_[0][:, c * 128:(c + 1) * 128], identity=idb[:]),
                         reads=[m_[1], ridb], writes=[pm[1]])
                mt = mT.next()
                P.op("act", lambda e, mt=mt: e.copy(out=mt[0][:], in_=pm[0][:, 0:512]), reads=[pm[1]], writes=[mt[1]])
                for hh in range(8):
                    if hh + LAG < 8:
                        pzs[hh + LAG] = emit_qk(hh + LAG)
                    emit_rest(hh, pzs[hh], mt)
                for g in range(2):
                    ov = oacc[:, 4 * g:4 * g + 4, :].rearrange("p h d -> p (h d)")
                    if t == 0:
                        P.op("act", lambda e, g=g, ov=ov: e.copy(out=ov, in_=po[g][0][:, 0:264]), reads=[po[g][1]], writes=[roacc])
                    else:
                        P.op("dve", lambda e, g=g, ov=ov: e.tensor_tensor(out=ov, in0=ov, in1=po[g][0][:, 0:264], op=ALU.add),
                             reads=[po[g][1], roacc], writes=[roacc])
            P.op("dve", lambda e: e.reciprocal(out=rec[:], in_=oacc[:, :, 64]), reads=[roacc], writes=[rrec])
            for hh in range(8):
                P.op("dve", lambda e, hh=hh: e.tensor_scalar(out=ycf[:, hh * 64:(hh + 1) * 64], in0=oacc[:, hh, 0:64], scalar1=rec[:, hh:hh + 1], scalar2=None,
                                                             op0=ALU.mult), reads=[roacc, rrec], writes=[rycf])
            P.op("dve", lambda e: e.tensor_tensor(out=ycb[:], in0=ycf[:], in1=ag_[0][:], op=ALU.mult), reads=[rycf, ag_[1]], writes=[rycb])
            for c in range(4):
                P.op("pe", lambda e, c=c: e.transpose(out=pm[0][:, c * 128:(c + 1) * 128], in_=ycb[:, c * 128:(c + 1) * 128], identity=idb[:]),
                     reads=[rycb, ridb], writes=[pm[1]])
            P.op("act", lambda e: e.copy(out=ycT[:].rearrange("p a b -> p (a b)"), in_=pm[0][:, 0:512]), reads=[pm[1]], writes=[rycT])
            for n in range(2):
                pz = pa.next()
                for kc in range(8):
                    lt = yab_[0][:, kc, :] if kc < 4 else ycT[:, kc - 4, :]
                    P.op("pe", lambda e, kc=kc, pz=pz, lt=lt, n=n: e.matmul(pz[0][:, :], lhsT=lt, rhs=WO[:, kc, n * 512:(n + 1) * 512],
                                                                           start=(kc == 0), stop=(kc == 7)),
                         reads=[yab_[1], rycT, rWO], writes=[pz[1]])
                P.op("dve", lambda e, pz=pz, n=n: e.tensor_tensor(out=h1[:, n * 512:(n + 1) * 512], in0=pz[0][:, :], in1=h_[0][:, n * 512:(n + 1) * 512], op=ALU.add),
                     reads=[pz[1], h_[1]], writes=[rh1])
            P.op("act", lambda e: e.copy(out=h1b[:], in_=h1[:]), reads=[rh1], writes=[rh1b])
            for c in range(8):
                P.op("pe", lambda e, c=c: e.transpose(out=pm[0][:, c * 128:(c + 1) * 128], in_=h1b[:, c * 128:(c + 1) * 128], identity=idb[:]),
                     reads=[rh1b, ridb], writes=[pm[1]])
            P.op("act", lambda e: e.copy(out=h1T[:].rearrange("p a b -> p (a b)"), in_=pm[0][:, :]), reads=[pm[1]], writes=[rh1T])
            for n in range(2):
                pz = pa.next()
                for kc in range(8):
                    P.op("pe", lambda e, kc=kc, pz=pz, n=n: e.matmul(pz[0][:, :], lhsT=h1T[:, kc, :], rhs=WG[:, kc, n * 512:(n + 1) * 512],
                                                                    start=(kc == 0), stop=(kc == 7)), reads=[rh1T, rWG], writes=[pz[1]])
                P.op("act", lambda e, pz=pz, n=n: e.activation(out=gsb[:, n * 512:(n + 1) * 512], in_=pz[0][:, :], func=AF.Sigmoid), reads=[pz[1]], writes=[rgsb])
            P.op("pool", lambda e: e.tensor_copy(out=pbb[:], in_=p_[0][:]), reads=[p_[1]], writes=[rpbb])
            for c in range(2):
                P.op("pe", lambda e, c=c: e.transpose(out=pm[0][:, c * 128:(c + 1) * 128], in_=pbb[:, c * 128:(c + 1) * 128], identity=idb[:]),
                     reads=[rpbb, ridb], writes=[pm[1]])
            P.op("act", lambda e: e.copy(out=pT[:].rearrange("p a b -> p (a b)"), in_=pm[0][:, 0:256]), reads=[pm[1]], writes=[rpT])
            h2_ = h2[b]
            for n in range(2):
                pz = pa.next()
                for kc in range(2):
                    P.op("pe", lambda e, kc=kc, pz=pz, n=n: e.matmul(pz[0][:, :], lhsT=pT[:, kc, :], rhs=WP[:, kc, n * 512:(n + 1) * 512],
                                                                    start=(kc == 0), stop=(kc == 1)), reads=[rpT, rWP], writes=[pz[1]])
                P.op("dve", lambda e, pz=pz, n=n: e.tensor_tensor(out=gsb[:, n * 512:(n + 1) * 512], in0=pz[0][:, :], in1=gsb[:, n * 512:(n + 1) * 512], op=ALU.mult),
                     reads=[pz[1], rgsb], writes=[rgsb])
            P.op("pool", lambda e: e.tensor_tensor(out=h2_[0][:], in0=h1[:], in1=gsb[:], op=ALU.add), reads=[rh1, rgsb], writes=[h2_[1]])
            hn_ = hnt[b]
            P.op("act", lambda e: e.activation(out=sqt[:], in_=h2_[0][:], func=AF.Square), reads=[h2_[1]], writes=[rsq])
            P.op("dve", lambda e: e.tensor_reduce(out=st1[:, 0:1], in_=sqt[:], axis=AX.X, op=ALU.add), reads=[rsq], writes=[rst1])
            P.op("dve", lambda e: e.tensor_scalar(out=st1[:, 1:2], in0=st1[:, 0:1], scalar1=1.0 / D, scalar2=EPS, op0=ALU.mult, op1=ALU.add),
                 reads=[rst1], writes=[rst1])
            P.op("act", lambda e: e.activation(out=st1[:, 2:3], in_=st1[:, 1:2], func=AF.Sqrt), reads=[rst1], writes=[rst1])
            P.op("dve", lambda e: e.reciprocal(out=st1[:, 3:4], in_=st1[:, 2:3]), reads=[rst1], writes=[rst1])
            P.op("dve", lambda e: e.scalar_tensor_tensor(out=hn_[0][:], in0=h2_[0][:], scalar=st1[:, 3:4], in1=gFb[:], op0=ALU.mult, op1=ALU.mult),
                 reads=[h2_[1], rst1, rgF], writes=[hn_[1]])
            P.op("pool", lambda e: e.tensor_tensor(out=hn_[0][:], in0=hn_[0][:], in1=h2_[0][:], op=ALU.subtract), reads=[hn_[1], h2_[1]], writes=[hn_[1]])
            P.op("dve", lambda e: e.scalar_tensor_tensor(out=hn_[0][:], in0=hn_[0][:], scalar=fsel[:, 0:1], in1=h2_[0][:], op0=ALU.mult, op1=ALU.add),
                 reads=[hn_[1], h2_[1], rfsel], writes=[hn_[1]])
            P.dma("pool", h_o[s * 128:(s + 1) * 128, :], hn_[0][:], reads=[hn_[1]])

        for s_ in range(NS):
            do_slot(s_)
        fw_ = P.all_dma_tokens()
        P.emit(final_waits={"pool": fw_, "sp": fw_})
    return nc


BF = ml_dtypes.bfloat16
S = 16384
HALO = 32
NQB = S // 128


def slot_qb(core, s):
    j = core % 4
    return 8 * (s // 2) + (j if s % 2 == 0 else 7 - j)


def rope_table():
    half = 32
    inv = (np.float32(10000.0) ** (-np.arange(half, dtype=np.float32) / np.float32(half))).astype(np.float32)
    ang = np.arange(S, dtype=np.float32)[:, None] * inv[None, :]
    return np.concatenate([np.cos(ang), np.sin(ang)], axis=1).astype(np.float32)


def prep_A_weights(i, inp):
    w = {}
    w["w_in"] = np.ascontiguousarray(inp["w_in"][i])
    b = inp["b_in"][i]
    w["b_bc"] = np.ascontiguousarray(b[None, :])
    w["b_fm"] = np.ascontiguousarray(b[:1280].reshape(10, 128).T)
    w["g_bc"] = np.ascontiguousarray(inp["norm_g"][i][None, :])
    w["wdw"] = np.ascontiguousarray(inp["conv_dw_w"][i].T.reshape(2, 128, 31).transpose(1, 0, 2))
    cv = np.zeros((128, 7, 2), np.float32)
    for k, name in enumerate(["conv_dw_b", "conv_ln_g", "conv_ln_b", "conv_pw_b", "pool_b", "pool_scale"]):
        cv[:, k, :] = inp[name][i].reshape(2, 128).T
    cv[:64, 6, 0] = 1 / 2; cv[64:, 6, 0] = 1 / 4; cv[:64, 6, 1] = 1 / 8; cv[64:, 6, 1] = 1 / 16
    w["cvec"] = cv
    w["pw_w"] = np.ascontiguousarray(inp["conv_pw_w"][i].reshape(2, 128, 256).transpose(1, 0, 2))
    pl = np.zeros((128, 2, 128), np.float32)
    pw = inp["pool_w"][i]
    for g in range(4):
        cc, p0 = g // 2, (g % 2) * 64
        pl[p0:p0 + 64, cc, p0:p0 + 64] = pw[g]
    w["plw"] = pl
    w["ident"] = np.eye(128, dtype=np.float32)
    return w


def prep_A_core(core, h, NS, cs_tab):
    bt = core // 4
    hA = np.empty((NS * 128, 1024), np.float32)
    hH = np.zeros((NS * HALO, 1024), np.float32)
    hok = np.ones((128, NS), np.float32)
    cs = np.empty((NS * 128, 64), np.float32)
    rc0 = np.empty((128, 2, 128), np.float32)
    wins = [2, 4, 8, 16]
    for s in range(NS):
        qb = slot_qb(core, s)
        t0 = qb * 128
        hA[s * 128:(s + 1) * 128] = h[bt, t0:t0 + 128]
        cs[s * 128:(s + 1) * 128] = cs_tab[t0:t0 + 128]
        if qb == 0:
            hok[:, s] = 0.0
        else:
            hH[s * HALO:(s + 1) * HALO] = h[bt, t0 - HALO:t0]
    qb0 = slot_qb(core, 0)
    t = qb0 * 128 + np.arange(128)
    for g in range(4):
        cc, p0 = g // 2, (g % 2) * 64
        rc0[p0:p0 + 64, cc, :] = (1.0 / np.minimum(t + 1, wins[g]).astype(np.float32))[None, :]
    return {"hA": hA, "hH": hH, "hok": hok, "cs": cs, "rc0": rc0}


def prep_B_consts(core):
    j = core % 4
    pidx = np.arange(128, dtype=np.float32)
    qoff = np.stack([128.0 * j + pidx, 128.0 * (3 - j) + pidx], 1).astype(np.float32)
    iota = np.tile(np.arange(512, dtype=np.float32)[None, :], (128, 1))
    eps = 2.0 ** -30
    fl_l = np.zeros((3, 32, 128), np.float32)
    fl_l[0] = -eps * 16
    fl_l[1] = -eps
    fl_l[2] = (-eps * 512 * np.arange(32, dtype=np.float32))[:, None]
    kk = np.arange(512)
    fl_r = np.stack([kk // 16, kk % 16, np.ones(512)], 0).astype(np.float32)
    return {"qoff": qoff, "iota": iota, "fl_l": fl_l.astype(BF), "fl_r": fl_r.astype(BF), "ident": np.eye(128, dtype=np.float32)}


def prep_B_weights(i, inp):
    return {"w_out": np.ascontiguousarray(inp["w_out"][i]), "w_g": np.ascontiguousarray(inp["ple_gate_w"][i]),
            "w_p": np.ascontiguousarray(inp["ple_w"][i]), "gF": np.ascontiguousarray(inp["final_norm_g"][None, :])}

AP = bass.AP
NSLOT = 32
_CACHE = {}


def _progs():
    if "A" not in _CACHE:
        _CACHE["A"] = build_A(NSLOT)
        _CACHE["B"] = build_B(NSLOT)
    return _CACHE["A"], _CACHE["B"]


def kernel(**inputs):
    inp = {k: np.asarray(v) for k, v in inputs.items()}
    ncA, ncB = _progs()
    h = np.array(inp["x"], dtype=np.float32, copy=True)
    cs_tab = rope_table()
    cores = list(range(8))
    for i in range(4):
        wA = prep_A_weights(i, inp)
        mapsA = []
        for c in cores:
            m = dict(wA)
            m.update(prep_A_core(c, h, NSLOT, cs_tab))
            mapsA.append(m)
        rA = run_bass_kernel_spmd(ncA, mapsA, core_ids=cores).results
        KT = [np.empty((128, 4, S), BF) for _ in range(2)]
        VA = [np.empty((S // 128, 128, 528), BF) for _ in range(2)]
        KI = [np.empty((128, S), BF) for _ in range(2)]
        for c in cores:
            bt = c // 4
            kT_o, va_o, ki_o = np.asarray(rA[c]["kT_o"]), np.asarray(rA[c]["va_o"]), np.asarray(rA[c]["ki_o"])
            for s in range(NSLOT):
                qb = slot_qb(c, s)
                KT[bt][:, :, qb * 128:(qb + 1) * 128] = kT_o[s]
                VA[bt][qb] = va_o[s]
                KI[bt][0:64, qb * 128:(qb + 1) * 128] = ki_o[s]
                KI[bt][64:128, qb * 128:(qb + 1) * 128] = ki_o[s]
        wB = prep_B_weights(i, inp)
        mapsB = []
        for c in cores:
            bt = c // 4
            m = dict(wB)
            m.update(prep_B_consts(c))
            toks = np.concatenate([np.arange(128) + 128 * slot_qb(c, s) for s in range(NSLOT)])
            m["qT"] = np.asarray(rA[c]["qT_o"]); m["qiT"] = np.asarray(rA[c]["qi_o"]); m["sg"] = np.asarray(rA[c]["sg_o"])
            m["ag"] = np.asarray(rA[c]["ag_o"]); m["yab"] = np.asarray(rA[c]["yab_o"])
            m["hA"] = np.ascontiguousarray(h[bt][toks]); m["pA"] = np.ascontiguousarray(inp["p"][i, bt][toks])
            m["KT"] = KT[bt]; m["VA"] = VA[bt]; m["KI"] = KI[bt]
            m["fsel"] = np.full((128, 1), 1.0 if i == 3 else 0.0, np.float32)
            mapsB.append(m)
        rB = run_bass_kernel_spmd(ncB, mapsB, core_ids=cores).results
        for c in cores:
            bt = c // 4
            toks = np.concatenate([np.arange(128) + 128 * slot_qb(c, s) for s in range(NSLOT)])
            h[bt][toks] = np.asarray(rB[c]["h_o"])
    return h.astype(np.float32)
```
